# Optimizing a Trainium2 kernel written in Bass

```python
import math
import jax, jax.numpy as jnp
from jax import lax
import numpy as np

D_MODEL = 1024
BATCH = 8
SEQ = 4096
DEPTH = 2

DA_HEADS = 4
DA_HEAD_DIM = 64
HG_HEADS = 4
HG_DK = 128
HG_DV = 128
HG_CHUNK = 64
LRU_WIDTH = 512
LRU_BLOCKS = 8
CONV_WIDTH = 4
LRU_C = 8.0
SB_HEADS = 8
SB_HEAD_DIM = 64
Q_BLOCK = 128
N_EXPERTS = 16
N_GROUPS = 4
E_PER_GROUP = N_EXPERTS // N_GROUPS
TOP_K = 2
D_FF = 512

DA_QK = DA_HEADS * 2 * DA_HEAD_DIM
DA_V = DA_HEADS * 2 * DA_HEAD_DIM
HG_W = HG_HEADS * HG_DK
HG_VW = HG_HEADS * HG_DV
EVEN_SPLITS = [DA_QK, DA_QK, DA_V, HG_W, HG_W, HG_VW, HG_VW]
EVEN_IN = sum(EVEN_SPLITS)
EVEN_OUT = DA_V + HG_VW
SB_W = SB_HEADS * SB_HEAD_DIM
ODD_SPLITS = [LRU_WIDTH, LRU_WIDTH, SB_W, SB_W, SB_W]
ODD_IN = sum(ODD_SPLITS)
ODD_OUT = LRU_WIDTH + SB_W
DEEPNORM_ALPHA = (2.0 * DEPTH) ** 0.25
DEEPNORM_BETA = (8.0 * DEPTH) ** -0.25

kernel_name = "hybrid_diffattn_hgrn2_rglru_stickbreak_grouped_moe"


def _split_idx(widths):
    return [int(v) for v in np.cumsum(widths)[:-1]]


def layer_norm(x, g, b, eps=1e-5):
    xf = x.astype(jnp.float32)
    mu = jnp.mean(xf, axis=-1, keepdims=True)
    var = jnp.mean(jnp.square(xf - mu), axis=-1, keepdims=True)
    return ((xf - mu) * lax.rsqrt(var + eps)).astype(x.dtype) * g + b


def rms_norm(x, g, eps=1e-6):
    xf = x.astype(jnp.float32)
    return (xf * lax.rsqrt(jnp.mean(xf * xf, axis=-1, keepdims=True) + eps)).astype(x.dtype) * g


def diff_attention(q, k, v, lam, gain, lam_init):
    B, H, T, _, dh = q.shape
    nb = T // Q_BLOCK
    scale = dh ** -0.5
    slopes = 2.0 ** (-8.0 * jnp.arange(1, H + 1, dtype=jnp.float32) / H)
    kpos = jnp.arange(T)
    qb = q.reshape(B, H, nb, Q_BLOCK, 2, dh).transpose(2, 0, 1, 3, 4, 5)
    starts = jnp.arange(nb) * Q_BLOCK

    def block(args):
        qi, start = args
        s = jnp.einsum('bhqcd,bhkcd->bhcqk', qi, k).astype(jnp.float32) * scale
        dist = ((start + jnp.arange(Q_BLOCK))[:, None] - kpos[None, :]).astype(jnp.float32)
        s = s - slopes[None, :, None, None, None] * dist[None, None, None]
        s = jnp.where((dist >= 0)[None, None, None], s, -jnp.inf)
        p = jax.nn.softmax(s, axis=-1)
        a = p[:, :, 0] - lam * p[:, :, 1]
        return jnp.einsum('bhqk,bhkv->bhqv', a.astype(v.dtype), v)

    o = lax.map(block, (qb, starts))
    o = o.transpose(1, 0, 3, 2, 4).reshape(B, T, H, 2 * dh)
    return rms_norm(o, gain) * (1.0 - lam_init)


def hgrn2_chunked(q, log_f, k, v):
    B, H, T, dk = q.shape
    dv = v.shape[-1]
    n = T // HG_CHUNK
    q = q.reshape(B, H, n, HG_CHUNK, dk)
    log_f = log_f.reshape(B, H, n, HG_CHUNK, dk)
    k = k.reshape(B, H, n, HG_CHUNK, dk)
    v = v.reshape(B, H, n, HG_CHUNK, dv)
    b = jnp.cumsum(log_f, axis=3)
    b_mid = b[:, :, :, HG_CHUNK // 2 - 1:HG_CHUNK // 2]
    b_last = b[:, :, :, -1:]
    causal = jnp.tril(jnp.ones((HG_CHUNK, HG_CHUNK), dtype=bool))
    att = jnp.einsum('bhncd,bhnsd->bhncs', q * jnp.exp(b - b_mid), k * jnp.exp(b_mid - b))
    att = jnp.where(causal, att, 0.0)
    o_intra = jnp.einsum('bhncs,bhnsv->bhncv', att, v)
    dS = jnp.einsum('bhncd,bhncv->bhndv', k * jnp.exp(b_last - b), v)
    decay = jnp.exp(b_last[:, :, :, 0])

    def step(S, inp):
        d, ds = inp
        return d[..., None] * S + ds, S

    S0 = jnp.zeros((B, H, dk, dv), dS.dtype)
    _, S_prev = lax.scan(step, S0, (jnp.moveaxis(decay, 2, 0), jnp.moveaxis(dS, 2, 0)))
    S_prev = jnp.moveaxis(S_prev, 0, 2)
    o_inter = jnp.einsum('bhncd,bhndv->bhncv', q * jnp.exp(b), S_prev)
    return (o_intra + o_inter).reshape(B, H, T, dv)


def causal_depthwise_conv(x, w, b):
    C = x.shape[-1]
    W = w.shape[0]
    y = lax.conv_general_dilated(x, w[:, None, :].astype(x.dtype), window_strides=(1,),
                                 padding=[(W - 1, 0)], dimension_numbers=('NWC', 'WIO', 'NWC'),
                                 feature_group_count=C)
    return y + b


def rg_lru(x, w_a, b_a, w_x, b_x, lam):
    B, T, C = x.shape
    xf = x.astype(jnp.float32)
    xb = xf.reshape(B, T, LRU_BLOCKS, C // LRU_BLOCKS)
    r = jax.nn.sigmoid(jnp.einsum('btgi,gij->btgj', xb, w_a).reshape(B, T, C) + b_a)
    i = jax.nn.sigmoid(jnp.einsum('btgi,gij->btgj', xb, w_x).reshape(B, T, C) + b_x)
    log_a = -LRU_C * r * jax.nn.softplus(-lam.astype(jnp.float32))
    a = jnp.exp(log_a)
    u = jnp.sqrt(jnp.maximum(-jnp.expm1(2.0 * log_a), 1e-12)) * (i * xf)

    def combine(e, l):
        a1, h1 = e
        a2, h2 = l
        return a1 * a2, a2 * h1 + h2

    _, h = lax.associative_scan(combine, (a, u), axis=1)
    return h.astype(x.dtype)


def stick_breaking_attention(q, k, v):
    B, H, T, d = q.shape
    nb = T // Q_BLOCK
    kpos = jnp.arange(T)
    qb = q.reshape(B, H, nb, Q_BLOCK, d).transpose(2, 0, 1, 3, 4)
    starts = jnp.arange(nb) * Q_BLOCK
    scale = d ** -0.5

    def block(args):
        qi, start = args
        z = jnp.einsum('bhqd,bhkd->bhqk', qi, k).astype(jnp.float32) * scale
        strict = (kpos[None, :] < (start + jnp.arange(Q_BLOCK))[:, None])[None, None]
        log_1m = jnp.where(strict, jax.nn.log_sigmoid(-z), 0.0)
        after = lax.cumsum(log_1m, axis=3, reverse=True) - log_1m
        a = jnp.where(strict, jnp.exp(jax.nn.log_sigmoid(z) + after), 0.0)
        return jnp.einsum('bhqk,bhkd->bhqd', a.astype(v.dtype), v)

    o = lax.map(block, (qb, starts))
    return o.transpose(1, 0, 3, 2, 4).reshape(B, T, H * d)


def even_mixer(h, w_in, w_out, lam_vecs, diff_gain, hg_gain, lb, lam_init):
    B, T, _ = h.shape
    proj = h @ w_in
    qa, ka, va, qh, fh, ih, gh = jnp.split(proj, _split_idx(EVEN_SPLITS), axis=-1)
    qa = qa.reshape(B, T, DA_HEADS, 2, DA_HEAD_DIM).transpose(0, 2, 1, 3, 4)
    ka = ka.reshape(B, T, DA_HEADS, 2, DA_HEAD_DIM).transpose(0, 2, 1, 3, 4)
    va = va.reshape(B, T, DA_HEADS, 2 * DA_HEAD_DIM).transpose(0, 2, 1, 3)
    lv = lam_vecs.astype(jnp.float32)
    lam = jnp.exp(jnp.sum(lv[0] * lv[1])) - jnp.exp(jnp.sum(lv[2] * lv[3])) + lam_init
    oa = diff_attention(qa, ka, va, lam, diff_gain, lam_init).reshape(B, T, DA_V)
    fl = fh.astype(jnp.float32)
    f = lb + (1.0 - lb) * jax.nn.sigmoid(fl)
    kh = (1.0 - lb) * jax.nn.sigmoid(-fl)
    heads = lambda t: t.reshape(B, T, HG_HEADS, -1).transpose(0, 2, 1, 3)
    ob = hgrn2_chunked(heads(jax.nn.silu(qh.astype(jnp.float32))), heads(jnp.log(f)), heads(kh),
                       heads(ih.astype(jnp.float32)))
    ob = ob.transpose(0, 2, 1, 3).astype(h.dtype)
    ob = rms_norm(ob, hg_gain) * jax.nn.silu(gh.reshape(B, T, HG_HEADS, HG_DV))
    return jnp.concatenate([oa, ob.reshape(B, T, HG_VW)], axis=-1) @ w_out


def odd_mixer(h, w_in, w_out, conv_w, conv_b, w_a, b_a, w_x, b_x, lam):
    B, T, _ = h.shape
    proj = h @ w_in
    xr, gr, qs, ks, vs = jnp.split(proj, _split_idx(ODD_SPLITS), axis=-1)
    xr = causal_depthwise_conv(xr, conv_w, conv_b)
    yr = jax.nn.gelu(gr) * rg_lru(xr, w_a, b_a, w_x, b_x, lam)
    heads = lambda t: t.reshape(B, T, SB_HEADS, SB_HEAD_DIM).transpose(0, 2, 1, 3)
    ys = stick_breaking_attention(heads(qs), heads(ks), heads(vs))
    return jnp.concatenate([yr, ys], axis=-1) @ w_out


def grouped_moe(h, router_w, router_b, w_gate, w_up, w_down):
    B, T, D = h.shape
    xt = h.reshape(B * T, D)
    logits = (xt @ router_w).astype(jnp.float32) + router_b
    probs = jax.nn.softmax(logits, axis=-1)
    pg = probs.reshape(-1, N_GROUPS, E_PER_GROUP)
    group_score = jnp.sum(lax.top_k(pg, TOP_K)[0], axis=-1)
    g_sel = jnp.argmax(group_score, axis=-1)
    in_group = jax.nn.one_hot(g_sel, N_GROUPS, dtype=jnp.float32)[:, :, None] > 0
    masked = jnp.where(in_group, pg, -1.0).reshape(-1, N_EXPERTS)
    top_p, top_i = lax.top_k(masked, TOP_K)
    weights = top_p / jnp.sum(top_p, axis=-1, keepdims=True)
    gates = jnp.sum(jax.nn.one_hot(top_i, N_EXPERTS, dtype=jnp.float32) * weights[..., None], axis=1)
    y = jnp.zeros_like(xt)
    for e in range(N_EXPERTS):
        he = jax.nn.silu(xt @ w_gate[e]) * (xt @ w_up[e])
        y = y + gates[:, e:e + 1].astype(xt.dtype) * (he @ w_down[e])
    return y.reshape(B, T, D)


def setup_inputs(seed: int = 0) -> dict:
    key = jax.random.key(seed)
    ks = jax.random.split(key, 32)
    n_even = (DEPTH + 1) // 2
    n_odd = DEPTH // 2
    nrm = lambda k, shape, s: jax.random.normal(k, shape, jnp.float32) * s
    gb = LRU_WIDTH // LRU_BLOCKS
    u = jax.random.uniform(ks[20], (n_odd, LRU_WIDTH), jnp.float32, 0.9, 0.999)
    s = u ** (1.0 / LRU_C)
    lru_lambda = jnp.log(s) - jnp.log1p(-s)
    return {
        "x": nrm(ks[0], (BATCH, SEQ, D_MODEL), 1.0),
        "c": nrm(ks[1], (BATCH, D_MODEL), 1.0),
        "ada_w": nrm(ks[2], (DEPTH, D_MODEL, 6 * D_MODEL), 0.1 * D_MODEL ** -0.5),
        "ada_b": nrm(ks[3], (DEPTH, 6 * D_MODEL), 0.01),
        "ln_g": 1.0 + nrm(ks[4], (DEPTH, 2, D_MODEL), 0.02),
        "ln_b": nrm(ks[5], (DEPTH, 2, D_MODEL), 0.02),
        "even_w_in": nrm(ks[6], (n_even, D_MODEL, EVEN_IN), D_MODEL ** -0.5),
        "even_w_out": nrm(ks[7], (n_even, EVEN_OUT, D_MODEL), DEEPNORM_BETA * EVEN_OUT ** -0.5),
        "diff_lambda": nrm(ks[8], (n_even, 4, DA_HEAD_DIM), 0.1),
        "diff_gain": 1.0 + nrm(ks[9], (n_even, 2 * DA_HEAD_DIM), 0.02),
        "hgrn_gamma": nrm(ks[10], (DEPTH + 1, HG_W), 0.1),
        "hgrn_gain": 1.0 + nrm(ks[11], (n_even, HG_DV), 0.02),
        "odd_w_in": nrm(ks[12], (n_odd, D_MODEL, ODD_IN), D_MODEL ** -0.5),
        "odd_w_out": nrm(ks[13], (n_odd, ODD_OUT, D_MODEL), DEEPNORM_BETA * ODD_OUT ** -0.5),
        "conv_w": nrm(ks[14], (n_odd, CONV_WIDTH, LRU_WIDTH), CONV_WIDTH ** -0.5),
        "conv_b": nrm(ks[15], (n_odd, LRU_WIDTH), 0.01),
        "lru_wa": nrm(ks[16], (n_odd, LRU_BLOCKS, gb, gb), gb ** -0.5),
        "lru_ba": nrm(ks[17], (n_odd, LRU_WIDTH), 0.01),
        "lru_wx": nrm(ks[18], (n_odd, LRU_BLOCKS, gb, gb), gb ** -0.5),
        "lru_bx": nrm(ks[19], (n_odd, LRU_WIDTH), 0.01),
        "lru_lambda": lru_lambda,
        "router_w": nrm(ks[21], (D_MODEL, N_EXPERTS), D_MODEL ** -0.5),
        "router_b": nrm(ks[22], (N_EXPERTS,), 0.01),
        "moe_w_gate": nrm(ks[23], (DEPTH, N_EXPERTS, D_MODEL, D_FF), D_MODEL ** -0.5),
        "moe_w_up": nrm(ks[24], (DEPTH, N_EXPERTS, D_MODEL, D_FF), D_MODEL ** -0.5),
        "moe_w_down": nrm(ks[25], (DEPTH, N_EXPERTS, D_FF, D_MODEL), DEEPNORM_BETA * D_FF ** -0.5),
    }


def reference(x, c, ada_w, ada_b, ln_g, ln_b, even_w_in, even_w_out, diff_lambda, diff_gain,
              hgrn_gamma, hgrn_gain, odd_w_in, odd_w_out, conv_w, conv_b, lru_wa, lru_ba, lru_wx,
              lru_bx, lru_lambda, router_w, router_b, moe_w_gate, moe_w_up, moe_w_down):
    lower_bounds = jnp.cumsum(jax.nn.softmax(hgrn_gamma.astype(jnp.float32), axis=0), axis=0)
    cond = jax.nn.silu(c)
    for l in range(DEPTH):
        mod = (cond @ ada_w[l] + ada_b[l])[:, None, :]
        sh1, sc1, g1, sh2, sc2, g2 = jnp.split(mod, 6, axis=-1)
        h = x * (1.0 + sc1) + sh1
        if l % 2 == 0:
            j = l // 2
            lam_init = 0.8 - 0.6 * math.exp(-0.3 * l)
            y = even_mixer(h, even_w_in[j], even_w_out[j], diff_lambda[j], diff_gain[j],
                           hgrn_gain[j], lower_bounds[l], lam_init)
        else:
            j = l // 2
            y = odd_mixer(h, odd_w_in[j], odd_w_out[j], conv_w[j], conv_b[j], lru_wa[j], lru_ba[j],
                          lru_wx[j], lru_bx[j], lru_lambda[j])
        x = layer_norm(DEEPNORM_ALPHA * x + (1.0 + g1) * y, ln_g[l, 0], ln_b[l, 0])
        h = x * (1.0 + sc2) + sh2
        y = grouped_moe(h, router_w, router_b, moe_w_gate[l], moe_w_up[l], moe_w_down[l])
        x = layer_norm(DEEPNORM_ALPHA * x + (1.0 + g2) * y, ln_g[l, 1], ln_b[l, 1])
    return x
```

```python
import contextlib
import numpy as np
import concourse.bass as bass
import concourse.mybir as mybir
from concourse.bass_utils import run_bass_kernel_spmd

F32 = mybir.dt.float32
BF16 = mybir.dt.bfloat16
AF = mybir.ActivationFunctionType
ALU = mybir.AluOpType
AX = mybir.AxisListType

ENGS = ['tensor', 'vector', 'scalar', 'gpsimd', 'sync']
SEM_LIMIT = 30000
D = 1024
ALPHA = 4.0 ** 0.25
N_CORES = 8


class Res:
    __slots__ = ('name', 'last_w', 'readers', 'ds')

    def __init__(self, name):
        self.name = name
        self.last_w = None
        self.readers = []
        self.ds = {}


class DSem:
    __slots__ = ('name', 'dcount', 'dsem', 'kind')

    def __init__(self, name, kind):
        self.name = name
        self.dcount = 0
        self.dsem = None
        self.kind = kind


class Prog:
    def __init__(self, nc):
        self.nc = nc
        self.ops = {e: [] for e in ENGS}
        self.seen = {x: {e: -1 for e in ENGS} for x in ENGS}
        self.seen_d = {x: {} for x in ENGS}
        self.dma_res = []
        self.free_ds = {'sw': [], 'hw': []}
        self.all_dma_toks = []
        self.stack = contextlib.ExitStack()
        self.pending = {x: [] for x in ENGS}
        self.scopes = []

    def sbuf(self, name, shape, dtype):
        st = self.scopes[-1] if self.scopes else self.stack
        return st.enter_context(self.nc.sbuf_tensor(name, list(shape), dtype))

    def push(self):
        self.scopes.append(contextlib.ExitStack())

    def pop(self):
        self.scopes.pop().close()

    def psum(self, name, shape, dtype):
        return self.stack.enter_context(self.nc.psum_tensor(name, list(shape), dtype))

    def _need(self, eng, tok, waits):
        if tok is None:
            return
        if tok[0] == 'e':
            _, e, idx = tok
            if e == eng and eng == 'tensor':
                return
            if self.seen[eng][e] >= idx:
                return
            self.seen[eng][e] = idx
            waits.append(tok)
        else:
            _, r, val = tok
            if self.seen_d[eng].get(id(r), 0) >= val:
                return
            self.seen_d[eng][id(r)] = val
            waits.append(tok)

    def op(self, eng, fn, reads=(), writes=(), dma_res=None):
        waits = []
        for r in reads:
            self._need(eng, r.last_w, waits)
        for r in writes:
            self._need(eng, r.last_w, waits)
            for t in r.readers:
                self._need(eng, t, waits)
        for t in self.pending[eng]:
            self._need(eng, t, waits)
        self.pending[eng] = []
        idx = len(self.ops[eng])
        if dma_res is not None:
            kind = 'sw' if eng == 'gpsimd' else 'hw'
            ds = dma_res.ds.get(kind)
            if ds is None:
                fl = self.free_ds[kind]
                if fl:
                    ds = fl.pop()
                else:
                    ds = DSem("%s%d" % (kind, len(self.dma_res)), kind)
                    self.dma_res.append(ds)
                dma_res.ds[kind] = ds
            dma_res = ds
            ds.dcount += 16
            tok = ('d', ds, ds.dcount)
            self.all_dma_toks.append(tok)
        else:
            tok = ('e', eng, idx)
        import sys
        fr = sys._getframe(2)
        site = "%s:%d<%s:%d" % (fr.f_code.co_name, fr.f_lineno, fr.f_back.f_code.co_name, fr.f_back.f_lineno)
        self.ops[eng].append(dict(fn=fn, waits=waits, dma_res=dma_res, site=site,
                                  dval=(dma_res.dcount if dma_res else 0), mile=False))
        for r in writes:
            r.last_w = tok
            r.readers = []
        for r in reads:
            if r not in writes:
                r.readers.append(tok)
        return tok

    def barrier(self):
        toks = []
        for e in ENGS:
            for idx in range(len(self.ops[e]) - 1, -1, -1):
                if self.ops[e][idx]['dma_res'] is None:
                    toks.append(('e', e, idx))
                    break
        dtoks = {}
        for t in self.all_dma_toks:
            dtoks[id(t[1])] = t
        self.all_dma_toks = []
        for x in ENGS:
            self.pending[x].extend(list(toks) + list(dtoks.values()))

    def emit(self):
        nc = self.nc
        self.tail = {}
        for x in ENGS:
            w = []
            for t in self.pending[x]:
                self._need(x, t, w)
            self.tail[x] = w
            for t in w:
                if t[0] == 'e':
                    self.ops[t[1]][t[2]]['mile'] = True
        for x in ENGS:
            for o in self.ops[x]:
                for t in o['waits']:
                    if t[0] == 'e':
                        self.ops[t[1]][t[2]]['mile'] = True
        nsem = 0
        for x in ENGS:
            cnt = 0
            cur = None
            for o in self.ops[x]:
                if o['mile'] and o['dma_res'] is None:
                    if cur is None or cnt >= SEM_LIMIT:
                        cur = self.stack.enter_context(nc.semaphore(f"m_{x}_{nsem}"))
                        nsem += 1
                        cnt = 0
                    cnt += 1
                    o['msem'] = (cur, cnt)
        per16 = (SEM_LIMIT // 16) * 16
        for r in self.dma_res:
            n = r.dcount
            r.dsem = []
            for i in range((n + per16 - 1) // per16):
                r.dsem.append(self.stack.enter_context(nc.semaphore(f"d_{r.name}_{i}")))
                nsem += 1
        self.nsem = nsem

        def dsem_of(r, val):
            k = (val - 16) // per16
            return r.dsem[k], val - k * per16

        ops = self.ops
        self.imap = {}
        ctr = [0]

        def body(x):
            def f(eng):
                for o in ops[x]:
                    self.imap[ctr[0] + len(o['waits'])] = (x, o['site'])
                    ctr[0] += len(o['waits']) + 1
                    for t in o['waits']:
                        if t[0] == 'e':
                            s, v = ops[t[1]][t[2]]['msem']
                        else:
                            s, v = dsem_of(t[1], t[2])
                        eng.wait_ge(s, v)
                    inst = o['fn'](eng)
                    if o['dma_res'] is not None:
                        s, v = dsem_of(o['dma_res'], o['dval'])
                        inst.then_inc(s, 16)
                    elif o['mile']:
                        s, v = o['msem']
                        inst.then_inc(s, 1)
                for t in self.tail[x]:
                    if t[0] == 'e':
                        s, v = ops[t[1]][t[2]]['msem']
                    else:
                        s, v = dsem_of(t[1], t[2])
                    eng.wait_ge(s, v)
            return f

        with nc.Block() as block:
            block.tensor(body('tensor'))
            block.vector(body('vector'))
            block.scalar(body('scalar'))
            block.gpsimd(body('gpsimd'))
            block.sync(body('sync'))
        self.stack.close()


class V:
    __slots__ = ('ap', 'r')

    def __init__(self, ap, r):
        self.ap = ap
        self.r = r

    def __getitem__(self, k):
        return V(self.ap[k], self.r)

    def re(self, s, **kw):
        return V(self.ap.rearrange(s, **kw), self.r)

    def pb(self, n=128):
        return V(self.ap.partition_broadcast(n), self.r)


def _rs(xs):
    out = []
    for x in xs:
        if isinstance(x, V) and x.r not in out:
            out.append(x.r)
    return out


def _a(x):
    return x.ap if isinstance(x, V) else x


class Bld:
    ARENA_WORDS = 52992

    def __init__(self, nc):
        self.nc = nc
        self.P = Prog(nc)
        self.n = 0
        self.arena = self.P.stack.enter_context(nc.sbuf_tensor("arena", [128, self.ARENA_WORDS], F32))
        self.off = 0
        self.marks = []
        self.peak = 0
        self.scope_res = [[]]

    def push(self):
        self.marks.append(self.off)
        self.scope_res.append([])

    def pop(self):
        self.off = self.marks.pop()
        self.P.barrier()
        for r in self.scope_res.pop():
            for kind, ds in r.ds.items():
                self.P.free_ds[kind].append(ds)
            r.ds = {}

    def sb(self, shape, dt, name=None):
        self.n += 1
        name = f"{name or 't'}_{self.n}"
        nel = int(np.prod(shape[1:]))
        nbytes = nel * (2 if dt == BF16 else 4)
        words = (nbytes + 63) // 64 * 16
        assert self.off + words <= self.ARENA_WORDS, f"SBUF arena overflow at {name}: {self.off * 4} + {words * 4}"
        ap = self.arena[0:shape[0], self.off:self.off + (nbytes + 3) // 4]
        self.off += words
        self.peak = max(self.peak, self.off * 4)
        if dt == BF16:
            ap = ap.bitcast(BF16)
            if nel % 2:
                ap = ap[:, 0:nel]
        if len(shape) == 3:
            ap = ap.rearrange("p (a b) -> p a b", a=shape[1])
        elif len(shape) == 4:
            ap = ap.rearrange("p (a b c) -> p a b c", a=shape[1], b=shape[2])
        r = Res(name)
        self.scope_res[-1].append(r)
        return V(ap, r)

    def pool(self, shape, dt, n, name=None):
        tiles = [self.sb(shape, dt, name) for _ in range(n)]
        st = [0]

        def nxt():
            st[0] += 1
            return tiles[(st[0] - 1) % n]
        return nxt

    def act(self, out, in_, func, bias=None, scale=None):
        kw = {}
        if bias is not None:
            kw['bias'] = _a(bias)
        if scale is not None:
            kw['scale'] = _a(scale)
        o, i = out.ap, in_.ap
        self.P.op('scalar', lambda e: e.activation(out=o, in_=i, func=func, **kw),
                  reads=_rs([in_, bias, scale]), writes=[out.r])

    def tt(self, eng, out, a, b, op):
        o, x, y = out.ap, a.ap, b.ap
        self.P.op(eng, lambda e: e.tensor_tensor(out=o, in0=x, in1=y, op=op), reads=_rs([a, b]), writes=[out.r])

    def ts(self, eng, out, a, s1, op0, s2=None, op1=None):
        o, x, p, q = out.ap, a.ap, _a(s1), _a(s2)
        if s2 is None:
            fn = lambda e: e.tensor_scalar(out=o, in0=x, scalar1=p, scalar2=None, op0=op0)
        else:
            fn = lambda e: e.tensor_scalar(out=o, in0=x, scalar1=p, scalar2=q, op0=op0, op1=op1)
        self.P.op(eng, fn, reads=_rs([a, s1, s2]), writes=[out.r])

    def stt(self, eng, out, a, s, b, op0, op1):
        o, x, p, y = out.ap, a.ap, _a(s), b.ap
        eng = 'vector'
        self.P.op(eng, lambda e: e.scalar_tensor_tensor(out=o, in0=x, scalar=p, in1=y, op0=op0, op1=op1),
                  reads=_rs([a, s, b]), writes=[out.r])

    def rsqrt(self, out, a, eps, tmp, scale=1.0):
        self.act(tmp, a, AF.Ln, bias=eps, scale=scale)
        self.act(out, tmp, AF.Exp, scale=-0.5)

    def cp(self, eng, out, a):
        o, x = out.ap, a.ap
        if eng == 'scalar':
            self.P.op(eng, lambda e: e.copy(out=o, in_=x), reads=[a.r], writes=[out.r])
        else:
            self.P.op(eng, lambda e: e.tensor_copy(out=o, in_=x), reads=[a.r], writes=[out.r])

    def red(self, out, a, op):
        o, x = out.ap, a.ap
        self.P.op('vector', lambda e: e.tensor_reduce(out=o, in_=x, axis=AX.X, op=op), reads=[a.r], writes=[out.r])

    def recip(self, out, a):
        o, x = out.ap, a.ap
        self.P.op('vector', lambda e: e.reciprocal(out=o, in_=x), reads=[a.r], writes=[out.r])

    def scan(self, out, d0, d1, init):
        o, x, y, z = out.ap, d0.ap, d1.ap, _a(init)
        self.P.op('vector', lambda e: e.tensor_tensor_scan(out=o, data0=x, data1=y, initial=z, op0=ALU.mult,
                                                          op1=ALU.add), reads=_rs([d0, d1, init]), writes=[out.r])

    def memset(self, eng, out, val):
        o = out.ap
        self.P.op(eng, lambda e: e.memset(o, val), writes=[out.r])

    def mm(self, out, lhsT, rhs, start=True, stop=True, xr=()):
        o, l, r = out.ap, lhsT.ap, rhs.ap
        self.P.op('tensor', lambda e: e.matmul(o, l, r, start=start, stop=stop),
                  reads=_rs([lhsT, rhs] + list(xr)), writes=[out.r])

    def tr(self, out, in_, ident):
        o, i, d = out.ap, in_.ap, ident.ap
        self.P.op('tensor', lambda e: e.transpose(o, i, d), reads=_rs([in_, ident]), writes=[out.r])

    def dma(self, q, out, in_, slow=False):
        o, i = out.ap, in_.ap
        kw = dict(allow_slow_non_contiguous=True) if slow else {}
        self.P.op(q, lambda e: e.dma_start(out=o, in_=i, **kw), reads=[in_.r], writes=[out.r], dma_res=out.r)


def host_consts(T):
    p = np.arange(128)
    sq = np.zeros((5, 128, 128), np.float32)
    sq[0] = np.eye(128)
    sq[1] = (p[:, None] > p[None, :])
    same = (p[:, None] // 64) == (p[None, :] // 64)
    mid = (p // 64) * 64 + 31
    s_, c_ = p[:, None], p[None, :]
    sq[2] = (same & (s_ > mid[None, :]) & (s_ <= c_)) * 1.0 - (same & (s_ > c_) & (s_ <= mid[None, :])) * 1.0
    sq[3] = same & (s_ <= c_)
    sq[4] = same & (s_ > c_)
    csq = np.ascontiguousarray(sq.transpose(1, 0, 2).reshape(128, 640))
    i = np.arange(512)
    mk = np.zeros((8, 128, 512), np.float32)
    for r in range(4):
        mk[r] = (128 * r + p[:, None]) <= i[None, :]
        mk[4 + r] = (128 * r + p[:, None]) < i[None, :]
    t = np.arange(T)
    aug = np.zeros((4, 2, 4, T), np.float32)
    for h in range(4):
        sl = 2.0 ** (-8.0 * (h + 1) / 4)
        aug[h, 0, 0] = -8 * sl * 64 * (t // 64)
        aug[h, 0, 1] = -8 * sl * (t % 64)
        aug[h, 0, 2] = 1
        aug[h, 0, 3] = 1
        aug[h, 1, 0] = 1
        aug[h, 1, 1] = 1
        aug[h, 1, 2] = 8 * sl * 64 * (t // 64)
        aug[h, 1, 3] = 8 * sl * (t % 64)
    return dict(c_sq=csq, c_mask=mk, c_aug=aug)


IN_SHAPES = dict(
    ada_w=[2, 1024, 6144], ada_b=[2, 6144], ln_g=[2, 2, 1024], ln_b=[2, 2, 1024],
    even_w_in=[1, 1024, 3584], even_w_out=[1, 1024, 1024], diff_lambda=[1, 4, 64], diff_gain=[1, 128],
    hgrn_gamma=[3, 512], hgrn_gain=[1, 128], odd_w_in=[1, 1024, 2560], odd_w_out=[1, 1024, 1024],
    conv_w=[1, 4, 512], conv_b=[1, 512], lru_wa=[4, 128, 128], lru_ba=[1, 512], lru_wx=[4, 128, 128],
    lru_bx=[1, 512], lru_lambda=[1, 512], router_w=[1024, 16], router_b=[1, 16],
    moe_w_gate=[2, 16, 1024, 512], moe_w_up=[2, 16, 1024, 512], moe_w_down=[2, 16, 512, 1024],
)


class _Stop(Exception):
    pass


STOP = None


def build(T, plan):
    nc = bass.Bass("TRN2", target_bir_lowering=False)
    b = Bld(nc)
    NT, NCH = T // 128, T // 512

    def din(name, shape, dt=F32):
        return V(nc.dram_tensor(name, list(shape), dt, kind="ExternalInput").ap(), Res(name))

    def dscr(name, shape, dt):
        return V(nc.dram_tensor(name, list(shape), dt).ap(), Res(name))

    I = {k: din(k, s) for k, s in IN_SHAPES.items()}
    x_in = din("x", [T, D])
    c_in = din("c", [1, D])
    c_sq = din("c_sq", [128, 640])
    c_mask = din("c_mask", [8, 128, 512])
    c_aug = din("c_aug", [4, 2, 4, T])
    out = V(nc.dram_tensor("out", [T, D], F32, kind="ExternalOutput").ap(), Res("out"))
    xs = [dscr("xs0", [T, D], F32), dscr("xs1", [T, D], F32)]
    modscr = dscr("modscr", [2, 6144], F32)

    PS = []
    for i in range(4):
        t = b.P.psum(f"ps{i}", [128, 1024], F32)
        PS.append([V(t[:, 0:512], Res(f"ps{i}a")), V(t[:, 512:1024], Res(f"ps{i}b")), t])

    def bank(i):
        return PS[i // 2][i % 2]

    csq = b.sb([128, 640], F32, "csq")
    b.dma('sync', csq, c_sq)
    ident = csq[:, 0:128]
    onesf = b.sb([128, 128], F32, "onesf")
    b.memset('vector', onesf, 1.0)
    onesb = b.sb([128, 128], BF16, "onesb")
    b.memset('vector', onesb, 1.0)

    b.push()
    cT = b.sb([128, 8], F32, "cT")
    b.dma('sync', cT, c_in.re("o (k p) -> p (o k)", p=128), slow=True)
    cTs = b.sb([128, 8], F32, "cTs")
    b.act(cTs, cT, AF.Silu)
    cTb = b.sb([128, 8], BF16, "cTb")
    b.cp('vector', cTb, cTs)
    wsec_pool = b.pool([128, 8, 1024], BF16, 2, "wsec")
    layers_needed = sorted({int(p[-1]) for p in plan})
    for l in layers_needed:
        modrow = b.sb([1, 6144], F32, "modrow")
        adab = b.sb([1, 6144], F32, "adab")
        b.dma('sync', adab, I['ada_b'][l:l + 1, :])
        for sec in range(6):
            ws = wsec_pool()
            b.dma('gpsimd', ws, I['ada_w'][l][:, sec * 1024:(sec + 1) * 1024].re("(k p) n -> p k n", p=128))
            for nh in range(2):
                ps = bank(nh)
                for kc in range(8):
                    b.mm(ps[0:1, :], cTb[:, kc:kc + 1], ws[:, kc, nh * 512:(nh + 1) * 512], kc == 0, kc == 7)
                c0 = sec * 1024 + nh * 512
                b.tt('vector', modrow[0:1, c0:c0 + 512], ps[0:1, :], adab[0:1, c0:c0 + 512], ALU.add)
            if sec in (1, 2, 4, 5):
                b.ts('vector', modrow[0:1, sec * 1024:(sec + 1) * 1024], modrow[0:1, sec * 1024:(sec + 1) * 1024],
                     1.0, ALU.add)
        b.dma('sync', modscr[l:l + 1, :], modrow)
    b.pop()

    xt_pool = b.pool([128, D], F32, 2, "xt")
    hf_pool = b.pool([128, 8, 128], F32, 2, "hf")
    scp = b.sb([128, 8], F32, "scp")
    shv = b.sb([128, 8], F32, "shv")
    gB = b.sb([128, D], F32, "gB")
    lnG = b.sb([128, D], F32, "lnG")
    lnB = b.sb([128, D], F32, "lnB")
    ework = [b.pool([128, D], F32, 2, "ew%d" % i) for i in range(2)]
    small = b.pool([128, 8], F32, 4, "small")

    def load_mod(l, s):
        sec_sh, sec_sc, sec_g = (0, 1, 2) if s == 0 else (3, 4, 5)
        b.dma('sync', shv, modscr[l, sec_sh * 1024:(sec_sh + 1) * 1024].re("(k p) -> p k", p=128), slow=True)
        b.dma('sync', scp, modscr[l, sec_sc * 1024:(sec_sc + 1) * 1024].re("(k p) -> p k", p=128), slow=True)
        b.dma('sync', gB, modscr[l, sec_g * 1024:(sec_g + 1) * 1024].pb())
        b.dma('sync', lnG, I['ln_g'][l, s, :].pb())
        b.dma('sync', lnB, I['ln_b'][l, s, :].pb())

    def prologue(xsrc, tok0, ntiles, hT, col0, router=None):
        for i in range(ntiles):
            t0 = tok0 + i * 128
            xt = xt_pool()
            b.dma('sync', xt, xsrc[t0:t0 + 128, :])
            pa, pb_ = PS[3][0], PS[3][1]
            for kc in range(8):
                pk = (pa if kc < 4 else pb_)[:, (kc % 4) * 128:(kc % 4 + 1) * 128]
                b.tr(pk, xt[:, kc * 128:(kc + 1) * 128], ident)
            hf = hf_pool()
            for kc in range(8):
                pk = (pa if kc < 4 else pb_)[:, (kc % 4) * 128:(kc % 4 + 1) * 128]
                b.act(hf[:, kc, :], pk, AF.Identity, bias=shv[:, kc:kc + 1], scale=scp[:, kc:kc + 1])
            b.cp('vector', hT[:, :, col0 + i * 128:col0 + (i + 1) * 128], hf)
            if router is not None:
                router(hf, col0 + i * 128)

    def epilogue(xsrc, xdst, t0, y):
        xe = xt_pool()
        b.dma('sync', xe, xsrc[t0:t0 + 128, :])
        t1 = ework[0]()
        if isinstance(y, tuple):
            b.tt('vector', t1[:, 0:512], y[0], gB[:, 0:512], ALU.mult)
            b.tt('vector', t1[:, 512:1024], y[1], gB[:, 512:1024], ALU.mult)
        else:
            b.tt('vector', t1, y, gB, ALU.mult)
        r = ework[1]()
        b.stt('gpsimd', r, xe, ALPHA, t1, ALU.mult, ALU.add)
        b.tt('gpsimd', t1, r, r, ALU.mult)
        st = small()
        b.red(st[:, 0:1], r, ALU.add)
        b.red(st[:, 1:2], t1, ALU.add)
        b.ts('vector', st[:, 2:4], st[:, 0:2], 1.0 / D, ALU.mult)
        b.tt('vector', st[:, 4:5], st[:, 2:3], st[:, 2:3], ALU.mult)
        b.tt('vector', st[:, 5:6], st[:, 3:4], st[:, 4:5], ALU.subtract)
        b.rsqrt(st[:, 6:7], st[:, 5:6], 1e-5, st[:, 7:8])
        b.ts('vector', t1, r, st[:, 2:3], ALU.subtract, st[:, 6:7], ALU.mult)
        b.tt('gpsimd', r, t1, lnG, ALU.mult)
        xo = xe
        b.tt('vector', xo, r, lnB, ALU.add)
        b.dma('sync', xdst[t0:t0 + 128, :], xo)

    def moe_sublayer(l, xsrc, xdst):
        b.push()
        load_mod(l, 1)
        TH = min(T, 1024)
        BIG = b.sb([128, 8, TH], BF16, "hTm")
        rw = b.sb([128, 8, 16], F32, "rw")
        b.dma('sync', rw, I['router_w'].re("(k p) e -> p k e", p=128))
        rb = b.sb([1, 16], F32, "rb")
        b.dma('sync', rb, I['router_b'])
        gatesT = b.sb([16, TH], F32, "gatesT")
        sel = b.sb([16, 16, 128], F32, "sel")
        b.memset('vector', sel, 0.0)
        for e in range(16):
            b.dma('sync', sel[e:e + 1, e, :], onesf[e:e + 1, :])
        yacc = b.sb([128, TH // 128, D], F32, "yacc")
        wg_pool = b.pool([128, 8, 512], BF16, 3, "wg")
        wu_pool = b.pool([128, 8, 512], BF16, 3, "wu")
        wd_pool = b.pool([128, 4, D], BF16, 3, "wd")
        s_pool = b.pool([128, 512], BF16, 2, "s")
        t_pool = b.pool([128, 512], BF16, 2, "tg")
        gbs_pool = b.pool([128, 512], F32, 2, "gbs")
        act_pool = b.pool([128, 4, 512], BF16, 3, "actT")
        rt = b.pool([128, 16], F32, 6, "rt")
        r4 = b.pool([128, 4], F32, 8, "r4")
        r1 = b.pool([128, 1], F32, 8, "r1")

        def router(hf, col):
            lg = bank(5)[:, 0:16]
            for kc in range(8):
                b.mm(lg, hf[:, kc, :], rw[:, kc, :], kc == 0, False)
            b.mm(lg, onesf[0:1, :], rb[0:1, :], False, True)
            mx = r1()
            b.red(mx, lg, ALU.max)
            nmx = r1()
            b.ts('vector', nmx, mx, -1.0, ALU.mult)
            ex = rt()
            b.act(ex, lg, AF.Exp, bias=nmx, scale=1.0)
            sm = r1()
            b.red(sm, ex, ALU.add)
            rs = r1()
            b.recip(rs, sm)
            p = rt()
            b.ts('vector', p, ex, rs, ALU.mult)
            pv = p.re("p (g j) -> p g j", j=4)
            prs = []
            for (i0, i1) in ((0, 1), (0, 2), (0, 3), (1, 2), (1, 3), (2, 3)):
                q = r4()
                b.tt('vector', q, pv[:, :, i0], pv[:, :, i1], ALU.add)
                prs.append(q)
            gs = prs[0]
            for q in prs[1:]:
                g2 = r4()
                b.tt('vector', g2, gs, q, ALU.max)
                gs = g2
            gm = r1()
            b.red(gm, gs, ALU.max)
            ing = r4()
            b.ts('vector', ing, gs, gm, ALU.is_equal)
            im1 = r4()
            b.ts('vector', im1, ing, -1.0, ALU.add)
            msk = rt()
            for g in range(4):
                b.ts('vector', msk[:, g * 4:(g + 1) * 4], p[:, g * 4:(g + 1) * 4], ing[:, g:g + 1], ALU.mult,
                     im1[:, g:g + 1], ALU.add)
            tp1 = r1()
            b.red(tp1, msk, ALU.max)
            is1 = rt()
            b.ts('vector', is1, msk, tp1, ALU.is_equal)
            m2 = rt()
            b.stt('vector', m2, is1, -2.0, msk, ALU.mult, ALU.add)
            tp2 = r1()
            b.red(tp2, m2, ALU.max)
            is2 = rt()
            b.ts('vector', is2, m2, tp2, ALU.is_equal)
            sl = rt()
            b.tt('vector', sl, is1, is2, ALU.add)
            den = r1()
            b.tt('vector', den, tp1, tp2, ALU.add)
            rden = r1()
            b.recip(rden, den)
            gt = rt()
            b.stt('vector', gt, p, rden, sl, ALU.mult, ALU.mult)
            gp = bank(5)[0:16, 128:256]
            b.tr(gp, gt, ident)
            b.cp('vector', gatesT[:, col:col + 128], gp)

        def load_w(e):
            wg, wu, wd = wg_pool(), wu_pool(), wd_pool()
            b.dma('gpsimd', wg, I['moe_w_gate'][l, e].re("(k p) f -> p k f", p=128))
            b.dma('gpsimd', wu, I['moe_w_up'][l, e].re("(k p) f -> p k f", p=128))
            b.dma('gpsimd', wd, I['moe_w_down'][l, e].re("(k p) n -> p k n", p=128))
            return wg, wu, wd

        ycnt = [0]
        for half in range(T // TH):
            tok0 = half * TH
            wnext = load_w(0)
            prologue(xsrc, tok0, TH // 128, BIG, 0, router=router)
            nch = TH // 512
            items = [(e, ch) for e in range(16) for ch in range(nch)]
            wcur = {}

            def stage1(e, ch, wts):
                wg, wu, wd = wts
                c0 = ch * 512
                gbp = bank(4)
                b.mm(gbp, sel[:, e, :], gatesT[:, c0:c0 + 512], True, True)
                gbs = gbs_pool()
                b.cp('scalar', gbs, gbp)
                actT = act_pool()
                for fc in range(4):
                    pg, pu = bank(2 * (fc % 2)), bank(2 * (fc % 2) + 1)
                    for kc in range(8):
                        b.mm(pg, wg[:, kc, fc * 128:(fc + 1) * 128], BIG[:, kc, c0:c0 + 512], kc == 0, kc == 7)
                    for kc in range(8):
                        b.mm(pu, wu[:, kc, fc * 128:(fc + 1) * 128], BIG[:, kc, c0:c0 + 512], kc == 0, kc == 7)
                    s_ = s_pool()
                    b.act(s_, pg, AF.Silu)
                    tg = t_pool()
                    b.tt('vector', tg, pu, gbs, ALU.mult)
                    b.tt('gpsimd', actT[:, fc, :], s_, tg, ALU.mult)
                return actT

            def stage2(e, ch, wts, actT):
                wd = wts[2]
                for ti in range(4):
                    ycnt[0] += 1
                    ya, yb = (PS[3][0], PS[3][1]) if ycnt[0] % 2 else (bank(4), bank(5))
                    for nh, yp in ((0, ya), (1, yb)):
                        for fc in range(4):
                            b.mm(yp, actT[:, fc, ti * 128:(ti + 1) * 128], wd[:, fc, nh * 512:(nh + 1) * 512],
                                 fc == 0, fc == 3)
                    tix = ch * 4 + ti
                    for nh, yp in ((0, ya), (1, yb)):
                        ys = yacc[:, tix, nh * 512:(nh + 1) * 512]
                        if e == 0:
                            b.cp('vector', ys, yp)
                        else:
                            b.tt('vector', ys, ys, yp, ALU.add)

            prev = None
            for (e, ch) in items:
                if ch == 0:
                    wcur[e] = wnext
                    if e + 1 < 16:
                        wnext = load_w(e + 1)
                a = stage1(e, ch, wcur[e])
                if prev is not None:
                    stage2(*prev)
                prev = (e, ch, wcur[e], a)
            stage2(*prev)
            for i in range(TH // 128):
                epilogue(xsrc, xdst, tok0 + i * 128, yacc[:, i, :])
        b.pop()

    def load_wsec(pool, win, sec):
        w = pool()
        b.dma('gpsimd', w, win[:, sec * 512:(sec + 1) * 512].re("(k p) n -> p k n", p=128))
        return w

    def proj_heads64(BIG, w, dst, stage_pool):
        for g in range(8):
            stg = stage_pool()
            for ch in range(NCH):
                pq = bank(ch % 2)
                for kc in range(8):
                    b.mm(pq[0:64, :], w[:, kc, g * 64:(g + 1) * 64], BIG[:, kc, ch * 512:(ch + 1) * 512], kc == 0, kc == 7)
                b.cp('scalar' if ch % 2 else 'vector', stg[0:64, ch * 512:(ch + 1) * 512], pq[0:64, :])
            b.dma('sync', dst[g], stg[0:64, :])

    def proj_tok(BIG, w, dst, st_pool):
        for ti in range(NT):
            pv = bank(ti % 2)
            for kc in range(8):
                b.mm(pv, BIG[:, kc, ti * 128:(ti + 1) * 128], w[:, kc, :], kc == 0, kc == 7)
            st = st_pool()
            b.cp('scalar' if ti % 2 else 'vector', st, pv)
            b.dma('sync', dst[ti * 128:(ti + 1) * 128, :], st)

    def outproj_epilogue(BIG, wout, xsrc, xdst):
        wo = b.sb([128, 8, D], BF16, "wo")
        b.dma('gpsimd', wo, wout.re("(k p) n -> p k n", p=128))
        for ti in range(NT):
            ya, yb = PS[2 + ti % 2][0], PS[2 + ti % 2][1]
            for nh, yp in ((0, ya), (1, yb)):
                for kc in range(8):
                    b.mm(yp, BIG[:, kc, ti * 128:(ti + 1) * 128], wo[:, kc, nh * 512:(nh + 1) * 512], kc == 0, kc == 7)
            epilogue(xsrc, xdst, ti * 128, (ya, yb))

    def stop_at(tag):
        if STOP == tag:
            raise _Stop()

    def colvec(src_row, n):
        t = b.sb([128, n], F32, "colv")
        b.dma('sync', t, src_row.re("(c p) -> p c", p=128), slow=True)
        return t

    def odd_sublayer(l, xsrc, xdst):
        j = l // 2
        b.push()
        load_mod(l, 0)
        BIG = b.sb([128, 8, T], BF16, "BIGo")
        prologue(xsrc, 0, NT, BIG, 0)
        win = I['odd_w_in'][j]
        QS = dscr("QS", [8, 64, T], BF16)
        KS = dscr("KS", [8, 64, T], BF16)
        VS = dscr("VS", [T, 512], BF16)
        YR = dscr("YR", [512, T], BF16)
        b.push()
        wpool = b.pool([128, 8, 512], BF16, 2, "wsec")
        cw = b.sb([128, 4, 4], F32, "cw")
        for tap in range(4):
            b.dma('sync', cw[:, tap, :], I['conv_w'][j][tap].re("(c p) -> p c", p=128), slow=True)
        cbv = colvec(I['conv_b'][j], 4)
        bav = colvec(I['lru_ba'][j], 4)
        bxv = colvec(I['lru_bx'][j], 4)
        lamv = colvec(I['lru_lambda'][j], 4)
        e1 = b.sb([128, 4], F32, "e1")
        b.act(e1, lamv, AF.Exp, scale=-1.0)
        spl = b.sb([128, 4], F32, "spl")
        b.act(spl, e1, AF.Ln, bias=1.0)
        negc8 = b.sb([128, 4], F32, "negc8")
        b.ts('vector', negc8, spl, -8.0, ALU.mult)
        wa = b.sb([128, 4, 128], F32, "wa")
        b.dma('sync', wa, I['lru_wa'].re("c p q -> p c q"))
        wx = b.sb([128, 4, 128], F32, "wx")
        b.dma('sync', wx, I['lru_wx'].re("c p q -> p c q"))
        wxr = load_wsec(wpool, win, 0)
        wgr = load_wsec(wpool, win, 1)
        hlast = b.sb([128, 4], F32, "hlast")
        b.memset('vector', hlast, 0.0)
        xrp = [b.sb([128, 515], F32, "xrp") for _ in range(4)]
        for cc in range(4):
            b.memset('gpsimd', xrp[cc][:, 0:3], 0.0)
        f_pool = [b.pool([128, 512], F32, 2, "lf%d" % i) for i in range(6)]
        yr_pool = b.pool([128, 512], BF16, 2, "yr")
        for tc in range(NCH):
            tsl = slice(tc * 512, (tc + 1) * 512)
            for cc in range(4):
                px = bank(0)
                for kc in range(8):
                    b.mm(px, wxr[:, kc, cc * 128:(cc + 1) * 128], BIG[:, kc, tsl], kc == 0, kc == 7)
                xp = xrp[cc]
                b.cp('scalar', xp[:, 3:515], px)
                xc = f_pool[0]()
                b.ts('vector', xc, xp[:, 3:515], cw[:, 3, cc:cc + 1], ALU.mult, cbv[:, cc:cc + 1], ALU.add)
                for tap in (2, 1, 0):
                    b.stt('vector', xc, xp[:, tap:tap + 512], cw[:, tap, cc:cc + 1], xc, ALU.mult, ALU.add)
                hal = small()
                b.cp('gpsimd', hal[:, 0:3], xp[:, 512:515])
                b.cp('gpsimd', xp[:, 0:3], hal[:, 0:3])
                pr, pi = bank(1), bank(2)
                b.mm(pr, wa[:, cc, :], xc)
                b.mm(pi, wx[:, cc, :], xc)
                r = f_pool[1]()
                b.act(r, pr, AF.Sigmoid, bias=bav[:, cc:cc + 1])
                ig = f_pool[2]()
                b.act(ig, pi, AF.Sigmoid, bias=bxv[:, cc:cc + 1])
                a = f_pool[3]()
                b.act(a, r, AF.Exp, scale=negc8[:, cc:cc + 1])
                om = r
                b.tt('gpsimd', om, a, a, ALU.mult)
                b.ts('vector', om, om, -1.0, ALU.mult, 1.0, ALU.add)
                b.ts('vector', om, om, 1e-12, ALU.max)
                b.act(om, om, AF.Sqrt)
                b.tt('gpsimd', ig, ig, xc, ALU.mult)
                u = f_pool[4]()
                b.tt('vector', u, ig, om, ALU.mult)
                hh = f_pool[5]()
                b.scan(hh, a, u, 0.0 if tc == 0 else hlast[:, cc:cc + 1])
                b.cp('vector', hlast[:, cc:cc + 1], hh[:, 511:512])
                pg = bank(3)
                for kc in range(8):
                    b.mm(pg, wgr[:, kc, cc * 128:(cc + 1) * 128], BIG[:, kc, tsl], kc == 0, kc == 7)
                g = xc
                b.cp('scalar', g, pg)
                g2 = ig
                b.tt('gpsimd', g2, g, g, ALU.mult)
                b.ts('vector', g2, g2, 0.044715, ALU.mult, 1.0, ALU.add)
                b.tt('gpsimd', g2, g2, g, ALU.mult)
                b.act(g2, g2, AF.Sigmoid, scale=1.5957691216057308)
                b.tt('gpsimd', g2, g2, g, ALU.mult)
                yr = yr_pool()
                b.tt('vector', yr, g2, hh, ALU.mult)
                b.dma('sync', YR[cc * 128:(cc + 1) * 128, tsl], yr)
        stop_at('oA1')
        stage_pool = b.pool([64, T], BF16, 2, "stg")
        w = load_wsec(wpool, win, 2)
        proj_heads64(BIG, w, QS, stage_pool)
        w = load_wsec(wpool, win, 3)
        proj_heads64(BIG, w, KS, stage_pool)
        w = load_wsec(wpool, win, 4)
        proj_tok(BIG, w, VS, b.pool([128, 512], BF16, 2, "vst"))
        b.pop()
        stop_at('oA2')
        b.push()
        for cc in range(4):
            b.dma('sync', BIG[:, cc, :], YR[cc * 128:(cc + 1) * 128, :])
        triSUb = b.sb([128, 128], BF16, "triSUb")
        b.cp('vector', triSUb, csq[:, 128:256])
        mkS = b.sb([128, 4, 512], BF16, "mkS")
        b.dma('gpsimd', mkS, c_mask[4:8].re("n p q -> p n q"))
        q_pool = b.pool([64, T], BF16, 2, "qT")
        k_pool = b.pool([64, T], BF16, 2, "kT")
        v_pool = b.pool([128, NT, 64], BF16, 2, "vS")
        e_pool = b.pool([128, 512], F32, 2, "e")
        sp_pool = b.pool([128, 512], F32, 3, "sp")
        spb_pool = b.pool([128, 512], BF16, 2, "spb")
        arg_pool = b.pool([128, 512], F32, 3, "arg")
        p_pool = b.pool([128, 512], BF16, 3, "P")
        sfx_pool = b.pool([128, 512], F32, 2, "sfx")
        ost_pool = b.pool([64, 512], BF16, 2, "ost")
        VSv = VS.re("(n p) c -> p n c", p=128)
        blk = [0]
        for h in range(8):
            qT, kT, vv = q_pool(), k_pool(), v_pool()
            b.dma('sync', qT, QS[h])
            b.dma('sync', kT, KS[h])
            b.dma('sync', vv, VSv[:, :, h * 64:(h + 1) * 64])
            for Q in range(NCH):
                qs = slice(Q * 512, (Q + 1) * 512)
                sfx = sfx_pool()
                blk[0] += 1
                OT = bank(6 + blk[0] % 2)[0:64, :]
                Js = list(reversed(range(4 * Q + 4)))
                last = len(Js) - 1

                def s1(idx, J):
                    r = J - 4 * Q
                    zt = bank(idx % 2)
                    b.mm(zt, kT[:, J * 128:(J + 1) * 128], qT[:, qs])
                    e = e_pool()
                    b.act(e, zt, AF.Exp, scale=0.125)
                    sp = sp_pool()
                    b.act(sp, e, AF.Ln, bias=1.0)
                    spb = spb_pool()
                    if r >= 0:
                        b.tt('gpsimd', spb, sp, mkS[:, r, :], ALU.mult)
                    else:
                        b.cp('gpsimd', spb, sp)
                    aw = bank(2 + idx % 2)
                    b.mm(aw, triSUb, spb)
                    bt = None
                    if idx < last:
                        bt = bank(4 + idx % 2)
                        b.mm(bt, onesb, spb)
                    return (r, zt, sp, aw, bt)

                def s2(idx, J, ctx):
                    r, zt, sp, aw, bt = ctx
                    arg = arg_pool()
                    b.stt('vector', arg, zt, 0.125, sp, ALU.mult, ALU.subtract)
                    b.tt('vector', arg, arg, aw, ALU.subtract)
                    if idx > 0:
                        b.tt('gpsimd', arg, arg, sfx, ALU.subtract)
                    P = p_pool()
                    b.act(P, arg, AF.Exp)
                    if r >= 0:
                        b.tt('gpsimd', P, P, mkS[:, r, :], ALU.mult)
                    if idx < last:
                        if idx == 0:
                            b.cp('vector', sfx, bt)
                        else:
                            b.tt('vector', sfx, sfx, bt, ALU.add)
                    b.mm(OT, vv[:, J, :], P, idx == 0, idx == last)

                ctx = s1(0, Js[0])
                for idx, J in enumerate(Js):
                    nxt = s1(idx + 1, Js[idx + 1]) if idx < last else None
                    s2(idx, J, ctx)
                    ctx = nxt
                if h % 2 == 0:
                    b.cp('vector', BIG[0:64, 4 + h // 2, qs], OT)
                else:
                    ost = ost_pool()
                    b.cp('vector', ost, OT)
                    b.dma('sync', BIG[64:128, 4 + h // 2, qs], ost)
        b.pop()
        stop_at('oB')
        b.push()
        outproj_epilogue(BIG, I['odd_w_out'][j], xsrc, xdst)
        b.pop()
        b.pop()

    def even_sublayer(l, xsrc, xdst):
        import math
        j = l // 2
        lam_init = 0.8 - 0.6 * math.exp(-0.3 * l)
        b.push()
        load_mod(l, 0)
        BIG = b.sb([128, 8, T], BF16, "BIGe")
        prologue(xsrc, 0, NT, BIG, 0)
        win = I['even_w_in'][j]
        QA = dscr("QA", [8, 64, T], BF16)
        KA = dscr("KA", [8, 64, T], BF16)
        VA = dscr("VA", [T, 512], BF16)
        QST = dscr("QST", [512, T], F32)
        KTs = dscr("KTs", [512, T], F32)
        SGT = dscr("SGT", [512, T], BF16)
        LF = dscr("LF", [T, 512], F32)
        KTOK = dscr("KTOK", [T, 512], F32)
        VH = dscr("VH", [T, 512], BF16)
        lbscr = dscr("lbscr", [2, 512], F32)
        gm = b.sb([1, 3, 512], F32, "gm")
        b.dma('sync', gm, I['hgrn_gamma'].re("(o a) c -> o a c", o=1))
        eg = b.sb([1, 3, 512], F32, "eg")
        b.act(eg, gm, AF.Exp)
        den = b.sb([1, 512], F32, "den")
        b.tt('vector', den, eg[:, 0, :], eg[:, 1, :], ALU.add)
        b.tt('vector', den, den, eg[:, 2, :], ALU.add)
        num = b.sb([1, 512], F32, "num")
        b.cp('vector', num, eg[:, 0, :])
        for i in range(1, l + 1):
            b.tt('vector', num, num, eg[:, i, :], ALU.add)
        rden = b.sb([1, 512], F32, "rden")
        b.recip(rden, den)
        lbr = b.sb([1, 2, 512], F32, "lbr")
        b.tt('vector', lbr[:, 0, :], num, rden, ALU.mult)
        b.ts('vector', lbr[:, 1, :], lbr[:, 0, :], -1.0, ALU.mult, 1.0, ALU.add)
        b.dma('sync', lbscr.re("(o a) c -> o a c", o=1), lbr)
        omlF = colvec(lbscr[1], 4)
        lbB = b.sb([128, 512], F32, "lbB")
        b.dma('sync', lbB, lbscr[0].pb())
        omlB = b.sb([128, 512], F32, "omlB")
        b.dma('sync', omlB, lbscr[1].pb())
        dl = b.sb([128, 256], F32, "dl")
        b.dma('sync', dl, I['diff_lambda'][j].re("a d -> (a d)").pb())
        lw = b.sb([128, 128], F32, "lw")
        lsm = b.sb([128, 8], F32, "lsm")
        b.tt('vector', lw[:, 0:64], dl[:, 0:64], dl[:, 64:128], ALU.mult)
        b.tt('vector', lw[:, 64:128], dl[:, 128:192], dl[:, 192:256], ALU.mult)
        b.red(lsm[:, 0:1], lw[:, 0:64], ALU.add)
        b.red(lsm[:, 1:2], lw[:, 64:128], ALU.add)
        b.act(lsm[:, 2:4], lsm[:, 0:2], AF.Exp)
        b.tt('vector', lsm[:, 4:5], lsm[:, 3:4], lsm[:, 2:3], ALU.subtract)
        b.ts('vector', lsm[:, 5:6], lsm[:, 4:5], -lam_init, ALU.add)
        neglam = lsm[:, 5:6]
        gv = b.sb([128, 4], F32, "gv")
        b.dma('sync', gv[:, 0:1], I['diff_gain'][j].re("(p o) -> p o", o=1), slow=True)
        b.dma('sync', gv[:, 1:2], I['hgrn_gain'][j].re("(p o) -> p o", o=1), slow=True)
        b.ts('vector', gv[:, 2:3], gv[:, 0:1], math.sqrt(128.0) * (1.0 - lam_init), ALU.mult)
        b.ts('vector', gv[:, 3:4], gv[:, 1:2], math.sqrt(128.0), ALU.mult)
        dgS, hgS = gv[:, 2:3], gv[:, 3:4]
        stop_at('e0')
        b.push()
        wpool = b.pool([128, 8, 512], BF16, 2, "wsec")
        stage_pool = b.pool([64, T], BF16, 2, "stg")
        w = load_wsec(wpool, win, 0)
        proj_heads64(BIG, w, QA, stage_pool)
        w = load_wsec(wpool, win, 1)
        proj_heads64(BIG, w, KA, stage_pool)
        st_pool = b.pool([128, 512], BF16, 2, "vst")
        w = load_wsec(wpool, win, 2)
        proj_tok(BIG, w, VA, st_pool)
        w = load_wsec(wpool, win, 5)
        proj_tok(BIG, w, VH, st_pool)
        fst_pool = b.pool([128, 512], F32, 3, "fst")
        bst_pool = b.pool([128, 512], BF16, 2, "bst")
        w = load_wsec(wpool, win, 3)
        for h in range(4):
            for ch in range(NCH):
                pq = bank(ch % 2)
                for kc in range(8):
                    b.mm(pq, w[:, kc, h * 128:(h + 1) * 128], BIG[:, kc, ch * 512:(ch + 1) * 512], kc == 0, kc == 7)
                st = fst_pool()
                b.act(st, pq, AF.Silu)
                b.dma('sync', QST[h * 128:(h + 1) * 128, ch * 512:(ch + 1) * 512], st)
        w = load_wsec(wpool, win, 6)
        for h in range(4):
            for ch in range(NCH):
                pq = bank(ch % 2)
                for kc in range(8):
                    b.mm(pq, w[:, kc, h * 128:(h + 1) * 128], BIG[:, kc, ch * 512:(ch + 1) * 512], kc == 0, kc == 7)
                st = bst_pool()
                b.act(st, pq, AF.Silu)
                b.dma('sync', SGT[h * 128:(h + 1) * 128, ch * 512:(ch + 1) * 512], st)
        w = load_wsec(wpool, win, 4)
        for h in range(4):
            for ch in range(NCH):
                pq = bank(ch % 2)
                for kc in range(8):
                    b.mm(pq, w[:, kc, h * 128:(h + 1) * 128], BIG[:, kc, ch * 512:(ch + 1) * 512], kc == 0, kc == 7)
                sg = fst_pool()
                b.act(sg, pq, AF.Sigmoid, scale=-1.0)
                st = fst_pool()
                b.ts('vector', st, sg, omlF[:, h:h + 1], ALU.mult)
                b.dma('sync', KTs[h * 128:(h + 1) * 128, ch * 512:(ch + 1) * 512], st)
        for ti in range(NT):
            pv = bank(2 + ti % 2)
            for kc in range(8):
                b.mm(pv, BIG[:, kc, ti * 128:(ti + 1) * 128], w[:, kc, :], kc == 0, kc == 7)
            sg = fst_pool()
            b.act(sg, pv, AF.Sigmoid)
            tq = fst_pool()
            b.tt('vector', tq, sg, omlB, ALU.mult)
            b.tt('gpsimd', sg, tq, lbB, ALU.add)
            lf = fst_pool()
            b.act(lf, sg, AF.Ln)
            b.dma('sync', LF[ti * 128:(ti + 1) * 128, :], lf)
            b.tt('gpsimd', sg, omlB, tq, ALU.subtract)
            b.dma('sync', KTOK[ti * 128:(ti + 1) * 128, :], sg)
        b.pop()
        stop_at('eA')
        b.push()
        mkI = b.sb([128, 4, 512], BF16, "mkI")
        b.dma('gpsimd', mkI, c_mask[0:4].re("n p q -> p n q"))
        qT = b.sb([68, 2, T], BF16, "qTa")
        kT = b.sb([68, 2, T], BF16, "kTa")
        vv = b.sb([128, NT, 128], BF16, "vA")
        qTg = V(qT.ap, Res("qaug"))
        kTg = V(kT.ap, Res("kaug"))
        p_pool = b.pool([128, 512], BF16, 4, "Pa")
        rl_pool = b.pool([128, 512], F32, 4, "rl")
        oc_pool = b.pool([128, 512], F32, 4, "oc")
        VAv = VA.re("(n p) c -> p n c", p=128)
        blk = [0]
        for h in range(4):
            for c in range(2):
                b.dma('sync', qT[0:64, c, :], QA[2 * h + c])
                b.dma('sync', kT[0:64, c, :], KA[2 * h + c])
                b.dma('gpsimd', qTg[64:68, c, :], c_aug[h, 0])
                b.dma('gpsimd', kTg[64:68, c, :], c_aug[h, 1])
            b.dma('sync', vv, VAv[:, :, h * 128:(h + 1) * 128])
            for Q in range(NCH):
                qs = slice(Q * 512, (Q + 1) * 512)
                oc = []
                for c in range(2):
                    blk[0] += 1
                    OT, LT = bank(2 + blk[0] % 2), bank(4 + blk[0] % 2)
                    nJ = 4 * Q + 4

                    def s1(J):
                        r = J - 4 * Q
                        st = bank(J % 2)
                        b.mm(st, kT[0:68, c, J * 128:(J + 1) * 128], qT[0:68, c, qs], xr=(qTg, kTg))
                        P = p_pool()
                        if r >= 0:
                            cl = rl_pool()
                            b.ts('vector', cl, st, 0.125, ALU.mult, 80.0, ALU.min)
                            b.act(P, cl, AF.Exp)
                            b.tt('gpsimd', P, P, mkI[:, r, :], ALU.mult)
                        else:
                            b.act(P, st, AF.Exp, scale=0.125)
                        return P

                    P = s1(0)
                    for J in range(nJ):
                        Pn = s1(J + 1) if J + 1 < nJ else None
                        b.mm(OT, vv[:, J, :], P, J == 0, J == nJ - 1)
                        b.mm(LT, onesb, P, J == 0, J == nJ - 1)
                        P = Pn
                    rl = rl_pool()
                    b.recip(rl, LT)
                    o_c = oc_pool()
                    b.tt('vector', o_c, OT, rl, ALU.mult)
                    oc.append(o_c)
                o = oc[0]
                b.stt('vector', o, oc[1], neglam, oc[0], ALU.mult, ALU.add)
                sq = oc[1]
                b.tt('gpsimd', sq, o, o, ALU.mult)
                ss = bank(6)
                b.mm(ss, onesf, sq)
                rstd = rl_pool()
                b.rsqrt(rstd, ss, 128e-6, rstd)
                b.stt('vector', BIG[:, h, qs], o, dgS, rstd, ALU.mult, ALU.mult)
        b.pop()
        stop_at('eB')
        b.push()
        hgGT = csq[:, 256:512]
        hgU = csq[:, 512:640]
        tri4 = b.sb([128, 4, 128], F32, "tri4")
        for h in range(4):
            b.cp('gpsimd', tri4[:, h, :], csq[:, 384:512])
        S0 = [b.sb([128, 128], F32, "S0") for _ in range(4)]
        S1 = [b.sb([128, 128], F32, "S1") for _ in range(4)]
        Sb0 = [b.sb([128, 128], BF16, "Sb0") for _ in range(4)]
        Sb1 = [b.sb([128, 128], BF16, "Sb1") for _ in range(4)]
        for h in range(4):
            b.memset('vector', S0[h], 0.0)
            b.memset('gpsimd', Sb0[h], 0.0)
        lf_pool = b.pool([128, 512], F32, 2, "LFt")
        kt_pool = b.pool([128, 512], F32, 2, "Kt")
        vt_pool = b.pool([128, 512], BF16, 2, "Vt")
        qs_pool = b.pool([128, 4, 128], F32, 2, "QsT")
        ktt_pool = b.pool([128, 4, 128], F32, 2, "KTt")
        sg_pool = b.pool([128, 4, 128], BF16, 2, "SGt")
        E_pool = [b.pool([128, 4, 128], F32, 2, "E%d" % i) for i in range(3)]
        E3_pool = b.pool([128, 512], F32, 2, "E3")
        bf_pool = [b.pool([128, 4, 128], BF16, 2, "hb%d" % i) for i in range(3)]
        kd_pool = b.pool([128, 512], BF16, 4, "kd")
        at_pool = b.pool([128, 512], BF16, 2, "attm")
        sq_pool = b.pool([128, 512], F32, 2, "hsq")
        QSTv = QST.re("(h d) t -> d h t", d=128)
        KTv = KTs.re("(h d) t -> d h t", d=128)
        SGTv = SGT.re("(h d) t -> d h t", d=128)
        for ti in range(NT):
            ts_ = slice(ti * 128, (ti + 1) * 128)
            LFt, Kt, Vt = lf_pool(), kt_pool(), vt_pool()
            QsT, KTt, SGt = qs_pool(), ktt_pool(), sg_pool()
            b.dma('sync', LFt, LF[ts_, :])
            b.dma('sync', Kt, KTOK[ts_, :])
            b.dma('sync', Vt, VH[ts_, :])
            b.dma('sync', QsT, QSTv[:, :, ts_])
            b.dma('sync', KTt, KTv[:, :, ts_])
            b.dma('sync', SGt, SGTv[:, :, ts_])
            for h in range(4):
                b.mm(bank(h // 2)[:, (h % 2) * 256:(h % 2 + 1) * 256], LFt[:, h * 128:(h + 1) * 128], hgGT)
            pbu = bank(2)
            b.mm(pbu, hgU, LFt)
            E1, E1i, E2 = E_pool[0](), E_pool[1](), E_pool[2]()
            for hf2 in range(2):
                pv = bank(hf2).re("p (h a c) -> p h a c", h=2, a=2)
                hs = slice(2 * hf2, 2 * hf2 + 2)
                b.act(E1[:, hs, :], pv[:, :, 0, :], AF.Exp)
                b.act(E1i[:, hs, :], pv[:, :, 0, :], AF.Exp, scale=-1.0)
                b.act(E2[:, hs, :], pv[:, :, 1, :], AF.Exp)
            stop_at('c0')
            E3 = E3_pool()
            b.act(E3, pbu, AF.Exp)
            qbT, kbT, qeT = bf_pool[0](), bf_pool[1](), bf_pool[2]()
            b.tt('vector', qbT, QsT, E1, ALU.mult)
            b.tt('gpsimd', kbT, KTt, E1i, ALU.mult)
            b.tt('vector', qeT, QsT, E2, ALU.mult)
            kdA, kdB = kd_pool(), kd_pool()
            b.stt('vector', kdA, Kt, csq[:, 447:448], E3, ALU.mult, ALU.mult)
            b.stt('vector', kdB, Kt, csq[:, 511:512], E3, ALU.mult, ALU.mult)
            stop_at('c1')
            patt = bank(3)
            for h in range(4):
                b.mm(patt[:, h * 128:(h + 1) * 128], kbT[:, h, :], qbT[:, h, :])
            attm = at_pool()
            b.tt('vector', attm, patt, tri4.re("p h c -> p (h c)"), ALU.mult)
            for h in range(4):
                for chk in range(2):
                    reg = bank(4 + h // 2)[:, ((h % 2) * 2 + chk) * 128:((h % 2) * 2 + chk + 1) * 128]
                    b.mm(reg, (kdA, kdB)[chk][:, h * 128:(h + 1) * 128], Vt[:, h * 128:(h + 1) * 128])
            stop_at('c2')
            po = bank(6)
            for h in range(4):
                hc = slice(h * 128, (h + 1) * 128)
                ca = slice(h * 128, h * 128 + 64)
                cb_ = slice(h * 128 + 64, h * 128 + 128)
                b.mm(po[:, ca], Vt[:, hc], attm[:, ca], True, False)
                b.mm(po[:, ca], Sb0[h], qeT[:, h, 0:64], False, True)
                dSa = bank(4 + h // 2)[:, ((h % 2) * 2) * 128:((h % 2) * 2 + 1) * 128]
                dSb = bank(4 + h // 2)[:, ((h % 2) * 2 + 1) * 128:((h % 2) * 2 + 2) * 128]
                b.stt('vector', S1[h], S0[h], E2[:, h, 63:64], dSa, ALU.mult, ALU.add)
                b.cp('gpsimd', Sb1[h], S1[h])
                b.mm(po[:, cb_], Vt[:, hc], attm[:, cb_], True, False)
                b.mm(po[:, cb_], Sb1[h], qeT[:, h, 64:128], False, True)
                b.stt('vector', S0[h], S1[h], E2[:, h, 127:128], dSb, ALU.mult, ALU.add)
                b.cp('gpsimd', Sb0[h], S0[h])
            stop_at('c3')
            sq = sq_pool()
            b.act(sq, po, AF.Square)
            pss = bank(7)
            b.mm(pss, onesf, sq)
            rstd = sq_pool()
            b.rsqrt(rstd, pss, 128e-6, rstd)
            b.stt('vector', sq, po, hgS, rstd, ALU.mult, ALU.mult)
            b.tt('gpsimd', BIG[:, 4:8, ts_], sq.re("p (h c) -> p h c", h=4), SGt, ALU.mult)
        b.pop()
        b.push()
        outproj_epilogue(BIG, I['even_w_out'][j], xsrc, xdst)
        b.pop()
        b.pop()

    cur = x_in
    for si, name in enumerate(plan):
        dst = out if si == len(plan) - 1 else xs[si % 2]
        l = int(name[-1])
        try:
            if name.startswith('moe'):
                moe_sublayer(l, cur, dst)
            elif name.startswith('even'):
                even_sublayer(l, cur, dst)
            elif name.startswith('odd'):
                odd_sublayer(l, cur, dst)
        except _Stop:
            break
        cur = dst
    b.P.barrier()
    b.P.emit()
    return nc, b


def prep_inputs(inputs, T):
    f = lambda a: np.ascontiguousarray(np.asarray(a, dtype=np.float32))
    shared = {}
    for k in IN_SHAPES:
        a = f(inputs[k])
        if k in ('lru_wa', 'lru_wx'):
            bd = np.zeros((4, 128, 128), np.float32)
            for g in range(8):
                bd[g // 2, (g % 2) * 64:(g % 2 + 1) * 64, (g % 2) * 64:(g % 2 + 1) * 64] = a[0, g]
            a = bd
        elif k == 'router_b':
            a = a.reshape(1, 16)
        shared[k] = a
    shared.update(host_consts(T))
    return shared


def kernel(**inputs):
    x = np.asarray(inputs['x'], dtype=np.float32)
    c = np.asarray(inputs['c'], dtype=np.float32)
    Bn, T, _ = x.shape
    shared = prep_inputs(inputs, T)
    nc, _ = build(T, ['even0', 'moe0', 'odd1', 'moe1'])
    in_maps = []
    for i in range(Bn):
        m = dict(shared)
        m['x'] = np.ascontiguousarray(x[i])
        m['c'] = np.ascontiguousarray(c[i:i + 1])
        in_maps.append(m)
    res = run_bass_kernel_spmd(nc, in_maps, core_ids=list(range(Bn)))
    return np.stack([np.asarray(r['out'], dtype=np.float32) for r in res.results], axis=0)
```

```python
import contextlib
import numpy as np
import concourse.bass as bass
import concourse.mybir as mybir
from concourse.bass_utils import run_bass_kernel_spmd

F32 = mybir.dt.float32
BF16 = mybir.dt.bfloat16
AF = mybir.ActivationFunctionType
ALU = mybir.AluOpType
AX = mybir.AxisListType

ENGS = ['tensor', 'vector', 'scalar', 'gpsimd', 'sync']
SEM_LIMIT = 30000
D = 1024
ALPHA = 4.0 ** 0.25
N_CORES = 8


class Res:
    __slots__ = ('name', 'last_w', 'readers', 'ds')

    def __init__(self, name):
        self.name = name
        self.last_w = None
        self.readers = []
        self.ds = {}


class DSem:
    __slots__ = ('name', 'dcount', 'dsem', 'kind')

    def __init__(self, name, kind):
        self.name = name
        self.dcount = 0
        self.dsem = None
        self.kind = kind


class Prog:
    def __init__(self, nc):
        self.nc = nc
        self.ops = {e: [] for e in ENGS}
        self.seen = {x: {e: -1 for e in ENGS} for x in ENGS}
        self.seen_d = {x: {} for x in ENGS}
        self.dma_res = []
        self.free_ds = {'sw': [], 'hw': []}
        self.all_dma_toks = []
        self.stack = contextlib.ExitStack()
        self.pending = {x: [] for x in ENGS}
        self.scopes = []

    def sbuf(self, name, shape, dtype):
        st = self.scopes[-1] if self.scopes else self.stack
        return st.enter_context(self.nc.sbuf_tensor(name, list(shape), dtype))

    def push(self):
        self.scopes.append(contextlib.ExitStack())

    def pop(self):
        self.scopes.pop().close()

    def psum(self, name, shape, dtype):
        return self.stack.enter_context(self.nc.psum_tensor(name, list(shape), dtype))

    def _need(self, eng, tok, waits):
        if tok is None:
            return
        if tok[0] == 'e':
            _, e, idx = tok
            if e == eng and eng == 'tensor':
                return
            if self.seen[eng][e] >= idx:
                return
            self.seen[eng][e] = idx
            waits.append(tok)
        else:
            _, r, val = tok
            if self.seen_d[eng].get(id(r), 0) >= val:
                return
            self.seen_d[eng][id(r)] = val
            waits.append(tok)

    def op(self, eng, fn, reads=(), writes=(), dma_res=None):
        waits = []
        for r in reads:
            self._need(eng, r.last_w, waits)
        for r in writes:
            self._need(eng, r.last_w, waits)
            for t in r.readers:
                self._need(eng, t, waits)
        for t in self.pending[eng]:
            self._need(eng, t, waits)
        self.pending[eng] = []
        idx = len(self.ops[eng])
        if dma_res is not None:
            kind = 'sw' if eng == 'gpsimd' else 'hw'
            ds = dma_res.ds.get(kind)
            if ds is None:
                fl = self.free_ds[kind]
                if fl:
                    ds = fl.pop()
                else:
                    ds = DSem("%s%d" % (kind, len(self.dma_res)), kind)
                    self.dma_res.append(ds)
                dma_res.ds[kind] = ds
            dma_res = ds
            ds.dcount += 16
            tok = ('d', ds, ds.dcount)
            self.all_dma_toks.append(tok)
        else:
            tok = ('e', eng, idx)
        import sys
        fr = sys._getframe(2)
        site = "%s:%d<%s:%d" % (fr.f_code.co_name, fr.f_lineno, fr.f_back.f_code.co_name, fr.f_back.f_lineno)
        self.ops[eng].append(dict(fn=fn, waits=waits, dma_res=dma_res, site=site,
                                  dval=(dma_res.dcount if dma_res else 0), mile=False))
        for r in writes:
            r.last_w = tok
            r.readers = []
        for r in reads:
            if r not in writes:
                r.readers.append(tok)
        return tok

    def barrier(self):
        toks = []
        for e in ENGS:
            for idx in range(len(self.ops[e]) - 1, -1, -1):
                if self.ops[e][idx]['dma_res'] is None:
                    toks.append(('e', e, idx))
                    break
        dtoks = {}
        for t in self.all_dma_toks:
            dtoks[id(t[1])] = t
        self.all_dma_toks = []
        for x in ENGS:
            self.pending[x].extend(list(toks) + list(dtoks.values()))

    def emit(self):
        nc = self.nc
        self.tail = {}
        for x in ENGS:
            w = []
            for t in self.pending[x]:
                self._need(x, t, w)
            self.tail[x] = w
            for t in w:
                if t[0] == 'e':
                    self.ops[t[1]][t[2]]['mile'] = True
        for x in ENGS:
            for o in self.ops[x]:
                for t in o['waits']:
                    if t[0] == 'e':
                        self.ops[t[1]][t[2]]['mile'] = True
        nsem = 0
        for x in ENGS:
            cnt = 0
            cur = None
            for o in self.ops[x]:
                if o['mile'] and o['dma_res'] is None:
                    if cur is None or cnt >= SEM_LIMIT:
                        cur = self.stack.enter_context(nc.semaphore(f"m_{x}_{nsem}"))
                        nsem += 1
                        cnt = 0
                    cnt += 1
                    o['msem'] = (cur, cnt)
        per16 = (SEM_LIMIT // 16) * 16
        for r in self.dma_res:
            n = r.dcount
            r.dsem = []
            for i in range((n + per16 - 1) // per16):
                r.dsem.append(self.stack.enter_context(nc.semaphore(f"d_{r.name}_{i}")))
                nsem += 1
        self.nsem = nsem

        def dsem_of(r, val):
            k = (val - 16) // per16
            return r.dsem[k], val - k * per16

        ops = self.ops
        self.imap = {}
        ctr = [0]

        def body(x):
            def f(eng):
                for o in ops[x]:
                    self.imap[ctr[0] + len(o['waits'])] = (x, o['site'])
                    ctr[0] += len(o['waits']) + 1
                    for t in o['waits']:
                        if t[0] == 'e':
                            s, v = ops[t[1]][t[2]]['msem']
                        else:
                            s, v = dsem_of(t[1], t[2])
                        eng.wait_ge(s, v)
                    inst = o['fn'](eng)
                    if o['dma_res'] is not None:
                        s, v = dsem_of(o['dma_res'], o['dval'])
                        inst.then_inc(s, 16)
                    elif o['mile']:
                        s, v = o['msem']
                        inst.then_inc(s, 1)
                for t in self.tail[x]:
                    if t[0] == 'e':
                        s, v = ops[t[1]][t[2]]['msem']
                    else:
                        s, v = dsem_of(t[1], t[2])
                    eng.wait_ge(s, v)
            return f

        with nc.Block() as block:
            block.tensor(body('tensor'))
            block.vector(body('vector'))
            block.scalar(body('scalar'))
            block.gpsimd(body('gpsimd'))
            block.sync(body('sync'))
        self.stack.close()


class V:
    __slots__ = ('ap', 'r')

    def __init__(self, ap, r):
        self.ap = ap
        self.r = r

    def __getitem__(self, k):
        return V(self.ap[k], self.r)

    def re(self, s, **kw):
        return V(self.ap.rearrange(s, **kw), self.r)

    def pb(self, n=128):
        return V(self.ap.partition_broadcast(n), self.r)


def _rs(xs):
    out = []
    for x in xs:
        if isinstance(x, V) and x.r not in out:
            out.append(x.r)
    return out


def _a(x):
    return x.ap if isinstance(x, V) else x


class Bld:
    ARENA_WORDS = 52992

    def __init__(self, nc):
        self.nc = nc
        self.P = Prog(nc)
        self.n = 0
        self.arena = self.P.stack.enter_context(nc.sbuf_tensor("arena", [128, self.ARENA_WORDS], F32))
        self.off = 0
        self.marks = []
        self.peak = 0
        self.scope_res = [[]]

    def push(self):
        self.marks.append(self.off)
        self.scope_res.append([])

    def pop(self):
        self.off = self.marks.pop()
        self.P.barrier()
        for r in self.scope_res.pop():
            for kind, ds in r.ds.items():
                self.P.free_ds[kind].append(ds)
            r.ds = {}

    def sb(self, shape, dt, name=None):
        self.n += 1
        name = f"{name or 't'}_{self.n}"
        nel = int(np.prod(shape[1:]))
        nbytes = nel * (2 if dt == BF16 else 4)
        words = (nbytes + 63) // 64 * 16
        assert self.off + words <= self.ARENA_WORDS, f"SBUF arena overflow at {name}: {self.off * 4} + {words * 4}"
        ap = self.arena[0:shape[0], self.off:self.off + (nbytes + 3) // 4]
        self.off += words
        self.peak = max(self.peak, self.off * 4)
        if dt == BF16:
            ap = ap.bitcast(BF16)
            if nel % 2:
                ap = ap[:, 0:nel]
        if len(shape) == 3:
            ap = ap.rearrange("p (a b) -> p a b", a=shape[1])
        elif len(shape) == 4:
            ap = ap.rearrange("p (a b c) -> p a b c", a=shape[1], b=shape[2])
        r = Res(name)
        self.scope_res[-1].append(r)
        return V(ap, r)

    def pool(self, shape, dt, n, name=None):
        tiles = [self.sb(shape, dt, name) for _ in range(n)]
        st = [0]

        def nxt():
            st[0] += 1
            return tiles[(st[0] - 1) % n]
        return nxt

    def act(self, out, in_, func, bias=None, scale=None):
        kw = {}
        if bias is not None:
            kw['bias'] = _a(bias)
        if scale is not None:
            kw['scale'] = _a(scale)
        o, i = out.ap, in_.ap
        self.P.op('scalar', lambda e: e.activation(out=o, in_=i, func=func, **kw),
                  reads=_rs([in_, bias, scale]), writes=[out.r])

    def tt(self, eng, out, a, b, op):
        o, x, y = out.ap, a.ap, b.ap
        self.P.op(eng, lambda e: e.tensor_tensor(out=o, in0=x, in1=y, op=op), reads=_rs([a, b]), writes=[out.r])

    def ts(self, eng, out, a, s1, op0, s2=None, op1=None):
        o, x, p, q = out.ap, a.ap, _a(s1), _a(s2)
        if s2 is None:
            fn = lambda e: e.tensor_scalar(out=o, in0=x, scalar1=p, scalar2=None, op0=op0)
        else:
            fn = lambda e: e.tensor_scalar(out=o, in0=x, scalar1=p, scalar2=q, op0=op0, op1=op1)
        self.P.op(eng, fn, reads=_rs([a, s1, s2]), writes=[out.r])

    def stt(self, eng, out, a, s, b, op0, op1):
        o, x, p, y = out.ap, a.ap, _a(s), b.ap
        eng = 'vector'
        self.P.op(eng, lambda e: e.scalar_tensor_tensor(out=o, in0=x, scalar=p, in1=y, op0=op0, op1=op1),
                  reads=_rs([a, s, b]), writes=[out.r])

    def rsqrt(self, out, a, eps, tmp, scale=1.0):
        self.act(tmp, a, AF.Ln, bias=eps, scale=scale)
        self.act(out, tmp, AF.Exp, scale=-0.5)

    def cp(self, eng, out, a):
        o, x = out.ap, a.ap
        if eng == 'scalar':
            self.P.op(eng, lambda e: e.copy(out=o, in_=x), reads=[a.r], writes=[out.r])
        else:
            self.P.op(eng, lambda e: e.tensor_copy(out=o, in_=x), reads=[a.r], writes=[out.r])

    def red(self, out, a, op):
        o, x = out.ap, a.ap
        self.P.op('vector', lambda e: e.tensor_reduce(out=o, in_=x, axis=AX.X, op=op), reads=[a.r], writes=[out.r])

    def recip(self, out, a):
        o, x = out.ap, a.ap
        self.P.op('vector', lambda e: e.reciprocal(out=o, in_=x), reads=[a.r], writes=[out.r])

    def scan(self, out, d0, d1, init):
        o, x, y, z = out.ap, d0.ap, d1.ap, _a(init)
        self.P.op('vector', lambda e: e.tensor_tensor_scan(out=o, data0=x, data1=y, initial=z, op0=ALU.mult,
                                                          op1=ALU.add), reads=_rs([d0, d1, init]), writes=[out.r])

    def memset(self, eng, out, val):
        o = out.ap
        self.P.op(eng, lambda e: e.memset(o, val), writes=[out.r])

    def mm(self, out, lhsT, rhs, start=True, stop=True, xr=()):
        o, l, r = out.ap, lhsT.ap, rhs.ap
        self.P.op('tensor', lambda e: e.matmul(o, l, r, start=start, stop=stop),
                  reads=_rs([lhsT, rhs] + list(xr)), writes=[out.r])

    def tr(self, out, in_, ident):
        o, i, d = out.ap, in_.ap, ident.ap
        self.P.op('tensor', lambda e: e.transpose(o, i, d), reads=_rs([in_, ident]), writes=[out.r])

    def dma(self, q, out, in_, slow=False):
        o, i = out.ap, in_.ap
        kw = dict(allow_slow_non_contiguous=True) if slow else {}
        self.P.op(q, lambda e: e.dma_start(out=o, in_=i, **kw), reads=[in_.r], writes=[out.r], dma_res=out.r)


def host_consts(T):
    p = np.arange(128)
    sq = np.zeros((5, 128, 128), np.float32)
    sq[0] = np.eye(128)
    sq[1] = (p[:, None] > p[None, :])
    same = (p[:, None] // 64) == (p[None, :] // 64)
    mid = (p // 64) * 64 + 31
    s_, c_ = p[:, None], p[None, :]
    sq[2] = (same & (s_ > mid[None, :]) & (s_ <= c_)) * 1.0 - (same & (s_ > c_) & (s_ <= mid[None, :])) * 1.0
    sq[3] = same & (s_ <= c_)
    sq[4] = same & (s_ > c_)
    csq = np.ascontiguousarray(sq.transpose(1, 0, 2).reshape(128, 640))
    i = np.arange(512)
    mk = np.zeros((8, 128, 512), np.float32)
    for r in range(4):
        mk[r] = (128 * r + p[:, None]) <= i[None, :]
        mk[4 + r] = (128 * r + p[:, None]) < i[None, :]
    t = np.arange(T)
    aug = np.zeros((4, 2, 4, T), np.float32)
    for h in range(4):
        sl = 2.0 ** (-8.0 * (h + 1) / 4)
        aug[h, 0, 0] = -8 * sl * 64 * (t // 64)
        aug[h, 0, 1] = -8 * sl * (t % 64)
        aug[h, 0, 2] = 1
        aug[h, 0, 3] = 1
        aug[h, 1, 0] = 1
        aug[h, 1, 1] = 1
        aug[h, 1, 2] = 8 * sl * 64 * (t // 64)
        aug[h, 1, 3] = 8 * sl * (t % 64)
    return dict(c_sq=csq, c_mask=mk, c_aug=aug)


IN_SHAPES = dict(
    ada_w=[2, 1024, 6144], ada_b=[2, 6144], ln_g=[2, 2, 1024], ln_b=[2, 2, 1024],
    even_w_in=[1, 1024, 3584], even_w_out=[1, 1024, 1024], diff_lambda=[1, 4, 64], diff_gain=[1, 128],
    hgrn_gamma=[3, 512], hgrn_gain=[1, 128], odd_w_in=[1, 1024, 2560], odd_w_out=[1, 1024, 1024],
    conv_w=[1, 4, 512], conv_b=[1, 512], lru_wa=[4, 128, 128], lru_ba=[1, 512], lru_wx=[4, 128, 128],
    lru_bx=[1, 512], lru_lambda=[1, 512], router_w=[1024, 16], router_b=[1, 16],
    moe_w_gate=[2, 16, 1024, 512], moe_w_up=[2, 16, 1024, 512], moe_w_down=[2, 16, 512, 1024],
)


class _Stop(Exception):
    pass


STOP = None


def build(T, plan):
    nc = bass.Bass("TRN2", target_bir_lowering=False)
    b = Bld(nc)
    NT, NCH = T // 128, T // 512

    def din(name, shape, dt=F32):
        return V(nc.dram_tensor(name, list(shape), dt, kind="ExternalInput").ap(), Res(name))

    def dscr(name, shape, dt):
        return V(nc.dram_tensor(name, list(shape), dt).ap(), Res(name))

    I = {k: din(k, s) for k, s in IN_SHAPES.items()}
    x_in = din("x", [T, D])
    c_in = din("c", [1, D])
    c_sq = din("c_sq", [128, 640])
    c_mask = din("c_mask", [8, 128, 512])
    c_aug = din("c_aug", [4, 2, 4, T])
    out = V(nc.dram_tensor("out", [T, D], F32, kind="ExternalOutput").ap(), Res("out"))
    xs = [dscr("xs0", [T, D], F32), dscr("xs1", [T, D], F32)]
    modscr = dscr("modscr", [2, 6144], F32)

    PS = []
    for i in range(4):
        t = b.P.psum(f"ps{i}", [128, 1024], F32)
        PS.append([V(t[:, 0:512], Res(f"ps{i}a")), V(t[:, 512:1024], Res(f"ps{i}b")), t])

    def bank(i):
        return PS[i // 2][i % 2]

    csq = b.sb([128, 640], F32, "csq")
    b.dma('sync', csq, c_sq)
    ident = csq[:, 0:128]
    onesf = b.sb([128, 128], F32, "onesf")
    b.memset('vector', onesf, 1.0)
    onesb = b.sb([128, 128], BF16, "onesb")
    b.memset('vector', onesb, 1.0)

    b.push()
    cT = b.sb([128, 8], F32, "cT")
    b.dma('sync', cT, c_in.re("o (k p) -> p (o k)", p=128), slow=True)
    cTs = b.sb([128, 8], F32, "cTs")
    b.act(cTs, cT, AF.Silu)
    cTb = b.sb([128, 8], BF16, "cTb")
    b.cp('vector', cTb, cTs)
    wsec_pool = b.pool([128, 8, 1024], BF16, 2, "wsec")
    layers_needed = sorted({int(p[-1]) for p in plan})
    for l in layers_needed:
        modrow = b.sb([1, 6144], F32, "modrow")
        adab = b.sb([1, 6144], F32, "adab")
        b.dma('sync', adab, I['ada_b'][l:l + 1, :])
        for sec in range(6):
            ws = wsec_pool()
            b.dma('gpsimd', ws, I['ada_w'][l][:, sec * 1024:(sec + 1) * 1024].re("(k p) n -> p k n", p=128))
            for nh in range(2):
                ps = bank(nh)
                for kc in range(8):
                    b.mm(ps[0:1, :], cTb[:, kc:kc + 1], ws[:, kc, nh * 512:(nh + 1) * 512], kc == 0, kc == 7)
                c0 = sec * 1024 + nh * 512
                b.tt('vector', modrow[0:1, c0:c0 + 512], ps[0:1, :], adab[0:1, c0:c0 + 512], ALU.add)
            if sec in (1, 2, 4, 5):
                b.ts('vector', modrow[0:1, sec * 1024:(sec + 1) * 1024], modrow[0:1, sec * 1024:(sec + 1) * 1024],
                     1.0, ALU.add)
        b.dma('sync', modscr[l:l + 1, :], modrow)
    b.pop()

    xt_pool = b.pool([128, D], F32, 2, "xt")
    hf_pool = b.pool([128, 8, 128], F32, 2, "hf")
    scp = b.sb([128, 8], F32, "scp")
    shv = b.sb([128, 8], F32, "shv")
    gB = b.sb([128, D], F32, "gB")
    lnG = b.sb([128, D], F32, "lnG")
    lnB = b.sb([128, D], F32, "lnB")
    ework = [b.pool([128, D], F32, 2, "ew%d" % i) for i in range(2)]
    small = b.pool([128, 8], F32, 4, "small")

    def load_mod(l, s):
        sec_sh, sec_sc, sec_g = (0, 1, 2) if s == 0 else (3, 4, 5)
        b.dma('sync', shv, modscr[l, sec_sh * 1024:(sec_sh + 1) * 1024].re("(k p) -> p k", p=128), slow=True)
        b.dma('sync', scp, modscr[l, sec_sc * 1024:(sec_sc + 1) * 1024].re("(k p) -> p k", p=128), slow=True)
        b.dma('sync', gB, modscr[l, sec_g * 1024:(sec_g + 1) * 1024].pb())
        b.dma('sync', lnG, I['ln_g'][l, s, :].pb())
        b.dma('sync', lnB, I['ln_b'][l, s, :].pb())

    def prologue(xsrc, tok0, ntiles, hT, col0, router=None):
        for i in range(ntiles):
            t0 = tok0 + i * 128
            xt = xt_pool()
            b.dma('sync', xt, xsrc[t0:t0 + 128, :])
            pa, pb_ = PS[3][0], PS[3][1]
            for kc in range(8):
                pk = (pa if kc < 4 else pb_)[:, (kc % 4) * 128:(kc % 4 + 1) * 128]
                b.tr(pk, xt[:, kc * 128:(kc + 1) * 128], ident)
            hf = hf_pool()
            for kc in range(8):
                pk = (pa if kc < 4 else pb_)[:, (kc % 4) * 128:(kc % 4 + 1) * 128]
                b.act(hf[:, kc, :], pk, AF.Identity, bias=shv[:, kc:kc + 1], scale=scp[:, kc:kc + 1])
            b.cp('vector', hT[:, :, col0 + i * 128:col0 + (i + 1) * 128], hf)
            if router is not None:
                router(hf, col0 + i * 128)

    def epilogue(xsrc, xdst, t0, y):
        xe = xt_pool()
        b.dma('sync', xe, xsrc[t0:t0 + 128, :])
        t1 = ework[0]()
        if isinstance(y, tuple):
            b.tt('vector', t1[:, 0:512], y[0], gB[:, 0:512], ALU.mult)
            b.tt('vector', t1[:, 512:1024], y[1], gB[:, 512:1024], ALU.mult)
        else:
            b.tt('vector', t1, y, gB, ALU.mult)
        r = ework[1]()
        b.stt('gpsimd', r, xe, ALPHA, t1, ALU.mult, ALU.add)
        b.tt('gpsimd', t1, r, r, ALU.mult)
        st = small()
        b.red(st[:, 0:1], r, ALU.add)
        b.red(st[:, 1:2], t1, ALU.add)
        b.ts('vector', st[:, 2:4], st[:, 0:2], 1.0 / D, ALU.mult)
        b.tt('vector', st[:, 4:5], st[:, 2:3], st[:, 2:3], ALU.mult)
        b.tt('vector', st[:, 5:6], st[:, 3:4], st[:, 4:5], ALU.subtract)
        b.rsqrt(st[:, 6:7], st[:, 5:6], 1e-5, st[:, 7:8])
        b.ts('vector', t1, r, st[:, 2:3], ALU.subtract, st[:, 6:7], ALU.mult)
        b.tt('gpsimd', r, t1, lnG, ALU.mult)
        xo = xe
        b.tt('vector', xo, r, lnB, ALU.add)
        b.dma('sync', xdst[t0:t0 + 128, :], xo)

    def moe_sublayer(l, xsrc, xdst):
        b.push()
        load_mod(l, 1)
        TH = min(T, 1024)
        BIG = b.sb([128, 8, TH], BF16, "hTm")
        rw = b.sb([128, 8, 16], F32, "rw")
        b.dma('sync', rw, I['router_w'].re("(k p) e -> p k e", p=128))
        rb = b.sb([1, 16], F32, "rb")
        b.dma('sync', rb, I['router_b'])
        gatesT = b.sb([16, TH], F32, "gatesT")
        sel = b.sb([16, 16, 128], F32, "sel")
        b.memset('vector', sel, 0.0)
        for e in range(16):
            b.dma('sync', sel[e:e + 1, e, :], onesf[e:e + 1, :])
        yacc = b.sb([128, TH // 128, D], F32, "yacc")
        wg_pool = b.pool([128, 8, 512], BF16, 3, "wg")
        wu_pool = b.pool([128, 8, 512], BF16, 3, "wu")
        wd_pool = b.pool([128, 4, D], BF16, 3, "wd")
        s_pool = b.pool([128, 512], BF16, 2, "s")
        t_pool = b.pool([128, 512], BF16, 2, "tg")
        gbs_pool = b.pool([128, 512], F32, 2, "gbs")
        act_pool = b.pool([128, 4, 512], BF16, 3, "actT")
        rt = b.pool([128, 16], F32, 6, "rt")
        r4 = b.pool([128, 4], F32, 8, "r4")
        r1 = b.pool([128, 1], F32, 8, "r1")

        def router(hf, col):
            lg = bank(5)[:, 0:16]
            for kc in range(8):
                b.mm(lg, hf[:, kc, :], rw[:, kc, :], kc == 0, False)
            b.mm(lg, onesf[0:1, :], rb[0:1, :], False, True)
            mx = r1()
            b.red(mx, lg, ALU.max)
            nmx = r1()
            b.ts('vector', nmx, mx, -1.0, ALU.mult)
            ex = rt()
            b.act(ex, lg, AF.Exp, bias=nmx, scale=1.0)
            sm = r1()
            b.red(sm, ex, ALU.add)
            rs = r1()
            b.recip(rs, sm)
            p = rt()
            b.ts('vector', p, ex, rs, ALU.mult)
            pv = p.re("p (g j) -> p g j", j=4)
            prs = []
            for (i0, i1) in ((0, 1), (0, 2), (0, 3), (1, 2), (1, 3), (2, 3)):
                q = r4()
                b.tt('vector', q, pv[:, :, i0], pv[:, :, i1], ALU.add)
                prs.append(q)
            gs = prs[0]
            for q in prs[1:]:
                g2 = r4()
                b.tt('vector', g2, gs, q, ALU.max)
                gs = g2
            gm = r1()
            b.red(gm, gs, ALU.max)
            ing = r4()
            b.ts('vector', ing, gs, gm, ALU.is_equal)
            im1 = r4()
            b.ts('vector', im1, ing, -1.0, ALU.add)
            msk = rt()
            for g in range(4):
                b.ts('vector', msk[:, g * 4:(g + 1) * 4], p[:, g * 4:(g + 1) * 4], ing[:, g:g + 1], ALU.mult,
                     im1[:, g:g + 1], ALU.add)
            tp1 = r1()
            b.red(tp1, msk, ALU.max)
            is1 = rt()
            b.ts('vector', is1, msk, tp1, ALU.is_equal)
            m2 = rt()
            b.stt('vector', m2, is1, -2.0, msk, ALU.mult, ALU.add)
            tp2 = r1()
            b.red(tp2, m2, ALU.max)
            is2 = rt()
            b.ts('vector', is2, m2, tp2, ALU.is_equal)
            sl = rt()
            b.tt('vector', sl, is1, is2, ALU.add)
            den = r1()
            b.tt('vector', den, tp1, tp2, ALU.add)
            rden = r1()
            b.recip(rden, den)
            gt = rt()
            b.stt('vector', gt, p, rden, sl, ALU.mult, ALU.mult)
            gp = bank(5)[0:16, 128:256]
            b.tr(gp, gt, ident)
            b.cp('vector', gatesT[:, col:col + 128], gp)

        def load_w(e):
            wg, wu, wd = wg_pool(), wu_pool(), wd_pool()
            b.dma('gpsimd', wg, I['moe_w_gate'][l, e].re("(k p) f -> p k f", p=128))
            b.dma('gpsimd', wu, I['moe_w_up'][l, e].re("(k p) f -> p k f", p=128))
            b.dma('gpsimd', wd, I['moe_w_down'][l, e].re("(k p) n -> p k n", p=128))
            return wg, wu, wd

        ycnt = [0]
        for half in range(T // TH):
            tok0 = half * TH
            wnext = load_w(0)
            prologue(xsrc, tok0, TH // 128, BIG, 0, router=router)
            nch = TH // 512
            items = [(e, ch) for e in range(16) for ch in range(nch)]
            wcur = {}

            def stage1(e, ch, wts):
                wg, wu, wd = wts
                c0 = ch * 512
                gbp = bank(4)
                b.mm(gbp, sel[:, e, :], gatesT[:, c0:c0 + 512], True, True)
                gbs = gbs_pool()
                b.cp('scalar', gbs, gbp)
                actT = act_pool()
                for fc in range(4):
                    pg, pu = bank(2 * (fc % 2)), bank(2 * (fc % 2) + 1)
                    for kc in range(8):
                        b.mm(pg, wg[:, kc, fc * 128:(fc + 1) * 128], BIG[:, kc, c0:c0 + 512], kc == 0, kc == 7)
                    for kc in range(8):
                        b.mm(pu, wu[:, kc, fc * 128:(fc + 1) * 128], BIG[:, kc, c0:c0 + 512], kc == 0, kc == 7)
                    s_ = s_pool()
                    b.act(s_, pg, AF.Silu)
                    tg = t_pool()
                    b.tt('vector', tg, pu, gbs, ALU.mult)
                    b.tt('gpsimd', actT[:, fc, :], s_, tg, ALU.mult)
                return actT

            def stage2(e, ch, wts, actT):
                wd = wts[2]
                for ti in range(4):
                    ycnt[0] += 1
                    ya, yb = (PS[3][0], PS[3][1]) if ycnt[0] % 2 else (bank(4), bank(5))
                    for nh, yp in ((0, ya), (1, yb)):
                        for fc in range(4):
                            b.mm(yp, actT[:, fc, ti * 128:(ti + 1) * 128], wd[:, fc, nh * 512:(nh + 1) * 512],
                                 fc == 0, fc == 3)
                    tix = ch * 4 + ti
                    for nh, yp in ((0, ya), (1, yb)):
                        ys = yacc[:, tix, nh * 512:(nh + 1) * 512]
                        if e == 0:
                            b.cp('vector', ys, yp)
                        else:
                            b.tt('vector', ys, ys, yp, ALU.add)

            prev = None
            for (e, ch) in items:
                if ch == 0:
                    wcur[e] = wnext
                    if e + 1 < 16:
                        wnext = load_w(e + 1)
                a = stage1(e, ch, wcur[e])
                if prev is not None:
                    stage2(*prev)
                prev = (e, ch, wcur[e], a)
            stage2(*prev)
            for i in range(TH // 128):
                epilogue(xsrc, xdst, tok0 + i * 128, yacc[:, i, :])
        b.pop()

    def load_wsec(pool, win, sec):
        w = pool()
        b.dma('gpsimd', w, win[:, sec * 512:(sec + 1) * 512].re("(k p) n -> p k n", p=128))
        return w

    def proj_heads64(BIG, w, dst, stage_pool):
        for g in range(8):
            stg = stage_pool()
            for ch in range(NCH):
                pq = bank(ch % 2)
                for kc in range(8):
                    b.mm(pq[0:64, :], w[:, kc, g * 64:(g + 1) * 64], BIG[:, kc, ch * 512:(ch + 1) * 512], kc == 0, kc == 7)
                b.cp('scalar' if ch % 2 else 'vector', stg[0:64, ch * 512:(ch + 1) * 512], pq[0:64, :])
            b.dma('sync', dst[g], stg[0:64, :])

    def proj_tok(BIG, w, dst, st_pool):
        for ti in range(NT):
            pv = bank(ti % 2)
            for kc in range(8):
                b.mm(pv, BIG[:, kc, ti * 128:(ti + 1) * 128], w[:, kc, :], kc == 0, kc == 7)
            st = st_pool()
            b.cp('scalar' if ti % 2 else 'vector', st, pv)
            b.dma('sync', dst[ti * 128:(ti + 1) * 128, :], st)

    def outproj_epilogue(BIG, wout, xsrc, xdst):
        wo = b.sb([128, 8, D], BF16, "wo")
        b.dma('gpsimd', wo, wout.re("(k p) n -> p k n", p=128))
        for ti in range(NT):
            ya, yb = PS[2 + ti % 2][0], PS[2 + ti % 2][1]
            for nh, yp in ((0, ya), (1, yb)):
                for kc in range(8):
                    b.mm(yp, BIG[:, kc, ti * 128:(ti + 1) * 128], wo[:, kc, nh * 512:(nh + 1) * 512], kc == 0, kc == 7)
            epilogue(xsrc, xdst, ti * 128, (ya, yb))

    def stop_at(tag):
        if STOP == tag:
            raise _Stop()

    def colvec(src_row, n):
        t = b.sb([128, n], F32, "colv")
        b.dma('sync', t, src_row.re("(c p) -> p c", p=128), slow=True)
        return t

    def odd_sublayer(l, xsrc, xdst):
        j = l // 2
        b.push()
        load_mod(l, 0)
        BIG = b.sb([128, 8, T], BF16, "BIGo")
        prologue(xsrc, 0, NT, BIG, 0)
        win = I['odd_w_in'][j]
        QS = dscr("QS", [8, 64, T], BF16)
        KS = dscr("KS", [8, 64, T], BF16)
        VS = dscr("VS", [T, 512], BF16)
        YR = dscr("YR", [512, T], BF16)
        b.push()
        wpool = b.pool([128, 8, 512], BF16, 2, "wsec")
        cw = b.sb([128, 4, 4], F32, "cw")
        for tap in range(4):
            b.dma('sync', cw[:, tap, :], I['conv_w'][j][tap].re("(c p) -> p c", p=128), slow=True)
        cbv = colvec(I['conv_b'][j], 4)
        bav = colvec(I['lru_ba'][j], 4)
        bxv = colvec(I['lru_bx'][j], 4)
        lamv = colvec(I['lru_lambda'][j], 4)
        e1 = b.sb([128, 4], F32, "e1")
        b.act(e1, lamv, AF.Exp, scale=-1.0)
        spl = b.sb([128, 4], F32, "spl")
        b.act(spl, e1, AF.Ln, bias=1.0)
        negc8 = b.sb([128, 4], F32, "negc8")
        b.ts('vector', negc8, spl, -8.0, ALU.mult)
        wa = b.sb([128, 4, 128], F32, "wa")
        b.dma('sync', wa, I['lru_wa'].re("c p q -> p c q"))
        wx = b.sb([128, 4, 128], F32, "wx")
        b.dma('sync', wx, I['lru_wx'].re("c p q -> p c q"))
        wxr = load_wsec(wpool, win, 0)
        wgr = load_wsec(wpool, win, 1)
        hlast = b.sb([128, 4], F32, "hlast")
        b.memset('vector', hlast, 0.0)
        xrp = [b.sb([128, 515], F32, "xrp") for _ in range(4)]
        for cc in range(4):
            b.memset('gpsimd', xrp[cc][:, 0:3], 0.0)
        f_pool = [b.pool([128, 512], F32, 2, "lf%d" % i) for i in range(6)]
        yr_pool = b.pool([128, 512], BF16, 2, "yr")
        for tc in range(NCH):
            tsl = slice(tc * 512, (tc + 1) * 512)
            for cc in range(4):
                px = bank(0)
                for kc in range(8):
                    b.mm(px, wxr[:, kc, cc * 128:(cc + 1) * 128], BIG[:, kc, tsl], kc == 0, kc == 7)
                xp = xrp[cc]
                b.cp('scalar', xp[:, 3:515], px)
                xc = f_pool[0]()
                b.ts('vector', xc, xp[:, 3:515], cw[:, 3, cc:cc + 1], ALU.mult, cbv[:, cc:cc + 1], ALU.add)
                for tap in (2, 1, 0):
                    b.stt('vector', xc, xp[:, tap:tap + 512], cw[:, tap, cc:cc + 1], xc, ALU.mult, ALU.add)
                hal = small()
                b.cp('gpsimd', hal[:, 0:3], xp[:, 512:515])
                b.cp('gpsimd', xp[:, 0:3], hal[:, 0:3])
                pr, pi = bank(1), bank(2)
                b.mm(pr, wa[:, cc, :], xc)
                b.mm(pi, wx[:, cc, :], xc)
                r = f_pool[1]()
                b.act(r, pr, AF.Sigmoid, bias=bav[:, cc:cc + 1])
                ig = f_pool[2]()
                b.act(ig, pi, AF.Sigmoid, bias=bxv[:, cc:cc + 1])
                a = f_pool[3]()
                b.act(a, r, AF.Exp, scale=negc8[:, cc:cc + 1])
                om = r
                b.tt('gpsimd', om, a, a, ALU.mult)
                b.ts('vector', om, om, -1.0, ALU.mult, 1.0, ALU.add)
                b.ts('vector', om, om, 1e-12, ALU.max)
                b.act(om, om, AF.Sqrt)
                b.tt('gpsimd', ig, ig, xc, ALU.mult)
                u = f_pool[4]()
                b.tt('vector', u, ig, om, ALU.mult)
                hh = f_pool[5]()
                b.scan(hh, a, u, 0.0 if tc == 0 else hlast[:, cc:cc + 1])
                b.cp('vector', hlast[:, cc:cc + 1], hh[:, 511:512])
                pg = bank(3)
                for kc in range(8):
                    b.mm(pg, wgr[:, kc, cc * 128:(cc + 1) * 128], BIG[:, kc, tsl], kc == 0, kc == 7)
                g = xc
                b.cp('scalar', g, pg)
                g2 = ig
                b.tt('gpsimd', g2, g, g, ALU.mult)
                b.ts('vector', g2, g2, 0.044715, ALU.mult, 1.0, ALU.add)
                b.tt('gpsimd', g2, g2, g, ALU.mult)
                b.act(g2, g2, AF.Sigmoid, scale=1.5957691216057308)
                b.tt('gpsimd', g2, g2, g, ALU.mult)
                yr = yr_pool()
                b.tt('vector', yr, g2, hh, ALU.mult)
                b.dma('sync', YR[cc * 128:(cc + 1) * 128, tsl], yr)
        stop_at('oA1')
        stage_pool = b.pool([64, T], BF16, 2, "stg")
        w = load_wsec(wpool, win, 2)
        proj_heads64(BIG, w, QS, stage_pool)
        w = load_wsec(wpool, win, 3)
        proj_heads64(BIG, w, KS, stage_pool)
        w = load_wsec(wpool, win, 4)
        proj_tok(BIG, w, VS, b.pool([128, 512], BF16, 2, "vst"))
        b.pop()
        stop_at('oA2')
        b.push()
        for cc in range(4):
            b.dma('sync', BIG[:, cc, :], YR[cc * 128:(cc + 1) * 128, :])
        tge = b.sb([128, 128], F32, "tge")
        b.tt('vector', tge, csq[:, 128:256], csq[:, 0:128], ALU.add)
        triLTb = b.sb([128, 128], BF16, "triLTb")
        b.ts('vector', triLTb, tge, -1.0, ALU.mult, 1.0, ALU.add)
        mkS = b.sb([128, 4, 512], BF16, "mkS")
        b.dma('gpsimd', mkS, c_mask[4:8].re("n p q -> p n q"))
        q_pool = b.pool([64, T], BF16, 2, "qT")
        k_pool = b.pool([64, T], BF16, 2, "kT")
        v_pool = b.pool([128, NT, 64], BF16, 2, "vS")
        e_pool = b.pool([128, 512], F32, 2, "e")
        sp_pool = b.pool([128, 512], F32, 3, "sp")
        spb_pool = b.pool([128, 512], BF16, 4, "spb")
        arg_pool = b.pool([128, 512], F32, 3, "arg")
        p_pool = b.pool([128, 512], BF16, 3, "P")
        sfx_pool = b.pool([128, 512], F32, 2, "sfx")
        ost_pool = b.pool([64, 512], BF16, 2, "ost")
        VSv = VS.re("(n p) c -> p n c", p=128)
        blk = [0]
        for h in range(8):
            qT, kT, vv = q_pool(), k_pool(), v_pool()
            b.dma('sync', qT, QS[h])
            b.dma('sync', kT, KS[h])
            b.dma('sync', vv, VSv[:, :, h * 64:(h + 1) * 64])
            for Q in range(NCH):
                qs = slice(Q * 512, (Q + 1) * 512)
                blk[0] += 1
                OT = bank(6 + blk[0] % 2)[0:64, :]
                sfx = sfx_pool()
                Js = list(reversed(range(4 * Q + 4)))
                last = len(Js) - 1

                def s1(idx, J):
                    r = J - 4 * Q
                    zt = bank(idx % 2)
                    b.mm(zt, kT[:, J * 128:(J + 1) * 128], qT[:, qs])
                    e = e_pool()
                    b.act(e, zt, AF.Exp, scale=0.125)
                    spb = spb_pool()
                    b.act(spb, e, AF.Ln, bias=1.0)
                    if r >= 0:
                        b.tt('gpsimd', spb, spb, mkS[:, r, :], ALU.mult)
                    cw = bank(2 + idx % 2)
                    b.mm(cw, triLTb, spb)
                    bt = bank(4 + idx % 2)
                    b.mm(bt, onesb, spb)
                    return (r, zt, cw, bt)

                def s2(idx, J, ctx):
                    r, zt, cw, bt = ctx
                    if idx == 0:
                        b.cp('vector', sfx, bt)
                    else:
                        b.tt('vector', sfx, sfx, bt, ALU.add)
                    arg = arg_pool()
                    b.stt('vector', arg, zt, 0.125, sfx, ALU.mult, ALU.subtract)
                    b.tt('vector', arg, arg, cw, ALU.add)
                    P = p_pool()
                    b.act(P, arg, AF.Exp)
                    if r >= 0:
                        b.tt('gpsimd', P, P, mkS[:, r, :], ALU.mult)
                    b.mm(OT, vv[:, J, :], P, idx == 0, idx == last)

                ctx = s1(0, Js[0])
                for idx, J in enumerate(Js):
                    nxt = s1(idx + 1, Js[idx + 1]) if idx < last else None
                    s2(idx, J, ctx)
                    ctx = nxt
                if h % 2 == 0:
                    b.cp('vector', BIG[0:64, 4 + h // 2, qs], OT)
                else:
                    ost = ost_pool()
                    b.cp('vector', ost, OT)
                    b.dma('sync', BIG[64:128, 4 + h // 2, qs], ost)
        b.pop()
        stop_at('oB')
        b.push()
        outproj_epilogue(BIG, I['odd_w_out'][j], xsrc, xdst)
        b.pop()
        b.pop()

    def even_sublayer(l, xsrc, xdst):
        import math
        j = l // 2
        lam_init = 0.8 - 0.6 * math.exp(-0.3 * l)
        b.push()
        load_mod(l, 0)
        BIG = b.sb([128, 8, T], BF16, "BIGe")
        prologue(xsrc, 0, NT, BIG, 0)
        win = I['even_w_in'][j]
        QA = dscr("QA", [8, 64, T], BF16)
        KA = dscr("KA", [8, 64, T], BF16)
        VA = dscr("VA", [T, 512], BF16)
        QST = dscr("QST", [512, T], F32)
        KTs = dscr("KTs", [512, T], F32)
        SGT = dscr("SGT", [512, T], BF16)
        LF = dscr("LF", [T, 512], F32)
        KTOK = dscr("KTOK", [T, 512], F32)
        VH = dscr("VH", [T, 512], BF16)
        lbscr = dscr("lbscr", [2, 512], F32)
        gm = b.sb([1, 3, 512], F32, "gm")
        b.dma('sync', gm, I['hgrn_gamma'].re("(o a) c -> o a c", o=1))
        eg = b.sb([1, 3, 512], F32, "eg")
        b.act(eg, gm, AF.Exp)
        den = b.sb([1, 512], F32, "den")
        b.tt('vector', den, eg[:, 0, :], eg[:, 1, :], ALU.add)
        b.tt('vector', den, den, eg[:, 2, :], ALU.add)
        num = b.sb([1, 512], F32, "num")
        b.cp('vector', num, eg[:, 0, :])
        for i in range(1, l + 1):
            b.tt('vector', num, num, eg[:, i, :], ALU.add)
        rden = b.sb([1, 512], F32, "rden")
        b.recip(rden, den)
        lbr = b.sb([1, 2, 512], F32, "lbr")
        b.tt('vector', lbr[:, 0, :], num, rden, ALU.mult)
        b.ts('vector', lbr[:, 1, :], lbr[:, 0, :], -1.0, ALU.mult, 1.0, ALU.add)
        b.dma('sync', lbscr.re("(o a) c -> o a c", o=1), lbr)
        omlF = colvec(lbscr[1], 4)
        lbB = b.sb([128, 512], F32, "lbB")
        b.dma('sync', lbB, lbscr[0].pb())
        omlB = b.sb([128, 512], F32, "omlB")
        b.dma('sync', omlB, lbscr[1].pb())
        dl = b.sb([128, 256], F32, "dl")
        b.dma('sync', dl, I['diff_lambda'][j].re("a d -> (a d)").pb())
        lw = b.sb([128, 128], F32, "lw")
        lsm = b.sb([128, 8], F32, "lsm")
        b.tt('vector', lw[:, 0:64], dl[:, 0:64], dl[:, 64:128], ALU.mult)
        b.tt('vector', lw[:, 64:128], dl[:, 128:192], dl[:, 192:256], ALU.mult)
        b.red(lsm[:, 0:1], lw[:, 0:64], ALU.add)
        b.red(lsm[:, 1:2], lw[:, 64:128], ALU.add)
        b.act(lsm[:, 2:4], lsm[:, 0:2], AF.Exp)
        b.tt('vector', lsm[:, 4:5], lsm[:, 3:4], lsm[:, 2:3], ALU.subtract)
        b.ts('vector', lsm[:, 5:6], lsm[:, 4:5], -lam_init, ALU.add)
        neglam = lsm[:, 5:6]
        gv = b.sb([128, 4], F32, "gv")
        b.dma('sync', gv[:, 0:1], I['diff_gain'][j].re("(p o) -> p o", o=1), slow=True)
        b.dma('sync', gv[:, 1:2], I['hgrn_gain'][j].re("(p o) -> p o", o=1), slow=True)
        b.ts('vector', gv[:, 2:3], gv[:, 0:1], math.sqrt(128.0) * (1.0 - lam_init), ALU.mult)
        b.ts('vector', gv[:, 3:4], gv[:, 1:2], math.sqrt(128.0), ALU.mult)
        dgS, hgS = gv[:, 2:3], gv[:, 3:4]
        stop_at('e0')
        b.push()
        wpool = b.pool([128, 8, 512], BF16, 2, "wsec")
        stage_pool = b.pool([64, T], BF16, 2, "stg")
        w = load_wsec(wpool, win, 0)
        proj_heads64(BIG, w, QA, stage_pool)
        w = load_wsec(wpool, win, 1)
        proj_heads64(BIG, w, KA, stage_pool)
        st_pool = b.pool([128, 512], BF16, 2, "vst")
        w = load_wsec(wpool, win, 2)
        proj_tok(BIG, w, VA, st_pool)
        w = load_wsec(wpool, win, 5)
        proj_tok(BIG, w, VH, st_pool)
        fst_pool = b.pool([128, 512], F32, 3, "fst")
        bst_pool = b.pool([128, 512], BF16, 2, "bst")
        w = load_wsec(wpool, win, 3)
        for h in range(4):
            for ch in range(NCH):
                pq = bank(ch % 2)
                for kc in range(8):
                    b.mm(pq, w[:, kc, h * 128:(h + 1) * 128], BIG[:, kc, ch * 512:(ch + 1) * 512], kc == 0, kc == 7)
                st = fst_pool()
                b.act(st, pq, AF.Silu)
                b.dma('sync', QST[h * 128:(h + 1) * 128, ch * 512:(ch + 1) * 512], st)
        w = load_wsec(wpool, win, 6)
        for h in range(4):
            for ch in range(NCH):
                pq = bank(ch % 2)
                for kc in range(8):
                    b.mm(pq, w[:, kc, h * 128:(h + 1) * 128], BIG[:, kc, ch * 512:(ch + 1) * 512], kc == 0, kc == 7)
                st = bst_pool()
                b.act(st, pq, AF.Silu)
                b.dma('sync', SGT[h * 128:(h + 1) * 128, ch * 512:(ch + 1) * 512], st)
        w = load_wsec(wpool, win, 4)
        for h in range(4):
            for ch in range(NCH):
                pq = bank(ch % 2)
                for kc in range(8):
                    b.mm(pq, w[:, kc, h * 128:(h + 1) * 128], BIG[:, kc, ch * 512:(ch + 1) * 512], kc == 0, kc == 7)
                sg = fst_pool()
                b.act(sg, pq, AF.Sigmoid, scale=-1.0)
                st = fst_pool()
                b.ts('vector', st, sg, omlF[:, h:h + 1], ALU.mult)
                b.dma('sync', KTs[h * 128:(h + 1) * 128, ch * 512:(ch + 1) * 512], st)
        for ti in range(NT):
            pv = bank(2 + ti % 2)
            for kc in range(8):
                b.mm(pv, BIG[:, kc, ti * 128:(ti + 1) * 128], w[:, kc, :], kc == 0, kc == 7)
            sg = fst_pool()
            b.act(sg, pv, AF.Sigmoid)
            tq = fst_pool()
            b.tt('vector', tq, sg, omlB, ALU.mult)
            b.tt('gpsimd', sg, tq, lbB, ALU.add)
            lf = fst_pool()
            b.act(lf, sg, AF.Ln)
            b.dma('sync', LF[ti * 128:(ti + 1) * 128, :], lf)
            b.tt('gpsimd', sg, omlB, tq, ALU.subtract)
            b.dma('sync', KTOK[ti * 128:(ti + 1) * 128, :], sg)
        b.pop()
        stop_at('eA')
        b.push()
        mkI = b.sb([128, 4, 512], BF16, "mkI")
        b.dma('gpsimd', mkI, c_mask[0:4].re("n p q -> p n q"))
        qT = b.sb([68, 2, T], BF16, "qTa")
        kT = b.sb([68, 2, T], BF16, "kTa")
        vv = b.sb([128, NT, 128], BF16, "vA")
        qTg = V(qT.ap, Res("qaug"))
        kTg = V(kT.ap, Res("kaug"))
        p_pool = b.pool([128, 512], BF16, 4, "Pa")
        rl_pool = b.pool([128, 512], F32, 4, "rl")
        oc_pool = b.pool([128, 512], F32, 4, "oc")
        VAv = VA.re("(n p) c -> p n c", p=128)
        blk = [0]
        for h in range(4):
            for c in range(2):
                b.dma('sync', qT[0:64, c, :], QA[2 * h + c])
                b.dma('sync', kT[0:64, c, :], KA[2 * h + c])
                b.dma('gpsimd', qTg[64:68, c, :], c_aug[h, 0])
                b.dma('gpsimd', kTg[64:68, c, :], c_aug[h, 1])
            b.dma('sync', vv, VAv[:, :, h * 128:(h + 1) * 128])
            for Q in range(NCH):
                qs = slice(Q * 512, (Q + 1) * 512)
                oc = []
                for c in range(2):
                    blk[0] += 1
                    OT, LT = bank(2 + blk[0] % 2), bank(4 + blk[0] % 2)
                    nJ = 4 * Q + 4

                    def s1(J):
                        r = J - 4 * Q
                        st = bank(J % 2)
                        b.mm(st, kT[0:68, c, J * 128:(J + 1) * 128], qT[0:68, c, qs], xr=(qTg, kTg))
                        P = p_pool()
                        if r >= 0:
                            cl = rl_pool()
                            b.ts('vector', cl, st, 0.125, ALU.mult, 80.0, ALU.min)
                            b.act(P, cl, AF.Exp)
                            b.tt('gpsimd', P, P, mkI[:, r, :], ALU.mult)
                        else:
                            b.act(P, st, AF.Exp, scale=0.125)
                        return P

                    P = s1(0)
                    for J in range(nJ):
                        Pn = s1(J + 1) if J + 1 < nJ else None
                        b.mm(OT, vv[:, J, :], P, J == 0, J == nJ - 1)
                        b.mm(LT, onesb, P, J == 0, J == nJ - 1)
                        P = Pn
                    rl = rl_pool()
                    b.recip(rl, LT)
                    o_c = oc_pool()
                    b.tt('vector', o_c, OT, rl, ALU.mult)
                    oc.append(o_c)
                o = oc[0]
                b.stt('vector', o, oc[1], neglam, oc[0], ALU.mult, ALU.add)
                sq = oc[1]
                b.tt('gpsimd', sq, o, o, ALU.mult)
                ss = bank(6)
                b.mm(ss, onesf, sq)
                rstd = rl_pool()
                b.rsqrt(rstd, ss, 128e-6, rstd)
                b.stt('vector', BIG[:, h, qs], o, dgS, rstd, ALU.mult, ALU.mult)
        b.pop()
        stop_at('eB')
        b.push()
        hgGT = csq[:, 256:512]
        hgU = csq[:, 512:640]
        tri4 = b.sb([128, 4, 128], F32, "tri4")
        for h in range(4):
            b.cp('gpsimd', tri4[:, h, :], csq[:, 384:512])
        S0 = [b.sb([128, 128], F32, "S0") for _ in range(4)]
        S1 = [b.sb([128, 128], F32, "S1") for _ in range(4)]
        Sb0 = [b.sb([128, 128], BF16, "Sb0") for _ in range(4)]
        Sb1 = [b.sb([128, 128], BF16, "Sb1") for _ in range(4)]
        for h in range(4):
            b.memset('vector', S0[h], 0.0)
            b.memset('gpsimd', Sb0[h], 0.0)
        lf_pool = b.pool([128, 512], F32, 2, "LFt")
        kt_pool = b.pool([128, 512], F32, 2, "Kt")
        vt_pool = b.pool([128, 512], BF16, 2, "Vt")
        qs_pool = b.pool([128, 4, 128], F32, 2, "QsT")
        ktt_pool = b.pool([128, 4, 128], F32, 2, "KTt")
        sg_pool = b.pool([128, 4, 128], BF16, 2, "SGt")
        E_pool = [b.pool([128, 4, 128], F32, 2, "E%d" % i) for i in range(3)]
        E3_pool = b.pool([128, 512], F32, 2, "E3")
        bf_pool = [b.pool([128, 4, 128], BF16, 2, "hb%d" % i) for i in range(3)]
        kd_pool = b.pool([128, 512], BF16, 4, "kd")
        at_pool = b.pool([128, 512], BF16, 2, "attm")
        sq_pool = b.pool([128, 512], F32, 2, "hsq")
        QSTv = QST.re("(h d) t -> d h t", d=128)
        KTv = KTs.re("(h d) t -> d h t", d=128)
        SGTv = SGT.re("(h d) t -> d h t", d=128)
        for ti in range(NT):
            ts_ = slice(ti * 128, (ti + 1) * 128)
            LFt, Kt, Vt = lf_pool(), kt_pool(), vt_pool()
            QsT, KTt, SGt = qs_pool(), ktt_pool(), sg_pool()
            b.dma('sync', LFt, LF[ts_, :])
            b.dma('sync', Kt, KTOK[ts_, :])
            b.dma('sync', Vt, VH[ts_, :])
            b.dma('sync', QsT, QSTv[:, :, ts_])
            b.dma('sync', KTt, KTv[:, :, ts_])
            b.dma('sync', SGt, SGTv[:, :, ts_])
            for h in range(4):
                b.mm(bank(h // 2)[:, (h % 2) * 256:(h % 2 + 1) * 256], LFt[:, h * 128:(h + 1) * 128], hgGT)
            pbu = bank(2)
            b.mm(pbu, hgU, LFt)
            E1, E1i, E2 = E_pool[0](), E_pool[1](), E_pool[2]()
            for hf2 in range(2):
                pv = bank(hf2).re("p (h a c) -> p h a c", h=2, a=2)
                hs = slice(2 * hf2, 2 * hf2 + 2)
                b.act(E1[:, hs, :], pv[:, :, 0, :], AF.Exp)
                b.act(E1i[:, hs, :], pv[:, :, 0, :], AF.Exp, scale=-1.0)
                b.act(E2[:, hs, :], pv[:, :, 1, :], AF.Exp)
            stop_at('c0')
            E3 = E3_pool()
            b.act(E3, pbu, AF.Exp)
            qbT, kbT, qeT = bf_pool[0](), bf_pool[1](), bf_pool[2]()
            b.tt('vector', qbT, QsT, E1, ALU.mult)
            b.tt('gpsimd', kbT, KTt, E1i, ALU.mult)
            b.tt('vector', qeT, QsT, E2, ALU.mult)
            kdA, kdB = kd_pool(), kd_pool()
            b.stt('vector', kdA, Kt, csq[:, 447:448], E3, ALU.mult, ALU.mult)
            b.stt('vector', kdB, Kt, csq[:, 511:512], E3, ALU.mult, ALU.mult)
            stop_at('c1')
            patt = bank(3)
            for h in range(4):
                b.mm(patt[:, h * 128:(h + 1) * 128], kbT[:, h, :], qbT[:, h, :])
            attm = at_pool()
            b.tt('vector', attm, patt, tri4.re("p h c -> p (h c)"), ALU.mult)
            for h in range(4):
                for chk in range(2):
                    reg = bank(4 + h // 2)[:, ((h % 2) * 2 + chk) * 128:((h % 2) * 2 + chk + 1) * 128]
                    b.mm(reg, (kdA, kdB)[chk][:, h * 128:(h + 1) * 128], Vt[:, h * 128:(h + 1) * 128])
            stop_at('c2')
            po = bank(6)
            for h in range(4):
                hc = slice(h * 128, (h + 1) * 128)
                ca = slice(h * 128, h * 128 + 64)
                cb_ = slice(h * 128 + 64, h * 128 + 128)
                b.mm(po[:, ca], Vt[:, hc], attm[:, ca], True, False)
                b.mm(po[:, ca], Sb0[h], qeT[:, h, 0:64], False, True)
                dSa = bank(4 + h // 2)[:, ((h % 2) * 2) * 128:((h % 2) * 2 + 1) * 128]
                dSb = bank(4 + h // 2)[:, ((h % 2) * 2 + 1) * 128:((h % 2) * 2 + 2) * 128]
                b.stt('vector', S1[h], S0[h], E2[:, h, 63:64], dSa, ALU.mult, ALU.add)
                b.cp('gpsimd', Sb1[h], S1[h])
                b.mm(po[:, cb_], Vt[:, hc], attm[:, cb_], True, False)
                b.mm(po[:, cb_], Sb1[h], qeT[:, h, 64:128], False, True)
                b.stt('vector', S0[h], S1[h], E2[:, h, 127:128], dSb, ALU.mult, ALU.add)
                b.cp('gpsimd', Sb0[h], S0[h])
            stop_at('c3')
            sq = sq_pool()
            b.act(sq, po, AF.Square)
            pss = bank(7)
            b.mm(pss, onesf, sq)
            rstd = sq_pool()
            b.rsqrt(rstd, pss, 128e-6, rstd)
            b.stt('vector', sq, po, hgS, rstd, ALU.mult, ALU.mult)
            b.tt('gpsimd', BIG[:, 4:8, ts_], sq.re("p (h c) -> p h c", h=4), SGt, ALU.mult)
        b.pop()
        b.push()
        outproj_epilogue(BIG, I['even_w_out'][j], xsrc, xdst)
        b.pop()
        b.pop()

    cur = x_in
    for si, name in enumerate(plan):
        dst = out if si == len(plan) - 1 else xs[si % 2]
        l = int(name[-1])
        try:
            if name.startswith('moe'):
                moe_sublayer(l, cur, dst)
            elif name.startswith('even'):
                even_sublayer(l, cur, dst)
            elif name.startswith('odd'):
                odd_sublayer(l, cur, dst)
        except _Stop:
            break
        cur = dst
    b.P.barrier()
    b.P.emit()
    return nc, b


def prep_inputs(inputs, T):
    f = lambda a: np.ascontiguousarray(np.asarray(a, dtype=np.float32))
    shared = {}
    for k in IN_SHAPES:
        a = f(inputs[k])
        if k in ('lru_wa', 'lru_wx'):
            bd = np.zeros((4, 128, 128), np.float32)
            for g in range(8):
                bd[g // 2, (g % 2) * 64:(g % 2 + 1) * 64, (g % 2) * 64:(g % 2 + 1) * 64] = a[0, g]
            a = bd
        elif k == 'router_b':
            a = a.reshape(1, 16)
        shared[k] = a
    shared.update(host_consts(T))
    return shared


def kernel(**inputs):
    x = np.asarray(inputs['x'], dtype=np.float32)
    c = np.asarray(inputs['c'], dtype=np.float32)
    Bn, T, _ = x.shape
    shared = prep_inputs(inputs, T)
    nc, _ = build(T, ['even0', 'moe0', 'odd1', 'moe1'])
    in_maps = []
    for i in range(Bn):
        m = dict(shared)
        m['x'] = np.ascontiguousarray(x[i])
        m['c'] = np.ascontiguousarray(c[i:i + 1])
        in_maps.append(m)
    res = run_bass_kernel_spmd(nc, in_maps, core_ids=list(range(Bn)))
    return np.stack([np.asarray(r['out'], dtype=np.float32) for r in res.results], axis=0)
```

```python
import contextlib
import numpy as np
import concourse.bass as bass
import concourse.mybir as mybir
from concourse.bass_utils import run_bass_kernel_spmd

F32 = mybir.dt.float32
BF16 = mybir.dt.bfloat16
AF = mybir.ActivationFunctionType
ALU = mybir.AluOpType
AX = mybir.AxisListType

ENGS = ['tensor', 'vector', 'scalar', 'gpsimd', 'sync']
SEM_LIMIT = 30000
D = 1024
ALPHA = 4.0 ** 0.25
N_CORES = 8


class Res:
    __slots__ = ('name', 'last_w', 'readers', 'ds')

    def __init__(self, name):
        self.name = name
        self.last_w = None
        self.readers = []
        self.ds = {}


class DSem:
    __slots__ = ('name', 'dcount', 'dsem', 'kind')

    def __init__(self, name, kind):
        self.name = name
        self.dcount = 0
        self.dsem = None
        self.kind = kind


class Prog:
    def __init__(self, nc):
        self.nc = nc
        self.ops = {e: [] for e in ENGS}
        self.seen = {x: {e: -1 for e in ENGS} for x in ENGS}
        self.seen_d = {x: {} for x in ENGS}
        self.dma_res = []
        self.free_ds = {'sw': [], 'hw': []}
        self.all_dma_toks = []
        self.stack = contextlib.ExitStack()
        self.pending = {x: [] for x in ENGS}
        self.scopes = []

    def sbuf(self, name, shape, dtype):
        st = self.scopes[-1] if self.scopes else self.stack
        return st.enter_context(self.nc.sbuf_tensor(name, list(shape), dtype))

    def push(self):
        self.scopes.append(contextlib.ExitStack())

    def pop(self):
        self.scopes.pop().close()

    def psum(self, name, shape, dtype):
        return self.stack.enter_context(self.nc.psum_tensor(name, list(shape), dtype))

    def _need(self, eng, tok, waits):
        if tok is None:
            return
        if tok[0] == 'e':
            _, e, idx = tok
            if e == eng and eng == 'tensor':
                return
            if self.seen[eng][e] >= idx:
                return
            self.seen[eng][e] = idx
            waits.append(tok)
        else:
            _, r, val = tok
            if self.seen_d[eng].get(id(r), 0) >= val:
                return
            self.seen_d[eng][id(r)] = val
            waits.append(tok)

    def op(self, eng, fn, reads=(), writes=(), dma_res=None):
        waits = []
        for r in reads:
            self._need(eng, r.last_w, waits)
        for r in writes:
            self._need(eng, r.last_w, waits)
            for t in r.readers:
                self._need(eng, t, waits)
        for t in self.pending[eng]:
            self._need(eng, t, waits)
        self.pending[eng] = []
        idx = len(self.ops[eng])
        if dma_res is not None:
            kind = 'sw' if eng == 'gpsimd' else 'hw'
            ds = dma_res.ds.get(kind)
            if ds is None:
                fl = self.free_ds[kind]
                if fl:
                    ds = fl.pop()
                else:
                    ds = DSem("%s%d" % (kind, len(self.dma_res)), kind)
                    self.dma_res.append(ds)
                dma_res.ds[kind] = ds
            dma_res = ds
            ds.dcount += 16
            tok = ('d', ds, ds.dcount)
            self.all_dma_toks.append(tok)
        else:
            tok = ('e', eng, idx)
        import sys
        fr = sys._getframe(2)
        site = "%s:%d<%s:%d" % (fr.f_code.co_name, fr.f_lineno, fr.f_back.f_code.co_name, fr.f_back.f_lineno)
        self.ops[eng].append(dict(fn=fn, waits=waits, dma_res=dma_res, site=site,
                                  dval=(dma_res.dcount if dma_res else 0), mile=False))
        for r in writes:
            r.last_w = tok
            r.readers = []
        for r in reads:
            if r not in writes:
                r.readers.append(tok)
        return tok

    def barrier(self):
        toks = []
        for e in ENGS:
            for idx in range(len(self.ops[e]) - 1, -1, -1):
                if self.ops[e][idx]['dma_res'] is None:
                    toks.append(('e', e, idx))
                    break
        dtoks = {}
        for t in self.all_dma_toks:
            dtoks[id(t[1])] = t
        self.all_dma_toks = []
        for x in ENGS:
            self.pending[x].extend(list(toks) + list(dtoks.values()))

    def emit(self):
        nc = self.nc
        self.tail = {}
        for x in ENGS:
            w = []
            for t in self.pending[x]:
                self._need(x, t, w)
            self.tail[x] = w
            for t in w:
                if t[0] == 'e':
                    self.ops[t[1]][t[2]]['mile'] = True
        for x in ENGS:
            for o in self.ops[x]:
                for t in o['waits']:
                    if t[0] == 'e':
                        self.ops[t[1]][t[2]]['mile'] = True
        nsem = 0
        for x in ENGS:
            cnt = 0
            cur = None
            for o in self.ops[x]:
                if o['mile'] and o['dma_res'] is None:
                    if cur is None or cnt >= SEM_LIMIT:
                        cur = self.stack.enter_context(nc.semaphore(f"m_{x}_{nsem}"))
                        nsem += 1
                        cnt = 0
                    cnt += 1
                    o['msem'] = (cur, cnt)
        per16 = (SEM_LIMIT // 16) * 16
        for r in self.dma_res:
            n = r.dcount
            r.dsem = []
            for i in range((n + per16 - 1) // per16):
                r.dsem.append(self.stack.enter_context(nc.semaphore(f"d_{r.name}_{i}")))
                nsem += 1
        self.nsem = nsem

        def dsem_of(r, val):
            k = (val - 16) // per16
            return r.dsem[k], val - k * per16

        ops = self.ops
        self.imap = {}
        ctr = [0]

        def body(x):
            def f(eng):
                for o in ops[x]:
                    self.imap[ctr[0] + len(o['waits'])] = (x, o['site'])
                    ctr[0] += len(o['waits']) + 1
                    for t in o['waits']:
                        if t[0] == 'e':
                            s, v = ops[t[1]][t[2]]['msem']
                        else:
                            s, v = dsem_of(t[1], t[2])
                        eng.wait_ge(s, v)
                    inst = o['fn'](eng)
                    if o['dma_res'] is not None:
                        s, v = dsem_of(o['dma_res'], o['dval'])
                        inst.then_inc(s, 16)
                    elif o['mile']:
                        s, v = o['msem']
                        inst.then_inc(s, 1)
                for t in self.tail[x]:
                    if t[0] == 'e':
                        s, v = ops[t[1]][t[2]]['msem']
                    else:
                        s, v = dsem_of(t[1], t[2])
                    eng.wait_ge(s, v)
            return f

        with nc.Block() as block:
            block.tensor(body('tensor'))
            block.vector(body('vector'))
            block.scalar(body('scalar'))
            block.gpsimd(body('gpsimd'))
            block.sync(body('sync'))
        self.stack.close()


class V:
    __slots__ = ('ap', 'r')

    def __init__(self, ap, r):
        self.ap = ap
        self.r = r

    def __getitem__(self, k):
        return V(self.ap[k], self.r)

    def re(self, s, **kw):
        return V(self.ap.rearrange(s, **kw), self.r)

    def pb(self, n=128):
        return V(self.ap.partition_broadcast(n), self.r)


def _rs(xs):
    out = []
    for x in xs:
        if isinstance(x, V) and x.r not in out:
            out.append(x.r)
    return out


def _a(x):
    return x.ap if isinstance(x, V) else x


class Bld:
    ARENA_WORDS = 52992

    def __init__(self, nc):
        self.nc = nc
        self.P = Prog(nc)
        self.n = 0
        self.arena = self.P.stack.enter_context(nc.sbuf_tensor("arena", [128, self.ARENA_WORDS], F32))
        self.off = 0
        self.marks = []
        self.peak = 0
        self.scope_res = [[]]

    def push(self):
        self.marks.append(self.off)
        self.scope_res.append([])

    def pop(self):
        self.off = self.marks.pop()
        self.P.barrier()
        for r in self.scope_res.pop():
            for kind, ds in r.ds.items():
                self.P.free_ds[kind].append(ds)
            r.ds = {}

    def sb(self, shape, dt, name=None):
        self.n += 1
        name = f"{name or 't'}_{self.n}"
        nel = int(np.prod(shape[1:]))
        nbytes = nel * (2 if dt == BF16 else 4)
        words = (nbytes + 63) // 64 * 16
        assert self.off + words <= self.ARENA_WORDS, f"SBUF arena overflow at {name}: {self.off * 4} + {words * 4}"
        ap = self.arena[0:shape[0], self.off:self.off + (nbytes + 3) // 4]
        self.off += words
        self.peak = max(self.peak, self.off * 4)
        if dt == BF16:
            ap = ap.bitcast(BF16)
            if nel % 2:
                ap = ap[:, 0:nel]
        if len(shape) == 3:
            ap = ap.rearrange("p (a b) -> p a b", a=shape[1])
        elif len(shape) == 4:
            ap = ap.rearrange("p (a b c) -> p a b c", a=shape[1], b=shape[2])
        r = Res(name)
        self.scope_res[-1].append(r)
        return V(ap, r)

    def pool(self, shape, dt, n, name=None):
        tiles = [self.sb(shape, dt, name) for _ in range(n)]
        st = [0]

        def nxt():
            st[0] += 1
            return tiles[(st[0] - 1) % n]
        return nxt

    def act(self, out, in_, func, bias=None, scale=None):
        kw = {}
        if bias is not None:
            kw['bias'] = _a(bias)
        if scale is not None:
            kw['scale'] = _a(scale)
        o, i = out.ap, in_.ap
        self.P.op('scalar', lambda e: e.activation(out=o, in_=i, func=func, **kw),
                  reads=_rs([in_, bias, scale]), writes=[out.r])

    def tt(self, eng, out, a, b, op):
        o, x, y = out.ap, a.ap, b.ap
        self.P.op(eng, lambda e: e.tensor_tensor(out=o, in0=x, in1=y, op=op), reads=_rs([a, b]), writes=[out.r])

    def ts(self, eng, out, a, s1, op0, s2=None, op1=None):
        o, x, p, q = out.ap, a.ap, _a(s1), _a(s2)
        if s2 is None:
            fn = lambda e: e.tensor_scalar(out=o, in0=x, scalar1=p, scalar2=None, op0=op0)
        else:
            fn = lambda e: e.tensor_scalar(out=o, in0=x, scalar1=p, scalar2=q, op0=op0, op1=op1)
        self.P.op(eng, fn, reads=_rs([a, s1, s2]), writes=[out.r])

    def stt(self, eng, out, a, s, b, op0, op1, xr=()):
        o, x, p, y = out.ap, a.ap, _a(s), b.ap
        eng = 'vector'
        self.P.op(eng, lambda e: e.scalar_tensor_tensor(out=o, in0=x, scalar=p, in1=y, op0=op0, op1=op1),
                  reads=_rs([a, s, b] + list(xr)), writes=[out.r])

    def rsqrt(self, out, a, eps, tmp, scale=1.0):
        self.act(tmp, a, AF.Ln, bias=eps, scale=scale)
        self.act(out, tmp, AF.Exp, scale=-0.5)

    def cp(self, eng, out, a):
        o, x = out.ap, a.ap
        if eng == 'scalar':
            self.P.op(eng, lambda e: e.copy(out=o, in_=x), reads=[a.r], writes=[out.r])
        else:
            self.P.op(eng, lambda e: e.tensor_copy(out=o, in_=x), reads=[a.r], writes=[out.r])

    def red(self, out, a, op):
        o, x = out.ap, a.ap
        self.P.op('vector', lambda e: e.tensor_reduce(out=o, in_=x, axis=AX.X, op=op), reads=[a.r], writes=[out.r])

    def recip(self, out, a):
        o, x = out.ap, a.ap
        self.P.op('vector', lambda e: e.reciprocal(out=o, in_=x), reads=[a.r], writes=[out.r])

    def scan(self, out, d0, d1, init):
        o, x, y, z = out.ap, d0.ap, d1.ap, _a(init)
        self.P.op('vector', lambda e: e.tensor_tensor_scan(out=o, data0=x, data1=y, initial=z, op0=ALU.mult,
                                                          op1=ALU.add), reads=_rs([d0, d1, init]), writes=[out.r])

    def memset(self, eng, out, val):
        o = out.ap
        self.P.op(eng, lambda e: e.memset(o, val), writes=[out.r])

    def mm(self, out, lhsT, rhs, start=True, stop=True, xr=()):
        o, l, r = out.ap, lhsT.ap, rhs.ap
        self.P.op('tensor', lambda e: e.matmul(o, l, r, start=start, stop=stop),
                  reads=_rs([lhsT, rhs] + list(xr)), writes=[out.r])

    def tr(self, out, in_, ident):
        o, i, d = out.ap, in_.ap, ident.ap
        self.P.op('tensor', lambda e: e.transpose(o, i, d), reads=_rs([in_, ident]), writes=[out.r])

    def dma(self, q, out, in_, slow=False):
        o, i = out.ap, in_.ap
        kw = dict(allow_slow_non_contiguous=True) if slow else {}
        self.P.op(q, lambda e: e.dma_start(out=o, in_=i, **kw), reads=[in_.r], writes=[out.r], dma_res=out.r)


def host_consts(T):
    p = np.arange(128)
    sq = np.zeros((5, 128, 128), np.float32)
    sq[0] = np.eye(128)
    sq[1] = (p[:, None] > p[None, :])
    same = (p[:, None] // 64) == (p[None, :] // 64)
    mid = (p // 64) * 64 + 31
    s_, c_ = p[:, None], p[None, :]
    sq[2] = (same & (s_ > mid[None, :]) & (s_ <= c_)) * 1.0 - (same & (s_ > c_) & (s_ <= mid[None, :])) * 1.0
    sq[3] = same & (s_ <= c_)
    sq[4] = same & (s_ > c_)
    csq = np.ascontiguousarray(sq.transpose(1, 0, 2).reshape(128, 640))
    i = np.arange(512)
    mk = np.zeros((8, 128, 512), np.float32)
    for r in range(4):
        mk[r] = (128 * r + p[:, None]) <= i[None, :]
        mk[4 + r] = (128 * r + p[:, None]) < i[None, :]
    t = np.arange(T)
    aug = np.zeros((4, 2, 4, T), np.float32)
    for h in range(4):
        sl = 2.0 ** (-8.0 * (h + 1) / 4)
        aug[h, 0, 0] = -8 * sl * 64 * (t // 64)
        aug[h, 0, 1] = -8 * sl * (t % 64)
        aug[h, 0, 2] = 1
        aug[h, 0, 3] = 1
        aug[h, 1, 0] = 1
        aug[h, 1, 1] = 1
        aug[h, 1, 2] = 8 * sl * 64 * (t // 64)
        aug[h, 1, 3] = 8 * sl * (t % 64)
    return dict(c_sq=csq, c_mask=mk, c_aug=aug)


IN_SHAPES = dict(
    ada_w=[2, 1024, 6144], ada_b=[2, 6144], ln_g=[2, 2, 1024], ln_b=[2, 2, 1024],
    even_w_in=[1, 1024, 3584], even_w_out=[1, 1024, 1024], diff_lambda=[1, 4, 64], diff_gain=[1, 128],
    hgrn_gamma=[3, 512], hgrn_gain=[1, 128], odd_w_in=[1, 1024, 2560], odd_w_out=[1, 1024, 1024],
    conv_w=[1, 4, 512], conv_b=[1, 512], lru_wa=[4, 128, 128], lru_ba=[1, 512], lru_wx=[4, 128, 128],
    lru_bx=[1, 512], lru_lambda=[1, 512], router_w=[1024, 16], router_b=[1, 16],
    moe_w_gate=[2, 16, 1024, 512], moe_w_up=[2, 16, 1024, 512], moe_w_down=[2, 16, 512, 1024],
)


class _Stop(Exception):
    pass


STOP = None


def build(T, plan):
    nc = bass.Bass("TRN2", target_bir_lowering=False)
    b = Bld(nc)
    NT, NCH = T // 128, T // 512

    def din(name, shape, dt=F32):
        return V(nc.dram_tensor(name, list(shape), dt, kind="ExternalInput").ap(), Res(name))

    def dscr(name, shape, dt):
        return V(nc.dram_tensor(name, list(shape), dt).ap(), Res(name))

    I = {k: din(k, s) for k, s in IN_SHAPES.items()}
    x_in = din("x", [T, D])
    c_in = din("c", [1, D])
    c_sq = din("c_sq", [128, 640])
    c_mask = din("c_mask", [8, 128, 512])
    c_aug = din("c_aug", [4, 2, 4, T])
    out = V(nc.dram_tensor("out", [T, D], F32, kind="ExternalOutput").ap(), Res("out"))
    xs = [dscr("xs0", [T, D], F32), dscr("xs1", [T, D], F32)]
    modscr = dscr("modscr", [2, 6144], F32)

    PS = []
    for i in range(4):
        t = b.P.psum(f"ps{i}", [128, 1024], F32)
        PS.append([V(t[:, 0:512], Res(f"ps{i}a")), V(t[:, 512:1024], Res(f"ps{i}b")), t])

    def bank(i):
        return PS[i // 2][i % 2]

    csq = b.sb([128, 640], F32, "csq")
    b.dma('sync', csq, c_sq)
    ident = csq[:, 0:128]
    onesf = b.sb([128, 128], F32, "onesf")
    b.memset('vector', onesf, 1.0)
    onesb = b.sb([128, 128], BF16, "onesb")
    b.memset('vector', onesb, 1.0)

    b.push()
    cT = b.sb([128, 8], F32, "cT")
    b.dma('sync', cT, c_in.re("o (k p) -> p (o k)", p=128), slow=True)
    cTs = b.sb([128, 8], F32, "cTs")
    b.act(cTs, cT, AF.Silu)
    cTb = b.sb([128, 8], BF16, "cTb")
    b.cp('vector', cTb, cTs)
    wsec_pool = b.pool([128, 8, 1024], BF16, 2, "wsec")
    layers_needed = sorted({int(p[-1]) for p in plan})
    for l in layers_needed:
        modrow = b.sb([1, 6144], F32, "modrow")
        adab = b.sb([1, 6144], F32, "adab")
        b.dma('sync', adab, I['ada_b'][l:l + 1, :])
        for sec in range(6):
            ws = wsec_pool()
            b.dma('gpsimd', ws, I['ada_w'][l][:, sec * 1024:(sec + 1) * 1024].re("(k p) n -> p k n", p=128))
            for nh in range(2):
                ps = bank(nh)
                for kc in range(8):
                    b.mm(ps[0:1, :], cTb[:, kc:kc + 1], ws[:, kc, nh * 512:(nh + 1) * 512], kc == 0, kc == 7)
                c0 = sec * 1024 + nh * 512
                b.tt('vector', modrow[0:1, c0:c0 + 512], ps[0:1, :], adab[0:1, c0:c0 + 512], ALU.add)
            if sec in (1, 2, 4, 5):
                b.ts('vector', modrow[0:1, sec * 1024:(sec + 1) * 1024], modrow[0:1, sec * 1024:(sec + 1) * 1024],
                     1.0, ALU.add)
        b.dma('sync', modscr[l:l + 1, :], modrow)
    b.pop()

    xt_pool = b.pool([128, D], F32, 2, "xt")
    hf_pool = b.pool([128, 8, 128], F32, 2, "hf")
    scp = b.sb([128, 8], F32, "scp")
    shv = b.sb([128, 8], F32, "shv")
    gB = b.sb([128, D], F32, "gB")
    lnG = b.sb([128, D], F32, "lnG")
    lnB = b.sb([128, D], F32, "lnB")
    ework = [b.pool([128, D], F32, 2, "ew%d" % i) for i in range(2)]
    small = b.pool([128, 8], F32, 4, "small")

    def load_mod(l, s):
        sec_sh, sec_sc, sec_g = (0, 1, 2) if s == 0 else (3, 4, 5)
        b.dma('sync', shv, modscr[l, sec_sh * 1024:(sec_sh + 1) * 1024].re("(k p) -> p k", p=128), slow=True)
        b.dma('sync', scp, modscr[l, sec_sc * 1024:(sec_sc + 1) * 1024].re("(k p) -> p k", p=128), slow=True)
        b.dma('sync', gB, modscr[l, sec_g * 1024:(sec_g + 1) * 1024].pb())
        b.dma('sync', lnG, I['ln_g'][l, s, :].pb())
        b.dma('sync', lnB, I['ln_b'][l, s, :].pb())

    def prologue(xsrc, tok0, ntiles, hT, col0, router=None):
        for i in range(ntiles):
            t0 = tok0 + i * 128
            xt = xt_pool()
            b.dma('sync', xt, xsrc[t0:t0 + 128, :])
            pa, pb_ = PS[3][0], PS[3][1]
            for kc in range(8):
                pk = (pa if kc < 4 else pb_)[:, (kc % 4) * 128:(kc % 4 + 1) * 128]
                b.tr(pk, xt[:, kc * 128:(kc + 1) * 128], ident)
            hf = hf_pool()
            for kc in range(8):
                pk = (pa if kc < 4 else pb_)[:, (kc % 4) * 128:(kc % 4 + 1) * 128]
                b.act(hf[:, kc, :], pk, AF.Identity, bias=shv[:, kc:kc + 1], scale=scp[:, kc:kc + 1])
            b.cp('vector', hT[:, :, col0 + i * 128:col0 + (i + 1) * 128], hf)
            if router is not None:
                router(hf, col0 + i * 128)

    def epilogue(xsrc, xdst, t0, y):
        xe = xt_pool()
        b.dma('sync', xe, xsrc[t0:t0 + 128, :])
        t1 = ework[0]()
        if isinstance(y, tuple):
            b.tt('vector', t1[:, 0:512], y[0], gB[:, 0:512], ALU.mult)
            b.tt('vector', t1[:, 512:1024], y[1], gB[:, 512:1024], ALU.mult)
        else:
            b.tt('vector', t1, y, gB, ALU.mult)
        r = ework[1]()
        b.stt('gpsimd', r, xe, ALPHA, t1, ALU.mult, ALU.add)
        b.tt('gpsimd', t1, r, r, ALU.mult)
        st = small()
        b.red(st[:, 0:1], r, ALU.add)
        b.red(st[:, 1:2], t1, ALU.add)
        b.ts('vector', st[:, 2:4], st[:, 0:2], 1.0 / D, ALU.mult)
        b.tt('vector', st[:, 4:5], st[:, 2:3], st[:, 2:3], ALU.mult)
        b.tt('vector', st[:, 5:6], st[:, 3:4], st[:, 4:5], ALU.subtract)
        b.rsqrt(st[:, 6:7], st[:, 5:6], 1e-5, st[:, 7:8])
        b.ts('vector', t1, r, st[:, 2:3], ALU.subtract, st[:, 6:7], ALU.mult)
        b.tt('gpsimd', r, t1, lnG, ALU.mult)
        xo = xe
        b.tt('vector', xo, r, lnB, ALU.add)
        b.dma('sync', xdst[t0:t0 + 128, :], xo)

    def moe_sublayer(l, xsrc, xdst):
        b.push()
        load_mod(l, 1)
        TH = min(T, 1024)
        BIG = b.sb([128, 8, TH], BF16, "hTm")
        rw = b.sb([128, 8, 16], F32, "rw")
        b.dma('sync', rw, I['router_w'].re("(k p) e -> p k e", p=128))
        rb = b.sb([1, 16], F32, "rb")
        b.dma('sync', rb, I['router_b'])
        gatesT = b.sb([16, TH], F32, "gatesT")
        sel = b.sb([16, 16, 128], F32, "sel")
        b.memset('vector', sel, 0.0)
        for e in range(16):
            b.dma('sync', sel[e:e + 1, e, :], onesf[e:e + 1, :])
        yacc = b.sb([128, TH // 128, D], F32, "yacc")
        wg_pool = b.pool([128, 8, 512], BF16, 3, "wg")
        wu_pool = b.pool([128, 8, 512], BF16, 3, "wu")
        wd_pool = b.pool([128, 4, D], BF16, 3, "wd")
        s_pool = b.pool([128, 512], BF16, 2, "s")
        t_pool = b.pool([128, 512], BF16, 2, "tg")
        gbs_pool = b.pool([128, 512], F32, 2, "gbs")
        act_pool = b.pool([128, 4, 512], BF16, 3, "actT")
        rt = b.pool([128, 16], F32, 6, "rt")
        r4 = b.pool([128, 4], F32, 8, "r4")
        r1 = b.pool([128, 1], F32, 8, "r1")

        def router(hf, col):
            lg = bank(5)[:, 0:16]
            for kc in range(8):
                b.mm(lg, hf[:, kc, :], rw[:, kc, :], kc == 0, False)
            b.mm(lg, onesf[0:1, :], rb[0:1, :], False, True)
            mx = r1()
            b.red(mx, lg, ALU.max)
            nmx = r1()
            b.ts('vector', nmx, mx, -1.0, ALU.mult)
            ex = rt()
            b.act(ex, lg, AF.Exp, bias=nmx, scale=1.0)
            sm = r1()
            b.red(sm, ex, ALU.add)
            rs = r1()
            b.recip(rs, sm)
            p = rt()
            b.ts('vector', p, ex, rs, ALU.mult)
            pv = p.re("p (g j) -> p g j", j=4)
            prs = []
            for (i0, i1) in ((0, 1), (0, 2), (0, 3), (1, 2), (1, 3), (2, 3)):
                q = r4()
                b.tt('vector', q, pv[:, :, i0], pv[:, :, i1], ALU.add)
                prs.append(q)
            gs = prs[0]
            for q in prs[1:]:
                g2 = r4()
                b.tt('vector', g2, gs, q, ALU.max)
                gs = g2
            gm = r1()
            b.red(gm, gs, ALU.max)
            ing = r4()
            b.ts('vector', ing, gs, gm, ALU.is_equal)
            im1 = r4()
            b.ts('vector', im1, ing, -1.0, ALU.add)
            msk = rt()
            for g in range(4):
                b.ts('vector', msk[:, g * 4:(g + 1) * 4], p[:, g * 4:(g + 1) * 4], ing[:, g:g + 1], ALU.mult,
                     im1[:, g:g + 1], ALU.add)
            tp1 = r1()
            b.red(tp1, msk, ALU.max)
            is1 = rt()
            b.ts('vector', is1, msk, tp1, ALU.is_equal)
            m2 = rt()
            b.stt('vector', m2, is1, -2.0, msk, ALU.mult, ALU.add)
            tp2 = r1()
            b.red(tp2, m2, ALU.max)
            is2 = rt()
            b.ts('vector', is2, m2, tp2, ALU.is_equal)
            sl = rt()
            b.tt('vector', sl, is1, is2, ALU.add)
            den = r1()
            b.tt('vector', den, tp1, tp2, ALU.add)
            rden = r1()
            b.recip(rden, den)
            gt = rt()
            b.stt('vector', gt, p, rden, sl, ALU.mult, ALU.mult)
            gp = bank(5)[0:16, 128:256]
            b.tr(gp, gt, ident)
            b.cp('vector', gatesT[:, col:col + 128], gp)

        def load_w(e):
            wg, wu, wd = wg_pool(), wu_pool(), wd_pool()
            b.dma('gpsimd', wg, I['moe_w_gate'][l, e].re("(k p) f -> p k f", p=128))
            b.dma('gpsimd', wu, I['moe_w_up'][l, e].re("(k p) f -> p k f", p=128))
            b.dma('gpsimd', wd, I['moe_w_down'][l, e].re("(k p) n -> p k n", p=128))
            return wg, wu, wd

        ycnt = [0]
        for half in range(T // TH):
            tok0 = half * TH
            wnext = load_w(0)
            prologue(xsrc, tok0, TH // 128, BIG, 0, router=router)
            nch = TH // 512
            items = [(e, ch) for e in range(16) for ch in range(nch)]
            wcur = {}

            def stage1(e, ch, wts):
                wg, wu, wd = wts
                c0 = ch * 512
                gbp = bank(4)
                b.mm(gbp, sel[:, e, :], gatesT[:, c0:c0 + 512], True, True)
                gbs = gbs_pool()
                b.cp('scalar', gbs, gbp)
                actT = act_pool()
                for fc in range(4):
                    pg, pu = bank(2 * (fc % 2)), bank(2 * (fc % 2) + 1)
                    for kc in range(8):
                        b.mm(pg, wg[:, kc, fc * 128:(fc + 1) * 128], BIG[:, kc, c0:c0 + 512], kc == 0, kc == 7)
                    for kc in range(8):
                        b.mm(pu, wu[:, kc, fc * 128:(fc + 1) * 128], BIG[:, kc, c0:c0 + 512], kc == 0, kc == 7)
                    s_ = s_pool()
                    b.act(s_, pg, AF.Silu)
                    tg = t_pool()
                    b.tt('vector', tg, pu, gbs, ALU.mult)
                    b.tt('gpsimd', actT[:, fc, :], s_, tg, ALU.mult)
                return actT

            def stage2(e, ch, wts, actT):
                wd = wts[2]
                for ti in range(4):
                    ycnt[0] += 1
                    ya, yb = (PS[3][0], PS[3][1]) if ycnt[0] % 2 else (bank(4), bank(5))
                    for nh, yp in ((0, ya), (1, yb)):
                        for fc in range(4):
                            b.mm(yp, actT[:, fc, ti * 128:(ti + 1) * 128], wd[:, fc, nh * 512:(nh + 1) * 512],
                                 fc == 0, fc == 3)
                    tix = ch * 4 + ti
                    for nh, yp in ((0, ya), (1, yb)):
                        ys = yacc[:, tix, nh * 512:(nh + 1) * 512]
                        if e == 0:
                            b.cp('vector', ys, yp)
                        else:
                            b.tt('vector', ys, ys, yp, ALU.add)

            prev = None
            for (e, ch) in items:
                if ch == 0:
                    wcur[e] = wnext
                    if e + 1 < 16:
                        wnext = load_w(e + 1)
                a = stage1(e, ch, wcur[e])
                if prev is not None:
                    stage2(*prev)
                prev = (e, ch, wcur[e], a)
            stage2(*prev)
            for i in range(TH // 128):
                epilogue(xsrc, xdst, tok0 + i * 128, yacc[:, i, :])
        b.pop()

    def load_wsec(pool, win, sec):
        w = pool()
        b.dma('gpsimd', w, win[:, sec * 512:(sec + 1) * 512].re("(k p) n -> p k n", p=128))
        return w

    def proj_heads64(BIG, w, dst, stage_pool):
        for g in range(8):
            stg = stage_pool()
            for ch in range(NCH):
                pq = bank(ch % 2)
                for kc in range(8):
                    b.mm(pq[0:64, :], w[:, kc, g * 64:(g + 1) * 64], BIG[:, kc, ch * 512:(ch + 1) * 512], kc == 0, kc == 7)
                b.cp('scalar' if ch % 2 else 'vector', stg[0:64, ch * 512:(ch + 1) * 512], pq[0:64, :])
            b.dma('sync', dst[g], stg[0:64, :])

    def proj_tok(BIG, w, dst, st_pool):
        for ti in range(NT):
            pv = bank(ti % 2)
            for kc in range(8):
                b.mm(pv, BIG[:, kc, ti * 128:(ti + 1) * 128], w[:, kc, :], kc == 0, kc == 7)
            st = st_pool()
            b.cp('scalar' if ti % 2 else 'vector', st, pv)
            b.dma('sync', dst[ti * 128:(ti + 1) * 128, :], st)

    def outproj_epilogue(BIG, wout, xsrc, xdst):
        wo = b.sb([128, 8, D], BF16, "wo")
        b.dma('gpsimd', wo, wout.re("(k p) n -> p k n", p=128))
        for ti in range(NT):
            ya, yb = PS[2 + ti % 2][0], PS[2 + ti % 2][1]
            for nh, yp in ((0, ya), (1, yb)):
                for kc in range(8):
                    b.mm(yp, BIG[:, kc, ti * 128:(ti + 1) * 128], wo[:, kc, nh * 512:(nh + 1) * 512], kc == 0, kc == 7)
            epilogue(xsrc, xdst, ti * 128, (ya, yb))

    def stop_at(tag):
        if STOP == tag:
            raise _Stop()

    def colvec(src_row, n):
        t = b.sb([128, n], F32, "colv")
        b.dma('sync', t, src_row.re("(c p) -> p c", p=128), slow=True)
        return t

    def odd_sublayer(l, xsrc, xdst):
        j = l // 2
        b.push()
        load_mod(l, 0)
        BIG = b.sb([128, 8, T], BF16, "BIGo")
        prologue(xsrc, 0, NT, BIG, 0)
        win = I['odd_w_in'][j]
        QS = dscr("QS", [8, 64, T], BF16)
        KS = dscr("KS", [8, 64, T], BF16)
        VS = dscr("VS", [T, 512], BF16)
        YR = dscr("YR", [512, T], BF16)
        b.push()
        wpool = b.pool([128, 8, 512], BF16, 2, "wsec")
        cw = b.sb([128, 4, 4], F32, "cw")
        for tap in range(4):
            b.dma('sync', cw[:, tap, :], I['conv_w'][j][tap].re("(c p) -> p c", p=128), slow=True)
        cbv = colvec(I['conv_b'][j], 4)
        bav = colvec(I['lru_ba'][j], 4)
        bxv = colvec(I['lru_bx'][j], 4)
        lamv = colvec(I['lru_lambda'][j], 4)
        e1 = b.sb([128, 4], F32, "e1")
        b.act(e1, lamv, AF.Exp, scale=-1.0)
        spl = b.sb([128, 4], F32, "spl")
        b.act(spl, e1, AF.Ln, bias=1.0)
        negc8 = b.sb([128, 4], F32, "negc8")
        b.ts('vector', negc8, spl, -8.0, ALU.mult)
        wa = b.sb([128, 4, 128], F32, "wa")
        b.dma('sync', wa, I['lru_wa'].re("c p q -> p c q"))
        wx = b.sb([128, 4, 128], F32, "wx")
        b.dma('sync', wx, I['lru_wx'].re("c p q -> p c q"))
        wxr = load_wsec(wpool, win, 0)
        wgr = load_wsec(wpool, win, 1)
        hlast = b.sb([128, 4], F32, "hlast")
        b.memset('vector', hlast, 0.0)
        xrp = [b.sb([128, 515], F32, "xrp") for _ in range(4)]
        for cc in range(4):
            b.memset('gpsimd', xrp[cc][:, 0:3], 0.0)
        f_pool = [b.pool([128, 512], F32, 2, "lf%d" % i) for i in range(6)]
        yr_pool = b.pool([128, 512], BF16, 2, "yr")
        for tc in range(NCH):
            tsl = slice(tc * 512, (tc + 1) * 512)
            for cc in range(4):
                px = bank(0)
                for kc in range(8):
                    b.mm(px, wxr[:, kc, cc * 128:(cc + 1) * 128], BIG[:, kc, tsl], kc == 0, kc == 7)
                xp = xrp[cc]
                b.cp('scalar', xp[:, 3:515], px)
                xc = f_pool[0]()
                b.ts('vector', xc, xp[:, 3:515], cw[:, 3, cc:cc + 1], ALU.mult, cbv[:, cc:cc + 1], ALU.add)
                for tap in (2, 1, 0):
                    b.stt('vector', xc, xp[:, tap:tap + 512], cw[:, tap, cc:cc + 1], xc, ALU.mult, ALU.add)
                hal = small()
                b.cp('gpsimd', hal[:, 0:3], xp[:, 512:515])
                b.cp('gpsimd', xp[:, 0:3], hal[:, 0:3])
                pr, pi = bank(1), bank(2)
                b.mm(pr, wa[:, cc, :], xc)
                b.mm(pi, wx[:, cc, :], xc)
                r = f_pool[1]()
                b.act(r, pr, AF.Sigmoid, bias=bav[:, cc:cc + 1])
                ig = f_pool[2]()
                b.act(ig, pi, AF.Sigmoid, bias=bxv[:, cc:cc + 1])
                a = f_pool[3]()
                b.act(a, r, AF.Exp, scale=negc8[:, cc:cc + 1])
                om = r
                b.tt('gpsimd', om, a, a, ALU.mult)
                b.ts('vector', om, om, -1.0, ALU.mult, 1.0, ALU.add)
                b.ts('vector', om, om, 1e-12, ALU.max)
                b.act(om, om, AF.Sqrt)
                b.tt('gpsimd', ig, ig, xc, ALU.mult)
                u = f_pool[4]()
                b.tt('vector', u, ig, om, ALU.mult)
                hh = f_pool[5]()
                b.scan(hh, a, u, 0.0 if tc == 0 else hlast[:, cc:cc + 1])
                b.cp('vector', hlast[:, cc:cc + 1], hh[:, 511:512])
                pg = bank(3)
                for kc in range(8):
                    b.mm(pg, wgr[:, kc, cc * 128:(cc + 1) * 128], BIG[:, kc, tsl], kc == 0, kc == 7)
                g = xc
                b.cp('scalar', g, pg)
                g2 = ig
                b.tt('gpsimd', g2, g, g, ALU.mult)
                b.ts('vector', g2, g2, 0.044715, ALU.mult, 1.0, ALU.add)
                b.tt('gpsimd', g2, g2, g, ALU.mult)
                b.act(g2, g2, AF.Sigmoid, scale=1.5957691216057308)
                b.tt('gpsimd', g2, g2, g, ALU.mult)
                yr = yr_pool()
                b.tt('vector', yr, g2, hh, ALU.mult)
                b.dma('sync', YR[cc * 128:(cc + 1) * 128, tsl], yr)
        stop_at('oA1')
        stage_pool = b.pool([64, T], BF16, 2, "stg")
        w = load_wsec(wpool, win, 2)
        proj_heads64(BIG, w, QS, stage_pool)
        w = load_wsec(wpool, win, 3)
        proj_heads64(BIG, w, KS, stage_pool)
        w = load_wsec(wpool, win, 4)
        proj_tok(BIG, w, VS, b.pool([128, 512], BF16, 2, "vst"))
        b.pop()
        stop_at('oA2')
        b.push()
        for cc in range(4):
            b.dma('sync', BIG[:, cc, :], YR[cc * 128:(cc + 1) * 128, :])
        tge = b.sb([128, 128], F32, "tge")
        b.tt('vector', tge, csq[:, 128:256], csq[:, 0:128], ALU.add)
        triGEb = b.sb([128, 128], BF16, "triGEb")
        b.cp('vector', triGEb, tge)
        zer = b.sb([128, 512], F32, "zer")
        b.memset('vector', zer, 0.0)
        mkS = b.sb([128, 4, 512], BF16, "mkS")
        b.dma('gpsimd', mkS, c_mask[4:8].re("n p q -> p n q"))
        q_pool = b.pool([64, T], BF16, 2, "qT")
        k_pool = b.pool([64, T], BF16, 2, "kT")
        v_pool = b.pool([128, NT, 64], BF16, 2, "vS")
        e_pool = b.pool([128, 512], F32, 3, "e")
        sp_pool = b.pool([128, 512], F32, 3, "sp")
        spb_pool = b.pool([128, 512], BF16, 4, "spb")
        arg_pool = b.pool([128, 512], F32, 4, "arg")
        p_pool = b.pool([128, 512], BF16, 3, "P")
        sfx_pool = b.pool([128, 512], F32, 2, "sfx")
        ost_pool = b.pool([64, 512], BF16, 2, "ost")
        VSv = VS.re("(n p) c -> p n c", p=128)
        blk = [0]
        for h in range(8):
            qT, kT, vv = q_pool(), k_pool(), v_pool()
            b.dma('sync', qT, QS[h])
            b.dma('sync', kT, KS[h])
            b.dma('sync', vv, VSv[:, :, h * 64:(h + 1) * 64])
            for Q in range(NCH):
                qs = slice(Q * 512, (Q + 1) * 512)
                blk[0] += 1
                OT = bank(6 + blk[0] % 2)[0:64, :]
                sfx = sfx_pool()
                Js = list(reversed(range(4 * Q + 4)))
                last = len(Js) - 1
                st_ = {}
                for k in range(-2, last + 1):
                    ia, ib, ic = k + 2, k + 1, k
                    if ia <= last:
                        zt = bank(ia % 2)
                        J = Js[ia]
                        b.mm(zt, kT[:, J * 128:(J + 1) * 128], qT[:, qs])
                        st_[ia] = dict(zt=zt, r=J - 4 * Q, J=J)
                    if ic >= 0:
                        c_ = st_[ic]
                        P = p_pool()
                        b.act(P, c_['arg'], AF.Exp)
                        if c_['r'] >= 0:
                            b.tt('gpsimd', P, P, mkS[:, c_['r'], :], ALU.mult)
                        c_['P'] = P
                    if ia <= last:
                        a_ = st_[ia]
                        e = e_pool()
                        b.act(e, a_['zt'], AF.Exp, scale=0.125)
                        spb = spb_pool()
                        b.act(spb, e, AF.Ln, bias=1.0)
                        if a_['r'] >= 0:
                            b.tt('gpsimd', spb, spb, mkS[:, a_['r'], :], ALU.mult)
                        a_['e'], a_['spb'] = e, spb
                    if 0 <= ib <= last:
                        b_ = st_[ib]
                        ge = bank(2 + ib % 2)
                        b.mm(ge, triGEb, b_['spb'])
                        bt = None
                        if ib < last:
                            bt = bank(4 + ib % 2)
                            b.mm(bt, onesb, b_['spb'])
                        arg = arg_pool()
                        b.stt('vector', arg, b_['zt'], 0.125, zer if ib == 0 else sfx, ALU.mult, ALU.subtract,
                              xr=(b_['e'],))
                        b.tt('vector', arg, arg, ge, ALU.subtract)
                        if ib < last:
                            if ib == 0:
                                b.cp('vector', sfx, bt)
                            else:
                                b.tt('vector', sfx, sfx, bt, ALU.add)
                        b_['arg'] = arg
                    if ic >= 0:
                        c_ = st_[ic]
                        b.mm(OT, vv[:, c_['J'], :], c_['P'], ic == 0, ic == last)
                        del st_[ic]
                if h % 2 == 0:
                    b.cp('vector', BIG[0:64, 4 + h // 2, qs], OT)
                else:
                    ost = ost_pool()
                    b.cp('vector', ost, OT)
                    b.dma('sync', BIG[64:128, 4 + h // 2, qs], ost)
        b.pop()
        stop_at('oB')
        b.push()
        outproj_epilogue(BIG, I['odd_w_out'][j], xsrc, xdst)
        b.pop()
        b.pop()

    def even_sublayer(l, xsrc, xdst):
        import math
        j = l // 2
        lam_init = 0.8 - 0.6 * math.exp(-0.3 * l)
        b.push()
        load_mod(l, 0)
        BIG = b.sb([128, 8, T], BF16, "BIGe")
        prologue(xsrc, 0, NT, BIG, 0)
        win = I['even_w_in'][j]
        QA = dscr("QA", [8, 64, T], BF16)
        KA = dscr("KA", [8, 64, T], BF16)
        VA = dscr("VA", [T, 512], BF16)
        QST = dscr("QST", [512, T], F32)
        KTs = dscr("KTs", [512, T], F32)
        SGT = dscr("SGT", [512, T], BF16)
        LF = dscr("LF", [T, 512], F32)
        KTOK = dscr("KTOK", [T, 512], F32)
        VH = dscr("VH", [T, 512], BF16)
        lbscr = dscr("lbscr", [2, 512], F32)
        gm = b.sb([1, 3, 512], F32, "gm")
        b.dma('sync', gm, I['hgrn_gamma'].re("(o a) c -> o a c", o=1))
        eg = b.sb([1, 3, 512], F32, "eg")
        b.act(eg, gm, AF.Exp)
        den = b.sb([1, 512], F32, "den")
        b.tt('vector', den, eg[:, 0, :], eg[:, 1, :], ALU.add)
        b.tt('vector', den, den, eg[:, 2, :], ALU.add)
        num = b.sb([1, 512], F32, "num")
        b.cp('vector', num, eg[:, 0, :])
        for i in range(1, l + 1):
            b.tt('vector', num, num, eg[:, i, :], ALU.add)
        rden = b.sb([1, 512], F32, "rden")
        b.recip(rden, den)
        lbr = b.sb([1, 2, 512], F32, "lbr")
        b.tt('vector', lbr[:, 0, :], num, rden, ALU.mult)
        b.ts('vector', lbr[:, 1, :], lbr[:, 0, :], -1.0, ALU.mult, 1.0, ALU.add)
        b.dma('sync', lbscr.re("(o a) c -> o a c", o=1), lbr)
        omlF = colvec(lbscr[1], 4)
        lbB = b.sb([128, 512], F32, "lbB")
        b.dma('sync', lbB, lbscr[0].pb())
        omlB = b.sb([128, 512], F32, "omlB")
        b.dma('sync', omlB, lbscr[1].pb())
        dl = b.sb([128, 256], F32, "dl")
        b.dma('sync', dl, I['diff_lambda'][j].re("a d -> (a d)").pb())
        lw = b.sb([128, 128], F32, "lw")
        lsm = b.sb([128, 8], F32, "lsm")
        b.tt('vector', lw[:, 0:64], dl[:, 0:64], dl[:, 64:128], ALU.mult)
        b.tt('vector', lw[:, 64:128], dl[:, 128:192], dl[:, 192:256], ALU.mult)
        b.red(lsm[:, 0:1], lw[:, 0:64], ALU.add)
        b.red(lsm[:, 1:2], lw[:, 64:128], ALU.add)
        b.act(lsm[:, 2:4], lsm[:, 0:2], AF.Exp)
        b.tt('vector', lsm[:, 4:5], lsm[:, 3:4], lsm[:, 2:3], ALU.subtract)
        b.ts('vector', lsm[:, 5:6], lsm[:, 4:5], -lam_init, ALU.add)
        neglam = lsm[:, 5:6]
        gv = b.sb([128, 4], F32, "gv")
        b.dma('sync', gv[:, 0:1], I['diff_gain'][j].re("(p o) -> p o", o=1), slow=True)
        b.dma('sync', gv[:, 1:2], I['hgrn_gain'][j].re("(p o) -> p o", o=1), slow=True)
        b.ts('vector', gv[:, 2:3], gv[:, 0:1], math.sqrt(128.0) * (1.0 - lam_init), ALU.mult)
        b.ts('vector', gv[:, 3:4], gv[:, 1:2], math.sqrt(128.0), ALU.mult)
        dgS, hgS = gv[:, 2:3], gv[:, 3:4]
        stop_at('e0')
        b.push()
        wpool = b.pool([128, 8, 512], BF16, 2, "wsec")
        stage_pool = b.pool([64, T], BF16, 2, "stg")
        w = load_wsec(wpool, win, 0)
        proj_heads64(BIG, w, QA, stage_pool)
        w = load_wsec(wpool, win, 1)
        proj_heads64(BIG, w, KA, stage_pool)
        st_pool = b.pool([128, 512], BF16, 2, "vst")
        w = load_wsec(wpool, win, 2)
        proj_tok(BIG, w, VA, st_pool)
        w = load_wsec(wpool, win, 5)
        proj_tok(BIG, w, VH, st_pool)
        fst_pool = b.pool([128, 512], F32, 3, "fst")
        bst_pool = b.pool([128, 512], BF16, 2, "bst")
        w = load_wsec(wpool, win, 3)
        for h in range(4):
            for ch in range(NCH):
                pq = bank(ch % 2)
                for kc in range(8):
                    b.mm(pq, w[:, kc, h * 128:(h + 1) * 128], BIG[:, kc, ch * 512:(ch + 1) * 512], kc == 0, kc == 7)
                st = fst_pool()
                b.act(st, pq, AF.Silu)
                b.dma('sync', QST[h * 128:(h + 1) * 128, ch * 512:(ch + 1) * 512], st)
        w = load_wsec(wpool, win, 6)
        for h in range(4):
            for ch in range(NCH):
                pq = bank(ch % 2)
                for kc in range(8):
                    b.mm(pq, w[:, kc, h * 128:(h + 1) * 128], BIG[:, kc, ch * 512:(ch + 1) * 512], kc == 0, kc == 7)
                st = bst_pool()
                b.act(st, pq, AF.Silu)
                b.dma('sync', SGT[h * 128:(h + 1) * 128, ch * 512:(ch + 1) * 512], st)
        w = load_wsec(wpool, win, 4)
        for h in range(4):
            for ch in range(NCH):
                pq = bank(ch % 2)
                for kc in range(8):
                    b.mm(pq, w[:, kc, h * 128:(h + 1) * 128], BIG[:, kc, ch * 512:(ch + 1) * 512], kc == 0, kc == 7)
                sg = fst_pool()
                b.act(sg, pq, AF.Sigmoid, scale=-1.0)
                st = fst_pool()
                b.ts('vector', st, sg, omlF[:, h:h + 1], ALU.mult)
                b.dma('sync', KTs[h * 128:(h + 1) * 128, ch * 512:(ch + 1) * 512], st)
        for ti in range(NT):
            pv = bank(2 + ti % 2)
            for kc in range(8):
                b.mm(pv, BIG[:, kc, ti * 128:(ti + 1) * 128], w[:, kc, :], kc == 0, kc == 7)
            sg = fst_pool()
            b.act(sg, pv, AF.Sigmoid)
            tq = fst_pool()
            b.tt('vector', tq, sg, omlB, ALU.mult)
            b.tt('gpsimd', sg, tq, lbB, ALU.add)
            lf = fst_pool()
            b.act(lf, sg, AF.Ln)
            b.dma('sync', LF[ti * 128:(ti + 1) * 128, :], lf)
            b.tt('gpsimd', sg, omlB, tq, ALU.subtract)
            b.dma('sync', KTOK[ti * 128:(ti + 1) * 128, :], sg)
        b.pop()
        stop_at('eA')
        b.push()
        mkI = b.sb([128, 4, 512], BF16, "mkI")
        b.dma('gpsimd', mkI, c_mask[0:4].re("n p q -> p n q"))
        qT = b.sb([68, 2, T], BF16, "qTa")
        kT = b.sb([68, 2, T], BF16, "kTa")
        vv = b.sb([128, NT, 128], BF16, "vA")
        qTg = V(qT.ap, Res("qaug"))
        kTg = V(kT.ap, Res("kaug"))
        p_pool = b.pool([128, 512], BF16, 4, "Pa")
        rl_pool = b.pool([128, 512], F32, 4, "rl")
        oc_pool = b.pool([128, 512], F32, 4, "oc")
        VAv = VA.re("(n p) c -> p n c", p=128)
        blk = [0]
        for h in range(4):
            for c in range(2):
                b.dma('sync', qT[0:64, c, :], QA[2 * h + c])
                b.dma('sync', kT[0:64, c, :], KA[2 * h + c])
                b.dma('gpsimd', qTg[64:68, c, :], c_aug[h, 0])
                b.dma('gpsimd', kTg[64:68, c, :], c_aug[h, 1])
            b.dma('sync', vv, VAv[:, :, h * 128:(h + 1) * 128])
            for Q in range(NCH):
                qs = slice(Q * 512, (Q + 1) * 512)
                oc = []
                for c in range(2):
                    blk[0] += 1
                    OT, LT = bank(2 + blk[0] % 2), bank(4 + blk[0] % 2)
                    nJ = 4 * Q + 4

                    def s1(J):
                        r = J - 4 * Q
                        st = bank(J % 2)
                        b.mm(st, kT[0:68, c, J * 128:(J + 1) * 128], qT[0:68, c, qs], xr=(qTg, kTg))
                        P = p_pool()
                        if r >= 0:
                            cl = rl_pool()
                            b.ts('vector', cl, st, 0.125, ALU.mult, 80.0, ALU.min)
                            b.act(P, cl, AF.Exp)
                            b.tt('gpsimd', P, P, mkI[:, r, :], ALU.mult)
                        else:
                            b.act(P, st, AF.Exp, scale=0.125)
                        return P

                    P = s1(0)
                    for J in range(nJ):
                        Pn = s1(J + 1) if J + 1 < nJ else None
                        b.mm(OT, vv[:, J, :], P, J == 0, J == nJ - 1)
                        b.mm(LT, onesb, P, J == 0, J == nJ - 1)
                        P = Pn
                    rl = rl_pool()
                    b.recip(rl, LT)
                    o_c = oc_pool()
                    b.tt('vector', o_c, OT, rl, ALU.mult)
                    oc.append(o_c)
                o = oc[0]
                b.stt('vector', o, oc[1], neglam, oc[0], ALU.mult, ALU.add)
                sq = oc[1]
                b.tt('gpsimd', sq, o, o, ALU.mult)
                ss = bank(6)
                b.mm(ss, onesf, sq)
                rstd = rl_pool()
                b.rsqrt(rstd, ss, 128e-6, rstd)
                b.stt('vector', BIG[:, h, qs], o, dgS, rstd, ALU.mult, ALU.mult)
        b.pop()
        stop_at('eB')
        b.push()
        hgGT = csq[:, 256:512]
        hgU = csq[:, 512:640]
        tri4 = b.sb([128, 4, 128], F32, "tri4")
        for h in range(4):
            b.cp('gpsimd', tri4[:, h, :], csq[:, 384:512])
        S0 = [b.sb([128, 128], F32, "S0") for _ in range(4)]
        S1 = [b.sb([128, 128], F32, "S1") for _ in range(4)]
        Sb0 = [b.sb([128, 128], BF16, "Sb0") for _ in range(4)]
        Sb1 = [b.sb([128, 128], BF16, "Sb1") for _ in range(4)]
        for h in range(4):
            b.memset('vector', S0[h], 0.0)
            b.memset('gpsimd', Sb0[h], 0.0)
        lf_pool = b.pool([128, 512], F32, 2, "LFt")
        kt_pool = b.pool([128, 512], F32, 2, "Kt")
        vt_pool = b.pool([128, 512], BF16, 2, "Vt")
        qs_pool = b.pool([128, 4, 128], F32, 2, "QsT")
        ktt_pool = b.pool([128, 4, 128], F32, 2, "KTt")
        sg_pool = b.pool([128, 4, 128], BF16, 2, "SGt")
        E_pool = [b.pool([128, 4, 128], F32, 2, "E%d" % i) for i in range(3)]
        E3_pool = b.pool([128, 512], F32, 2, "E3")
        bf_pool = [b.pool([128, 4, 128], BF16, 2, "hb%d" % i) for i in range(3)]
        kd_pool = b.pool([128, 512], BF16, 4, "kd")
        at_pool = b.pool([128, 512], BF16, 2, "attm")
        sq_pool = b.pool([128, 512], F32, 2, "hsq")
        QSTv = QST.re("(h d) t -> d h t", d=128)
        KTv = KTs.re("(h d) t -> d h t", d=128)
        SGTv = SGT.re("(h d) t -> d h t", d=128)
        for ti in range(NT):
            ts_ = slice(ti * 128, (ti + 1) * 128)
            LFt, Kt, Vt = lf_pool(), kt_pool(), vt_pool()
            QsT, KTt, SGt = qs_pool(), ktt_pool(), sg_pool()
            b.dma('sync', LFt, LF[ts_, :])
            b.dma('sync', Kt, KTOK[ts_, :])
            b.dma('sync', Vt, VH[ts_, :])
            b.dma('sync', QsT, QSTv[:, :, ts_])
            b.dma('sync', KTt, KTv[:, :, ts_])
            b.dma('sync', SGt, SGTv[:, :, ts_])
            for h in range(4):
                b.mm(bank(h // 2)[:, (h % 2) * 256:(h % 2 + 1) * 256], LFt[:, h * 128:(h + 1) * 128], hgGT)
            pbu = bank(2)
            b.mm(pbu, hgU, LFt)
            E1, E1i, E2 = E_pool[0](), E_pool[1](), E_pool[2]()
            for hf2 in range(2):
                pv = bank(hf2).re("p (h a c) -> p h a c", h=2, a=2)
                hs = slice(2 * hf2, 2 * hf2 + 2)
                b.act(E1[:, hs, :], pv[:, :, 0, :], AF.Exp)
                b.act(E1i[:, hs, :], pv[:, :, 0, :], AF.Exp, scale=-1.0)
                b.act(E2[:, hs, :], pv[:, :, 1, :], AF.Exp)
            stop_at('c0')
            E3 = E3_pool()
            b.act(E3, pbu, AF.Exp)
            qbT, kbT, qeT = bf_pool[0](), bf_pool[1](), bf_pool[2]()
            b.tt('vector', qbT, QsT, E1, ALU.mult)
            b.tt('gpsimd', kbT, KTt, E1i, ALU.mult)
            b.tt('vector', qeT, QsT, E2, ALU.mult)
            kdA, kdB = kd_pool(), kd_pool()
            b.stt('vector', kdA, Kt, csq[:, 447:448], E3, ALU.mult, ALU.mult)
            b.stt('vector', kdB, Kt, csq[:, 511:512], E3, ALU.mult, ALU.mult)
            stop_at('c1')
            patt = bank(3)
            for h in range(4):
                b.mm(patt[:, h * 128:(h + 1) * 128], kbT[:, h, :], qbT[:, h, :])
            attm = at_pool()
            b.tt('vector', attm, patt, tri4.re("p h c -> p (h c)"), ALU.mult)
            for h in range(4):
                for chk in range(2):
                    reg = bank(4 + h // 2)[:, ((h % 2) * 2 + chk) * 128:((h % 2) * 2 + chk + 1) * 128]
                    b.mm(reg, (kdA, kdB)[chk][:, h * 128:(h + 1) * 128], Vt[:, h * 128:(h + 1) * 128])
            stop_at('c2')
            po = bank(6)
            for h in range(4):
                hc = slice(h * 128, (h + 1) * 128)
                ca = slice(h * 128, h * 128 + 64)
                cb_ = slice(h * 128 + 64, h * 128 + 128)
                b.mm(po[:, ca], Vt[:, hc], attm[:, ca], True, False)
                b.mm(po[:, ca], Sb0[h], qeT[:, h, 0:64], False, True)
                dSa = bank(4 + h // 2)[:, ((h % 2) * 2) * 128:((h % 2) * 2 + 1) * 128]
                dSb = bank(4 + h // 2)[:, ((h % 2) * 2 + 1) * 128:((h % 2) * 2 + 2) * 128]
                b.stt('vector', S1[h], S0[h], E2[:, h, 63:64], dSa, ALU.mult, ALU.add)
                b.cp('gpsimd', Sb1[h], S1[h])
                b.mm(po[:, cb_], Vt[:, hc], attm[:, cb_], True, False)
                b.mm(po[:, cb_], Sb1[h], qeT[:, h, 64:128], False, True)
                b.stt('vector', S0[h], S1[h], E2[:, h, 127:128], dSb, ALU.mult, ALU.add)
                b.cp('gpsimd', Sb0[h], S0[h])
            stop_at('c3')
            sq = sq_pool()
            b.act(sq, po, AF.Square)
            pss = bank(7)
            b.mm(pss, onesf, sq)
            rstd = sq_pool()
            b.rsqrt(rstd, pss, 128e-6, rstd)
            b.stt('vector', sq, po, hgS, rstd, ALU.mult, ALU.mult)
            b.tt('gpsimd', BIG[:, 4:8, ts_], sq.re("p (h c) -> p h c", h=4), SGt, ALU.mult)
        b.pop()
        b.push()
        outproj_epilogue(BIG, I['even_w_out'][j], xsrc, xdst)
        b.pop()
        b.pop()

    cur = x_in
    for si, name in enumerate(plan):
        dst = out if si == len(plan) - 1 else xs[si % 2]
        l = int(name[-1])
        try:
            if name.startswith('moe'):
                moe_sublayer(l, cur, dst)
            elif name.startswith('even'):
                even_sublayer(l, cur, dst)
            elif name.startswith('odd'):
                odd_sublayer(l, cur, dst)
        except _Stop:
            break
        cur = dst
    b.P.barrier()
    b.P.emit()
    return nc, b


def prep_inputs(inputs, T):
    f = lambda a: np.ascontiguousarray(np.asarray(a, dtype=np.float32))
    shared = {}
    for k in IN_SHAPES:
        a = f(inputs[k])
        if k in ('lru_wa', 'lru_wx'):
            bd = np.zeros((4, 128, 128), np.float32)
            for g in range(8):
                bd[g // 2, (g % 2) * 64:(g % 2 + 1) * 64, (g % 2) * 64:(g % 2 + 1) * 64] = a[0, g]
            a = bd
        elif k == 'router_b':
            a = a.reshape(1, 16)
        shared[k] = a
    shared.update(host_consts(T))
    return shared


def kernel(**inputs):
    x = np.asarray(inputs['x'], dtype=np.float32)
    c = np.asarray(inputs['c'], dtype=np.float32)
    Bn, T, _ = x.shape
    shared = prep_inputs(inputs, T)
    nc, _ = build(T, ['even0', 'moe0', 'odd1', 'moe1'])
    in_maps = []
    for i in range(Bn):
        m = dict(shared)
        m['x'] = np.ascontiguousarray(x[i])
        m['c'] = np.ascontiguousarray(c[i:i + 1])
        in_maps.append(m)
    res = run_bass_kernel_spmd(nc, in_maps, core_ids=list(range(Bn)))
    return np.stack([np.asarray(r['out'], dtype=np.float32) for r in res.results], axis=0)
```

```python
import contextlib
import numpy as np
import concourse.bass as bass
import concourse.mybir as mybir
from concourse.bass_utils import run_bass_kernel_spmd

F32 = mybir.dt.float32
BF16 = mybir.dt.bfloat16
AF = mybir.ActivationFunctionType
ALU = mybir.AluOpType
AX = mybir.AxisListType

ENGS = ['tensor', 'vector', 'scalar', 'gpsimd', 'sync']
SEM_LIMIT = 30000
D = 1024
ALPHA = 4.0 ** 0.25
N_CORES = 8


class Res:
    __slots__ = ('name', 'last_w', 'readers', 'ds')

    def __init__(self, name):
        self.name = name
        self.last_w = None
        self.readers = []
        self.ds = {}


class DSem:
    __slots__ = ('name', 'dcount', 'dsem', 'kind')

    def __init__(self, name, kind):
        self.name = name
        self.dcount = 0
        self.dsem = None
        self.kind = kind


class Prog:
    def __init__(self, nc):
        self.nc = nc
        self.ops = {e: [] for e in ENGS}
        self.seen = {x: {e: -1 for e in ENGS} for x in ENGS}
        self.seen_d = {x: {} for x in ENGS}
        self.dma_res = []
        self.free_ds = {'sw': [], 'hw': []}
        self.all_dma_toks = []
        self.stack = contextlib.ExitStack()
        self.pending = {x: [] for x in ENGS}
        self.scopes = []

    def sbuf(self, name, shape, dtype):
        st = self.scopes[-1] if self.scopes else self.stack
        return st.enter_context(self.nc.sbuf_tensor(name, list(shape), dtype))

    def push(self):
        self.scopes.append(contextlib.ExitStack())

    def pop(self):
        self.scopes.pop().close()

    def psum(self, name, shape, dtype):
        return self.stack.enter_context(self.nc.psum_tensor(name, list(shape), dtype))

    def _need(self, eng, tok, waits):
        if tok is None:
            return
        if tok[0] == 'e':
            _, e, idx = tok
            if e == eng and eng == 'tensor':
                return
            if self.seen[eng][e] >= idx:
                return
            self.seen[eng][e] = idx
            waits.append(tok)
        else:
            _, r, val = tok
            if self.seen_d[eng].get(id(r), 0) >= val:
                return
            self.seen_d[eng][id(r)] = val
            waits.append(tok)

    def op(self, eng, fn, reads=(), writes=(), dma_res=None):
        waits = []
        for r in reads:
            self._need(eng, r.last_w, waits)
        for r in writes:
            self._need(eng, r.last_w, waits)
            for t in r.readers:
                self._need(eng, t, waits)
        for t in self.pending[eng]:
            self._need(eng, t, waits)
        self.pending[eng] = []
        idx = len(self.ops[eng])
        if dma_res is not None:
            kind = 'sw' if eng == 'gpsimd' else 'hw'
            ds = dma_res.ds.get(kind)
            if ds is None:
                fl = self.free_ds[kind]
                if fl:
                    ds = fl.pop()
                else:
                    ds = DSem("%s%d" % (kind, len(self.dma_res)), kind)
                    self.dma_res.append(ds)
                dma_res.ds[kind] = ds
            dma_res = ds
            ds.dcount += 16
            tok = ('d', ds, ds.dcount)
            self.all_dma_toks.append(tok)
        else:
            tok = ('e', eng, idx)
        import sys
        fr = sys._getframe(2)
        site = "%s:%d<%s:%d" % (fr.f_code.co_name, fr.f_lineno, fr.f_back.f_code.co_name, fr.f_back.f_lineno)
        self.ops[eng].append(dict(fn=fn, waits=waits, dma_res=dma_res, site=site,
                                  dval=(dma_res.dcount if dma_res else 0), mile=False))
        for r in writes:
            r.last_w = tok
            r.readers = []
        for r in reads:
            if r not in writes:
                r.readers.append(tok)
        return tok

    def barrier(self):
        toks = []
        for e in ENGS:
            for idx in range(len(self.ops[e]) - 1, -1, -1):
                if self.ops[e][idx]['dma_res'] is None:
                    toks.append(('e', e, idx))
                    break
        dtoks = {}
        for t in self.all_dma_toks:
            dtoks[id(t[1])] = t
        self.all_dma_toks = []
        for x in ENGS:
            self.pending[x].extend(list(toks) + list(dtoks.values()))

    def emit(self):
        nc = self.nc
        self.tail = {}
        for x in ENGS:
            w = []
            for t in self.pending[x]:
                self._need(x, t, w)
            self.tail[x] = w
            for t in w:
                if t[0] == 'e':
                    self.ops[t[1]][t[2]]['mile'] = True
        for x in ENGS:
            for o in self.ops[x]:
                for t in o['waits']:
                    if t[0] == 'e':
                        self.ops[t[1]][t[2]]['mile'] = True
        nsem = 0
        for x in ENGS:
            cnt = 0
            cur = None
            for o in self.ops[x]:
                if o['mile'] and o['dma_res'] is None:
                    if cur is None or cnt >= SEM_LIMIT:
                        cur = self.stack.enter_context(nc.semaphore(f"m_{x}_{nsem}"))
                        nsem += 1
                        cnt = 0
                    cnt += 1
                    o['msem'] = (cur, cnt)
        per16 = (SEM_LIMIT // 16) * 16
        for r in self.dma_res:
            n = r.dcount
            r.dsem = []
            for i in range((n + per16 - 1) // per16):
                r.dsem.append(self.stack.enter_context(nc.semaphore(f"d_{r.name}_{i}")))
                nsem += 1
        self.nsem = nsem

        def dsem_of(r, val):
            k = (val - 16) // per16
            return r.dsem[k], val - k * per16

        ops = self.ops
        self.imap = {}
        ctr = [0]

        def body(x):
            def f(eng):
                for o in ops[x]:
                    self.imap[ctr[0] + len(o['waits'])] = (x, o['site'])
                    ctr[0] += len(o['waits']) + 1
                    for t in o['waits']:
                        if t[0] == 'e':
                            s, v = ops[t[1]][t[2]]['msem']
                        else:
                            s, v = dsem_of(t[1], t[2])
                        eng.wait_ge(s, v)
                    inst = o['fn'](eng)
                    if o['dma_res'] is not None:
                        s, v = dsem_of(o['dma_res'], o['dval'])
                        inst.then_inc(s, 16)
                    elif o['mile']:
                        s, v = o['msem']
                        inst.then_inc(s, 1)
                for t in self.tail[x]:
                    if t[0] == 'e':
                        s, v = ops[t[1]][t[2]]['msem']
                    else:
                        s, v = dsem_of(t[1], t[2])
                    eng.wait_ge(s, v)
            return f

        with nc.Block() as block:
            block.tensor(body('tensor'))
            block.vector(body('vector'))
            block.scalar(body('scalar'))
            block.gpsimd(body('gpsimd'))
            block.sync(body('sync'))
        self.stack.close()


class V:
    __slots__ = ('ap', 'r')

    def __init__(self, ap, r):
        self.ap = ap
        self.r = r

    def __getitem__(self, k):
        return V(self.ap[k], self.r)

    def re(self, s, **kw):
        return V(self.ap.rearrange(s, **kw), self.r)

    def pb(self, n=128):
        return V(self.ap.partition_broadcast(n), self.r)


def _rs(xs):
    out = []
    for x in xs:
        if isinstance(x, V) and x.r not in out:
            out.append(x.r)
    return out


def _a(x):
    return x.ap if isinstance(x, V) else x


class Bld:
    ARENA_WORDS = 52992

    def __init__(self, nc):
        self.nc = nc
        self.P = Prog(nc)
        self.n = 0
        self.arena = self.P.stack.enter_context(nc.sbuf_tensor("arena", [128, self.ARENA_WORDS], F32))
        self.off = 0
        self.marks = []
        self.peak = 0
        self.scope_res = [[]]

    def push(self):
        self.marks.append(self.off)
        self.scope_res.append([])

    def pop(self):
        self.off = self.marks.pop()
        self.P.barrier()
        for r in self.scope_res.pop():
            for kind, ds in r.ds.items():
                self.P.free_ds[kind].append(ds)
            r.ds = {}

    def sb(self, shape, dt, name=None):
        self.n += 1
        name = f"{name or 't'}_{self.n}"
        nel = int(np.prod(shape[1:]))
        nbytes = nel * (2 if dt == BF16 else 4)
        words = (nbytes + 63) // 64 * 16
        assert self.off + words <= self.ARENA_WORDS, f"SBUF arena overflow at {name}: {self.off * 4} + {words * 4}"
        ap = self.arena[0:shape[0], self.off:self.off + (nbytes + 3) // 4]
        self.off += words
        self.peak = max(self.peak, self.off * 4)
        if dt == BF16:
            ap = ap.bitcast(BF16)
            if nel % 2:
                ap = ap[:, 0:nel]
        if len(shape) == 3:
            ap = ap.rearrange("p (a b) -> p a b", a=shape[1])
        elif len(shape) == 4:
            ap = ap.rearrange("p (a b c) -> p a b c", a=shape[1], b=shape[2])
        r = Res(name)
        self.scope_res[-1].append(r)
        return V(ap, r)

    def pool(self, shape, dt, n, name=None):
        tiles = [self.sb(shape, dt, name) for _ in range(n)]
        st = [0]

        def nxt():
            st[0] += 1
            return tiles[(st[0] - 1) % n]
        return nxt

    def act(self, out, in_, func, bias=None, scale=None):
        kw = {}
        if bias is not None:
            kw['bias'] = _a(bias)
        if scale is not None:
            kw['scale'] = _a(scale)
        o, i = out.ap, in_.ap
        self.P.op('scalar', lambda e: e.activation(out=o, in_=i, func=func, **kw),
                  reads=_rs([in_, bias, scale]), writes=[out.r])

    def tt(self, eng, out, a, b, op):
        o, x, y = out.ap, a.ap, b.ap
        self.P.op(eng, lambda e: e.tensor_tensor(out=o, in0=x, in1=y, op=op), reads=_rs([a, b]), writes=[out.r])

    def ts(self, eng, out, a, s1, op0, s2=None, op1=None):
        o, x, p, q = out.ap, a.ap, _a(s1), _a(s2)
        if s2 is None:
            fn = lambda e: e.tensor_scalar(out=o, in0=x, scalar1=p, scalar2=None, op0=op0)
        else:
            fn = lambda e: e.tensor_scalar(out=o, in0=x, scalar1=p, scalar2=q, op0=op0, op1=op1)
        self.P.op(eng, fn, reads=_rs([a, s1, s2]), writes=[out.r])

    def stt(self, eng, out, a, s, b, op0, op1, xr=()):
        o, x, p, y = out.ap, a.ap, _a(s), b.ap
        eng = 'vector'
        self.P.op(eng, lambda e: e.scalar_tensor_tensor(out=o, in0=x, scalar=p, in1=y, op0=op0, op1=op1),
                  reads=_rs([a, s, b] + list(xr)), writes=[out.r])

    def rsqrt(self, out, a, eps, tmp, scale=1.0):
        self.act(tmp, a, AF.Ln, bias=eps, scale=scale)
        self.act(out, tmp, AF.Exp, scale=-0.5)

    def cp(self, eng, out, a):
        o, x = out.ap, a.ap
        if eng == 'scalar':
            self.P.op(eng, lambda e: e.copy(out=o, in_=x), reads=[a.r], writes=[out.r])
        else:
            self.P.op(eng, lambda e: e.tensor_copy(out=o, in_=x), reads=[a.r], writes=[out.r])

    def red(self, out, a, op):
        o, x = out.ap, a.ap
        self.P.op('vector', lambda e: e.tensor_reduce(out=o, in_=x, axis=AX.X, op=op), reads=[a.r], writes=[out.r])

    def recip(self, out, a):
        o, x = out.ap, a.ap
        self.P.op('vector', lambda e: e.reciprocal(out=o, in_=x), reads=[a.r], writes=[out.r])

    def scan(self, out, d0, d1, init):
        o, x, y, z = out.ap, d0.ap, d1.ap, _a(init)
        self.P.op('vector', lambda e: e.tensor_tensor_scan(out=o, data0=x, data1=y, initial=z, op0=ALU.mult,
                                                          op1=ALU.add), reads=_rs([d0, d1, init]), writes=[out.r])

    def memset(self, eng, out, val):
        o = out.ap
        self.P.op(eng, lambda e: e.memset(o, val), writes=[out.r])

    def mm(self, out, lhsT, rhs, start=True, stop=True, xr=()):
        o, l, r = out.ap, lhsT.ap, rhs.ap
        self.P.op('tensor', lambda e: e.matmul(o, l, r, start=start, stop=stop),
                  reads=_rs([lhsT, rhs] + list(xr)), writes=[out.r])

    def tr(self, out, in_, ident):
        o, i, d = out.ap, in_.ap, ident.ap
        self.P.op('tensor', lambda e: e.transpose(o, i, d), reads=_rs([in_, ident]), writes=[out.r])

    def dma(self, q, out, in_, slow=False):
        o, i = out.ap, in_.ap
        kw = dict(allow_slow_non_contiguous=True) if slow else {}
        self.P.op(q, lambda e: e.dma_start(out=o, in_=i, **kw), reads=[in_.r], writes=[out.r], dma_res=out.r)


def host_consts(T):
    p = np.arange(128)
    sq = np.zeros((5, 128, 128), np.float32)
    sq[0] = np.eye(128)
    sq[1] = (p[:, None] > p[None, :])
    same = (p[:, None] // 64) == (p[None, :] // 64)
    mid = (p // 64) * 64 + 31
    s_, c_ = p[:, None], p[None, :]
    sq[2] = (same & (s_ > mid[None, :]) & (s_ <= c_)) * 1.0 - (same & (s_ > c_) & (s_ <= mid[None, :])) * 1.0
    sq[3] = same & (s_ <= c_)
    sq[4] = same & (s_ > c_)
    csq = np.ascontiguousarray(sq.transpose(1, 0, 2).reshape(128, 640))
    i = np.arange(512)
    mk = np.zeros((8, 128, 512), np.float32)
    for r in range(4):
        mk[r] = (128 * r + p[:, None]) <= i[None, :]
        mk[4 + r] = (128 * r + p[:, None]) < i[None, :]
    t = np.arange(T)
    aug = np.zeros((4, 2, 4, T), np.float32)
    for h in range(4):
        sl = 2.0 ** (-8.0 * (h + 1) / 4)
        aug[h, 0, 0] = -8 * sl * 64 * (t // 64)
        aug[h, 0, 1] = -8 * sl * (t % 64)
        aug[h, 0, 2] = 1
        aug[h, 0, 3] = 1
        aug[h, 1, 0] = 1
        aug[h, 1, 1] = 1
        aug[h, 1, 2] = 8 * sl * 64 * (t // 64)
        aug[h, 1, 3] = 8 * sl * (t % 64)
    return dict(c_sq=csq, c_mask=mk, c_aug=aug)


IN_SHAPES = dict(
    ada_w=[2, 1024, 6144], ada_b=[2, 6144], ln_g=[2, 2, 1024], ln_b=[2, 2, 1024],
    even_w_in=[1, 1024, 3584], even_w_out=[1, 1024, 1024], diff_lambda=[1, 4, 64], diff_gain=[1, 128],
    hgrn_gamma=[3, 512], hgrn_gain=[1, 128], odd_w_in=[1, 1024, 2560], odd_w_out=[1, 1024, 1024],
    conv_w=[1, 4, 512], conv_b=[1, 512], lru_wa=[4, 128, 128], lru_ba=[1, 512], lru_wx=[4, 128, 128],
    lru_bx=[1, 512], lru_lambda=[1, 512], router_w=[1024, 16], router_b=[1, 16],
    moe_w_gate=[2, 16, 1024, 512], moe_w_up=[2, 16, 1024, 512], moe_w_down=[2, 16, 512, 1024],
)


class _Stop(Exception):
    pass


STOP = None


def build(T, plan):
    nc = bass.Bass("TRN2", target_bir_lowering=False)
    b = Bld(nc)
    NT, NCH = T // 128, T // 512

    def din(name, shape, dt=F32):
        return V(nc.dram_tensor(name, list(shape), dt, kind="ExternalInput").ap(), Res(name))

    def dscr(name, shape, dt):
        return V(nc.dram_tensor(name, list(shape), dt).ap(), Res(name))

    I = {k: din(k, s) for k, s in IN_SHAPES.items()}
    x_in = din("x", [T, D])
    c_in = din("c", [1, D])
    c_sq = din("c_sq", [128, 640])
    c_mask = din("c_mask", [8, 128, 512])
    c_aug = din("c_aug", [4, 2, 4, T])
    out = V(nc.dram_tensor("out", [T, D], F32, kind="ExternalOutput").ap(), Res("out"))
    xs = [dscr("xs0", [T, D], F32), dscr("xs1", [T, D], F32)]
    modscr = dscr("modscr", [2, 6144], F32)

    PS = []
    for i in range(4):
        t = b.P.psum(f"ps{i}", [128, 1024], F32)
        PS.append([V(t[:, 0:512], Res(f"ps{i}a")), V(t[:, 512:1024], Res(f"ps{i}b")), t])

    def bank(i):
        return PS[i // 2][i % 2]

    csq = b.sb([128, 640], F32, "csq")
    b.dma('sync', csq, c_sq)
    ident = csq[:, 0:128]
    onesf = b.sb([128, 128], F32, "onesf")
    b.memset('vector', onesf, 1.0)
    onesb = b.sb([128, 128], BF16, "onesb")
    b.memset('vector', onesb, 1.0)

    b.push()
    cT = b.sb([128, 8], F32, "cT")
    b.dma('sync', cT, c_in.re("o (k p) -> p (o k)", p=128), slow=True)
    cTs = b.sb([128, 8], F32, "cTs")
    b.act(cTs, cT, AF.Silu)
    cTb = b.sb([128, 8], BF16, "cTb")
    b.cp('vector', cTb, cTs)
    wsec_pool = b.pool([128, 8, 1024], BF16, 2, "wsec")
    layers_needed = sorted({int(p[-1]) for p in plan})
    for l in layers_needed:
        modrow = b.sb([1, 6144], F32, "modrow")
        adab = b.sb([1, 6144], F32, "adab")
        b.dma('sync', adab, I['ada_b'][l:l + 1, :])
        for sec in range(6):
            ws = wsec_pool()
            b.dma('gpsimd', ws, I['ada_w'][l][:, sec * 1024:(sec + 1) * 1024].re("(k p) n -> p k n", p=128))
            for nh in range(2):
                ps = bank(nh)
                for kc in range(8):
                    b.mm(ps[0:1, :], cTb[:, kc:kc + 1], ws[:, kc, nh * 512:(nh + 1) * 512], kc == 0, kc == 7)
                c0 = sec * 1024 + nh * 512
                b.tt('vector', modrow[0:1, c0:c0 + 512], ps[0:1, :], adab[0:1, c0:c0 + 512], ALU.add)
            if sec in (1, 2, 4, 5):
                b.ts('vector', modrow[0:1, sec * 1024:(sec + 1) * 1024], modrow[0:1, sec * 1024:(sec + 1) * 1024],
                     1.0, ALU.add)
        b.dma('sync', modscr[l:l + 1, :], modrow)
    b.pop()

    xt_pool = b.pool([128, D], F32, 2, "xt")
    hf_pool = b.pool([128, 8, 128], F32, 2, "hf")
    scp = b.sb([128, 8], F32, "scp")
    shv = b.sb([128, 8], F32, "shv")
    gB = b.sb([128, D], F32, "gB")
    lnG = b.sb([128, D], F32, "lnG")
    lnB = b.sb([128, D], F32, "lnB")
    ework = [b.pool([128, D], F32, 2, "ew%d" % i) for i in range(2)]
    small = b.pool([128, 8], F32, 4, "small")

    def load_mod(l, s):
        sec_sh, sec_sc, sec_g = (0, 1, 2) if s == 0 else (3, 4, 5)
        b.dma('sync', shv, modscr[l, sec_sh * 1024:(sec_sh + 1) * 1024].re("(k p) -> p k", p=128), slow=True)
        b.dma('sync', scp, modscr[l, sec_sc * 1024:(sec_sc + 1) * 1024].re("(k p) -> p k", p=128), slow=True)
        b.dma('sync', gB, modscr[l, sec_g * 1024:(sec_g + 1) * 1024].pb())
        b.dma('sync', lnG, I['ln_g'][l, s, :].pb())
        b.dma('sync', lnB, I['ln_b'][l, s, :].pb())

    def prologue(xsrc, tok0, ntiles, hT, col0, router=None):
        for i in range(ntiles):
            t0 = tok0 + i * 128
            xt = xt_pool()
            b.dma('sync', xt, xsrc[t0:t0 + 128, :])
            pa, pb_ = PS[3][0], PS[3][1]
            for kc in range(8):
                pk = (pa if kc < 4 else pb_)[:, (kc % 4) * 128:(kc % 4 + 1) * 128]
                b.tr(pk, xt[:, kc * 128:(kc + 1) * 128], ident)
            hf = hf_pool()
            for kc in range(8):
                pk = (pa if kc < 4 else pb_)[:, (kc % 4) * 128:(kc % 4 + 1) * 128]
                b.act(hf[:, kc, :], pk, AF.Identity, bias=shv[:, kc:kc + 1], scale=scp[:, kc:kc + 1])
            b.cp('vector', hT[:, :, col0 + i * 128:col0 + (i + 1) * 128], hf)
            if router is not None:
                router(hf, col0 + i * 128)

    def epi_a(xsrc, t0, y):
        xe = xt_pool()
        b.dma('sync', xe, xsrc[t0:t0 + 128, :])
        t1 = ework[0]()
        if isinstance(y, tuple):
            b.tt('vector', t1[:, 0:512], y[0], gB[:, 0:512], ALU.mult)
            b.tt('vector', t1[:, 512:1024], y[1], gB[:, 512:1024], ALU.mult)
        else:
            b.tt('vector', t1, y, gB, ALU.mult)
        r = ework[1]()
        b.stt('vector', r, xe, ALPHA, t1, ALU.mult, ALU.add)
        b.act(t1, r, AF.Square)
        st = small()
        b.red(st[:, 0:1], r, ALU.add)
        b.red(st[:, 1:2], t1, ALU.add)
        return (t0, xe, t1, r, st)

    def epi_b(xdst, ctx):
        t0, xe, t1, r, st = ctx
        b.ts('vector', st[:, 2:4], st[:, 0:2], 1.0 / D, ALU.mult)
        b.tt('vector', st[:, 4:5], st[:, 2:3], st[:, 2:3], ALU.mult)
        b.tt('vector', st[:, 5:6], st[:, 3:4], st[:, 4:5], ALU.subtract)
        b.rsqrt(st[:, 6:7], st[:, 5:6], 1e-5, st[:, 7:8])
        b.ts('vector', t1, r, st[:, 2:3], ALU.subtract, st[:, 6:7], ALU.mult)
        b.tt('gpsimd', r, t1, lnG, ALU.mult)
        xo = xe
        b.tt('vector', xo, r, lnB, ALU.add)
        b.dma('sync', xdst[t0:t0 + 128, :], xo)

    def epilogue_seq(xsrc, xdst, tiles, produce=None):
        n = len(tiles)
        ctx = epi_a(xsrc, tiles[0], produce(0))
        for i in range(n):
            nxt = epi_a(xsrc, tiles[i + 1], produce(i + 1)) if i + 1 < n else None
            epi_b(xdst, ctx)
            ctx = nxt

    def moe_sublayer(l, xsrc, xdst):
        b.push()
        load_mod(l, 1)
        TH = min(T, 1024)
        BIG = b.sb([128, 8, TH], BF16, "hTm")
        rw = b.sb([128, 8, 16], F32, "rw")
        b.dma('sync', rw, I['router_w'].re("(k p) e -> p k e", p=128))
        rb = b.sb([1, 16], F32, "rb")
        b.dma('sync', rb, I['router_b'])
        gatesT = b.sb([16, TH], F32, "gatesT")
        sel = b.sb([16, 16, 128], F32, "sel")
        b.memset('vector', sel, 0.0)
        for e in range(16):
            b.dma('sync', sel[e:e + 1, e, :], onesf[e:e + 1, :])
        yacc = b.sb([128, TH // 128, D], F32, "yacc")
        wg_pool = b.pool([128, 8, 512], BF16, 3, "wg")
        wu_pool = b.pool([128, 8, 512], BF16, 3, "wu")
        wd_pool = b.pool([128, 4, D], BF16, 3, "wd")
        s_pool = b.pool([128, 512], BF16, 2, "s")
        t_pool = b.pool([128, 512], BF16, 2, "tg")
        gbs_pool = b.pool([128, 512], F32, 2, "gbs")
        act_pool = b.pool([128, 4, 512], BF16, 3, "actT")
        rt = b.pool([128, 16], F32, 6, "rt")
        r4 = b.pool([128, 4], F32, 8, "r4")
        r1 = b.pool([128, 1], F32, 8, "r1")

        def router(hf, col):
            lg = bank(5)[:, 0:16]
            for kc in range(8):
                b.mm(lg, hf[:, kc, :], rw[:, kc, :], kc == 0, False)
            b.mm(lg, onesf[0:1, :], rb[0:1, :], False, True)
            mx = r1()
            b.red(mx, lg, ALU.max)
            nmx = r1()
            b.ts('vector', nmx, mx, -1.0, ALU.mult)
            ex = rt()
            b.act(ex, lg, AF.Exp, bias=nmx, scale=1.0)
            sm = r1()
            b.red(sm, ex, ALU.add)
            rs = r1()
            b.recip(rs, sm)
            p = rt()
            b.ts('vector', p, ex, rs, ALU.mult)
            pv = p.re("p (g j) -> p g j", j=4)
            prs = []
            for (i0, i1) in ((0, 1), (0, 2), (0, 3), (1, 2), (1, 3), (2, 3)):
                q = r4()
                b.tt('vector', q, pv[:, :, i0], pv[:, :, i1], ALU.add)
                prs.append(q)
            gs = prs[0]
            for q in prs[1:]:
                g2 = r4()
                b.tt('vector', g2, gs, q, ALU.max)
                gs = g2
            gm = r1()
            b.red(gm, gs, ALU.max)
            ing = r4()
            b.ts('vector', ing, gs, gm, ALU.is_equal)
            im1 = r4()
            b.ts('vector', im1, ing, -1.0, ALU.add)
            msk = rt()
            for g in range(4):
                b.ts('vector', msk[:, g * 4:(g + 1) * 4], p[:, g * 4:(g + 1) * 4], ing[:, g:g + 1], ALU.mult,
                     im1[:, g:g + 1], ALU.add)
            tp1 = r1()
            b.red(tp1, msk, ALU.max)
            is1 = rt()
            b.ts('vector', is1, msk, tp1, ALU.is_equal)
            m2 = rt()
            b.stt('vector', m2, is1, -2.0, msk, ALU.mult, ALU.add)
            tp2 = r1()
            b.red(tp2, m2, ALU.max)
            is2 = rt()
            b.ts('vector', is2, m2, tp2, ALU.is_equal)
            sl = rt()
            b.tt('vector', sl, is1, is2, ALU.add)
            den = r1()
            b.tt('vector', den, tp1, tp2, ALU.add)
            rden = r1()
            b.recip(rden, den)
            gt = rt()
            b.stt('vector', gt, p, rden, sl, ALU.mult, ALU.mult)
            gp = bank(5)[0:16, 128:256]
            b.tr(gp, gt, ident)
            b.cp('vector', gatesT[:, col:col + 128], gp)

        def load_w(e):
            wg, wu, wd = wg_pool(), wu_pool(), wd_pool()
            b.dma('gpsimd', wg, I['moe_w_gate'][l, e].re("(k p) f -> p k f", p=128))
            b.dma('gpsimd', wu, I['moe_w_up'][l, e].re("(k p) f -> p k f", p=128))
            b.dma('gpsimd', wd, I['moe_w_down'][l, e].re("(k p) n -> p k n", p=128))
            return wg, wu, wd

        ycnt = [0]
        for half in range(T // TH):
            tok0 = half * TH
            wnext = load_w(0)
            prologue(xsrc, tok0, TH // 128, BIG, 0, router=router)
            nch = TH // 512
            items = [(e, ch) for e in range(16) for ch in range(nch)]
            wcur = {}

            def stage1_begin(e, ch):
                c0 = ch * 512
                gbp = bank(4)
                b.mm(gbp, sel[:, e, :], gatesT[:, c0:c0 + 512], True, True)
                gbs = gbs_pool()
                b.cp('scalar', gbs, gbp)
                return gbs, act_pool()

            def stage1_fc(e, ch, wts, gbs, actT, fc):
                wg, wu, wd = wts
                c0 = ch * 512
                pg, pu = bank(2 * (fc % 2)), bank(2 * (fc % 2) + 1)
                for kc in range(8):
                    b.mm(pg, wg[:, kc, fc * 128:(fc + 1) * 128], BIG[:, kc, c0:c0 + 512], kc == 0, kc == 7)
                for kc in range(8):
                    b.mm(pu, wu[:, kc, fc * 128:(fc + 1) * 128], BIG[:, kc, c0:c0 + 512], kc == 0, kc == 7)
                s_ = s_pool()
                b.act(s_, pg, AF.Silu)
                tg = t_pool()
                b.tt('vector', tg, pu, gbs, ALU.mult)
                b.tt('gpsimd', actT[:, fc, :], s_, tg, ALU.mult)

            def stage2_tile(e, ch, wts, actT, ti):
                wd = wts[2]
                ycnt[0] += 1
                ya, yb = (PS[3][0], PS[3][1]) if ycnt[0] % 2 else (bank(4), bank(5))
                for nh, yp in ((0, ya), (1, yb)):
                    for fc in range(4):
                        b.mm(yp, actT[:, fc, ti * 128:(ti + 1) * 128], wd[:, fc, nh * 512:(nh + 1) * 512],
                             fc == 0, fc == 3)
                tix = ch * 4 + ti
                for nh, yp in ((0, ya), (1, yb)):
                    ys = yacc[:, tix, nh * 512:(nh + 1) * 512]
                    if e == 0:
                        b.cp('vector', ys, yp)
                    else:
                        b.tt('vector', ys, ys, yp, ALU.add)

            prev = None
            for (e, ch) in items:
                if ch == 0:
                    wcur[e] = wnext
                    if e + 1 < 16:
                        wnext = load_w(e + 1)
                gbs, actT = stage1_begin(e, ch)
                for fc in range(4):
                    stage1_fc(e, ch, wcur[e], gbs, actT, fc)
                    if prev is not None:
                        stage2_tile(*prev, fc)
                prev = (e, ch, wcur[e], actT)
            for ti in range(4):
                stage2_tile(*prev, ti)
            epilogue_seq(xsrc, xdst, [tok0 + i * 128 for i in range(TH // 128)], lambda i: yacc[:, i, :])
        b.pop()

    def load_wsec(pool, win, sec):
        w = pool()
        b.dma('gpsimd', w, win[:, sec * 512:(sec + 1) * 512].re("(k p) n -> p k n", p=128))
        return w

    def proj_heads64(BIG, w, dst, stage_pool):
        for g in range(8):
            stg = stage_pool()
            for ch in range(NCH):
                pq = bank(ch % 2)
                for kc in range(8):
                    b.mm(pq[0:64, :], w[:, kc, g * 64:(g + 1) * 64], BIG[:, kc, ch * 512:(ch + 1) * 512], kc == 0, kc == 7)
                b.cp('scalar' if ch % 2 else 'vector', stg[0:64, ch * 512:(ch + 1) * 512], pq[0:64, :])
            b.dma('sync', dst[g], stg[0:64, :])

    def proj_tok(BIG, w, dst, st_pool):
        for ti in range(NT):
            pv = bank(ti % 2)
            for kc in range(8):
                b.mm(pv, BIG[:, kc, ti * 128:(ti + 1) * 128], w[:, kc, :], kc == 0, kc == 7)
            st = st_pool()
            b.cp('scalar' if ti % 2 else 'vector', st, pv)
            b.dma('sync', dst[ti * 128:(ti + 1) * 128, :], st)

    def outproj_epilogue(BIG, wout, xsrc, xdst):
        wo = b.sb([128, 8, D], BF16, "wo")
        b.dma('gpsimd', wo, wout.re("(k p) n -> p k n", p=128))
        def produce(ti):
            ya, yb = PS[2 + ti % 2][0], PS[2 + ti % 2][1]
            for nh, yp in ((0, ya), (1, yb)):
                for kc in range(8):
                    b.mm(yp, BIG[:, kc, ti * 128:(ti + 1) * 128], wo[:, kc, nh * 512:(nh + 1) * 512], kc == 0, kc == 7)
            return (ya, yb)

        epilogue_seq(xsrc, xdst, [ti * 128 for ti in range(NT)], produce)

    def stop_at(tag):
        if STOP == tag:
            raise _Stop()

    def colvec(src_row, n):
        t = b.sb([128, n], F32, "colv")
        b.dma('sync', t, src_row.re("(c p) -> p c", p=128), slow=True)
        return t

    def odd_sublayer(l, xsrc, xdst):
        j = l // 2
        b.push()
        load_mod(l, 0)
        BIG = b.sb([128, 8, T], BF16, "BIGo")
        prologue(xsrc, 0, NT, BIG, 0)
        win = I['odd_w_in'][j]
        QS = dscr("QS", [8, 64, T], BF16)
        KS = dscr("KS", [8, 64, T], BF16)
        VS = dscr("VS", [T, 512], BF16)
        YR = dscr("YR", [512, T], BF16)
        b.push()
        wpool = b.pool([128, 8, 512], BF16, 2, "wsec")
        cw = b.sb([128, 4, 4], F32, "cw")
        for tap in range(4):
            b.dma('sync', cw[:, tap, :], I['conv_w'][j][tap].re("(c p) -> p c", p=128), slow=True)
        cbv = colvec(I['conv_b'][j], 4)
        bav = colvec(I['lru_ba'][j], 4)
        bxv = colvec(I['lru_bx'][j], 4)
        lamv = colvec(I['lru_lambda'][j], 4)
        e1 = b.sb([128, 4], F32, "e1")
        b.act(e1, lamv, AF.Exp, scale=-1.0)
        spl = b.sb([128, 4], F32, "spl")
        b.act(spl, e1, AF.Ln, bias=1.0)
        negc8 = b.sb([128, 4], F32, "negc8")
        b.ts('vector', negc8, spl, -8.0, ALU.mult)
        wa = b.sb([128, 4, 128], F32, "wa")
        b.dma('sync', wa, I['lru_wa'].re("c p q -> p c q"))
        wx = b.sb([128, 4, 128], F32, "wx")
        b.dma('sync', wx, I['lru_wx'].re("c p q -> p c q"))
        wxr = load_wsec(wpool, win, 0)
        wgr = load_wsec(wpool, win, 1)
        hlast = b.sb([128, 4], F32, "hlast")
        b.memset('vector', hlast, 0.0)
        xrp = [b.sb([128, 515], F32, "xrp") for _ in range(4)]
        for cc in range(4):
            b.memset('gpsimd', xrp[cc][:, 0:3], 0.0)
        f_pool = [b.pool([128, 512], F32, 2, "lf%d" % i) for i in range(6)]
        yr_pool = b.pool([128, 512], BF16, 2, "yr")
        for tc in range(NCH):
            tsl = slice(tc * 512, (tc + 1) * 512)
            for cc in range(4):
                px = bank(0)
                for kc in range(8):
                    b.mm(px, wxr[:, kc, cc * 128:(cc + 1) * 128], BIG[:, kc, tsl], kc == 0, kc == 7)
                xp = xrp[cc]
                b.cp('scalar', xp[:, 3:515], px)
                xc = f_pool[0]()
                b.ts('vector', xc, xp[:, 3:515], cw[:, 3, cc:cc + 1], ALU.mult, cbv[:, cc:cc + 1], ALU.add)
                for tap in (2, 1, 0):
                    b.stt('vector', xc, xp[:, tap:tap + 512], cw[:, tap, cc:cc + 1], xc, ALU.mult, ALU.add)
                hal = small()
                b.cp('gpsimd', hal[:, 0:3], xp[:, 512:515])
                b.cp('gpsimd', xp[:, 0:3], hal[:, 0:3])
                pr, pi = bank(1), bank(2)
                b.mm(pr, wa[:, cc, :], xc)
                b.mm(pi, wx[:, cc, :], xc)
                r = f_pool[1]()
                b.act(r, pr, AF.Sigmoid, bias=bav[:, cc:cc + 1])
                ig = f_pool[2]()
                b.act(ig, pi, AF.Sigmoid, bias=bxv[:, cc:cc + 1])
                a = f_pool[3]()
                b.act(a, r, AF.Exp, scale=negc8[:, cc:cc + 1])
                om = r
                b.tt('gpsimd', om, a, a, ALU.mult)
                b.ts('vector', om, om, -1.0, ALU.mult, 1.0, ALU.add)
                b.ts('vector', om, om, 1e-12, ALU.max)
                b.act(om, om, AF.Sqrt)
                b.tt('gpsimd', ig, ig, xc, ALU.mult)
                u = f_pool[4]()
                b.tt('vector', u, ig, om, ALU.mult)
                hh = f_pool[5]()
                b.scan(hh, a, u, 0.0 if tc == 0 else hlast[:, cc:cc + 1])
                b.cp('vector', hlast[:, cc:cc + 1], hh[:, 511:512])
                pg = bank(3)
                for kc in range(8):
                    b.mm(pg, wgr[:, kc, cc * 128:(cc + 1) * 128], BIG[:, kc, tsl], kc == 0, kc == 7)
                g = xc
                b.cp('scalar', g, pg)
                g2 = ig
                b.tt('gpsimd', g2, g, g, ALU.mult)
                b.ts('vector', g2, g2, 0.044715, ALU.mult, 1.0, ALU.add)
                b.tt('gpsimd', g2, g2, g, ALU.mult)
                b.act(g2, g2, AF.Sigmoid, scale=1.5957691216057308)
                b.tt('gpsimd', g2, g2, g, ALU.mult)
                yr = yr_pool()
                b.tt('vector', yr, g2, hh, ALU.mult)
                b.dma('sync', YR[cc * 128:(cc + 1) * 128, tsl], yr)
        stop_at('oA1')
        stage_pool = b.pool([64, T], BF16, 2, "stg")
        w = load_wsec(wpool, win, 2)
        proj_heads64(BIG, w, QS, stage_pool)
        w = load_wsec(wpool, win, 3)
        proj_heads64(BIG, w, KS, stage_pool)
        w = load_wsec(wpool, win, 4)
        proj_tok(BIG, w, VS, b.pool([128, 512], BF16, 2, "vst"))
        b.pop()
        stop_at('oA2')
        b.push()
        for cc in range(4):
            b.dma('sync', BIG[:, cc, :], YR[cc * 128:(cc + 1) * 128, :])
        tge = b.sb([128, 128], F32, "tge")
        b.tt('vector', tge, csq[:, 128:256], csq[:, 0:128], ALU.add)
        triGEb = b.sb([128, 128], BF16, "triGEb")
        b.cp('vector', triGEb, tge)
        zer = b.sb([128, 512], F32, "zer")
        b.memset('vector', zer, 0.0)
        mkS = b.sb([128, 4, 512], BF16, "mkS")
        b.dma('gpsimd', mkS, c_mask[4:8].re("n p q -> p n q"))
        q_pool = b.pool([64, T], BF16, 2, "qT")
        k_pool = b.pool([64, T], BF16, 2, "kT")
        v_pool = b.pool([128, NT, 64], BF16, 2, "vS")
        e_pool = b.pool([128, 512], F32, 3, "e")
        sp_pool = b.pool([128, 512], F32, 3, "sp")
        spb_pool = b.pool([128, 512], BF16, 4, "spb")
        arg_pool = b.pool([128, 512], F32, 4, "arg")
        p_pool = b.pool([128, 512], BF16, 3, "P")
        sfx_pool = b.pool([128, 512], F32, 2, "sfx")
        ost_pool = b.pool([64, 512], BF16, 2, "ost")
        VSv = VS.re("(n p) c -> p n c", p=128)
        blk = [0]
        for h in range(8):
            qT, kT, vv = q_pool(), k_pool(), v_pool()
            b.dma('sync', qT, QS[h])
            b.dma('sync', kT, KS[h])
            b.dma('sync', vv, VSv[:, :, h * 64:(h + 1) * 64])
            for Q in range(NCH):
                qs = slice(Q * 512, (Q + 1) * 512)
                blk[0] += 1
                OT = bank(6 + blk[0] % 2)[0:64, :]
                sfx = sfx_pool()
                Js = list(reversed(range(4 * Q + 4)))
                last = len(Js) - 1
                st_ = {}
                for k in range(-2, last + 1):
                    ia, ib, ic = k + 2, k + 1, k
                    if ia <= last:
                        zt = bank(ia % 2)
                        J = Js[ia]
                        b.mm(zt, kT[:, J * 128:(J + 1) * 128], qT[:, qs])
                        st_[ia] = dict(zt=zt, r=J - 4 * Q, J=J)
                    if ic >= 0:
                        c_ = st_[ic]
                        P = p_pool()
                        b.act(P, c_['arg'], AF.Exp)
                        if c_['r'] >= 0:
                            b.tt('gpsimd', P, P, mkS[:, c_['r'], :], ALU.mult)
                        c_['P'] = P
                    if ia <= last:
                        a_ = st_[ia]
                        e = e_pool()
                        b.act(e, a_['zt'], AF.Exp, scale=0.125)
                        spb = spb_pool()
                        b.act(spb, e, AF.Ln, bias=1.0)
                        if a_['r'] >= 0:
                            b.tt('gpsimd', spb, spb, mkS[:, a_['r'], :], ALU.mult)
                        a_['e'], a_['spb'] = e, spb
                    if 0 <= ib <= last:
                        b_ = st_[ib]
                        ge = bank(2 + ib % 2)
                        b.mm(ge, triGEb, b_['spb'])
                        bt = None
                        if ib < last:
                            bt = bank(4 + ib % 2)
                            b.mm(bt, onesb, b_['spb'])
                        arg = arg_pool()
                        b.stt('vector', arg, b_['zt'], 0.125, zer if ib == 0 else sfx, ALU.mult, ALU.subtract,
                              xr=(b_['e'],))
                        b.tt('vector', arg, arg, ge, ALU.subtract)
                        if ib < last:
                            if ib == 0:
                                b.cp('vector', sfx, bt)
                            else:
                                b.tt('vector', sfx, sfx, bt, ALU.add)
                        b_['arg'] = arg
                    if ic >= 0:
                        c_ = st_[ic]
                        b.mm(OT, vv[:, c_['J'], :], c_['P'], ic == 0, ic == last)
                        del st_[ic]
                if h % 2 == 0:
                    b.cp('vector', BIG[0:64, 4 + h // 2, qs], OT)
                else:
                    ost = ost_pool()
                    b.cp('vector', ost, OT)
                    b.dma('sync', BIG[64:128, 4 + h // 2, qs], ost)
        b.pop()
        stop_at('oB')
        b.push()
        outproj_epilogue(BIG, I['odd_w_out'][j], xsrc, xdst)
        b.pop()
        b.pop()

    def even_sublayer(l, xsrc, xdst):
        import math
        j = l // 2
        lam_init = 0.8 - 0.6 * math.exp(-0.3 * l)
        b.push()
        load_mod(l, 0)
        BIG = b.sb([128, 8, T], BF16, "BIGe")
        prologue(xsrc, 0, NT, BIG, 0)
        win = I['even_w_in'][j]
        QA = dscr("QA", [8, 64, T], BF16)
        KA = dscr("KA", [8, 64, T], BF16)
        VA = dscr("VA", [T, 512], BF16)
        QST = dscr("QST", [512, T], F32)
        KTs = dscr("KTs", [512, T], F32)
        SGT = dscr("SGT", [512, T], BF16)
        LF = dscr("LF", [T, 512], F32)
        KTOK = dscr("KTOK", [T, 512], F32)
        VH = dscr("VH", [T, 512], BF16)
        lbscr = dscr("lbscr", [2, 512], F32)
        gm = b.sb([1, 3, 512], F32, "gm")
        b.dma('sync', gm, I['hgrn_gamma'].re("(o a) c -> o a c", o=1))
        eg = b.sb([1, 3, 512], F32, "eg")
        b.act(eg, gm, AF.Exp)
        den = b.sb([1, 512], F32, "den")
        b.tt('vector', den, eg[:, 0, :], eg[:, 1, :], ALU.add)
        b.tt('vector', den, den, eg[:, 2, :], ALU.add)
        num = b.sb([1, 512], F32, "num")
        b.cp('vector', num, eg[:, 0, :])
        for i in range(1, l + 1):
            b.tt('vector', num, num, eg[:, i, :], ALU.add)
        rden = b.sb([1, 512], F32, "rden")
        b.recip(rden, den)
        lbr = b.sb([1, 2, 512], F32, "lbr")
        b.tt('vector', lbr[:, 0, :], num, rden, ALU.mult)
        b.ts('vector', lbr[:, 1, :], lbr[:, 0, :], -1.0, ALU.mult, 1.0, ALU.add)
        b.dma('sync', lbscr.re("(o a) c -> o a c", o=1), lbr)
        omlF = colvec(lbscr[1], 4)
        lbB = b.sb([128, 512], F32, "lbB")
        b.dma('sync', lbB, lbscr[0].pb())
        omlB = b.sb([128, 512], F32, "omlB")
        b.dma('sync', omlB, lbscr[1].pb())
        dl = b.sb([128, 256], F32, "dl")
        b.dma('sync', dl, I['diff_lambda'][j].re("a d -> (a d)").pb())
        lw = b.sb([128, 128], F32, "lw")
        lsm = b.sb([128, 8], F32, "lsm")
        b.tt('vector', lw[:, 0:64], dl[:, 0:64], dl[:, 64:128], ALU.mult)
        b.tt('vector', lw[:, 64:128], dl[:, 128:192], dl[:, 192:256], ALU.mult)
        b.red(lsm[:, 0:1], lw[:, 0:64], ALU.add)
        b.red(lsm[:, 1:2], lw[:, 64:128], ALU.add)
        b.act(lsm[:, 2:4], lsm[:, 0:2], AF.Exp)
        b.tt('vector', lsm[:, 4:5], lsm[:, 3:4], lsm[:, 2:3], ALU.subtract)
        b.ts('vector', lsm[:, 5:6], lsm[:, 4:5], -lam_init, ALU.add)
        neglam = lsm[:, 5:6]
        gv = b.sb([128, 4], F32, "gv")
        b.dma('sync', gv[:, 0:1], I['diff_gain'][j].re("(p o) -> p o", o=1), slow=True)
        b.dma('sync', gv[:, 1:2], I['hgrn_gain'][j].re("(p o) -> p o", o=1), slow=True)
        b.ts('vector', gv[:, 2:3], gv[:, 0:1], math.sqrt(128.0) * (1.0 - lam_init), ALU.mult)
        b.ts('vector', gv[:, 3:4], gv[:, 1:2], math.sqrt(128.0), ALU.mult)
        dgS, hgS = gv[:, 2:3], gv[:, 3:4]
        stop_at('e0')
        b.push()
        wpool = b.pool([128, 8, 512], BF16, 2, "wsec")
        stage_pool = b.pool([64, T], BF16, 2, "stg")
        w = load_wsec(wpool, win, 0)
        proj_heads64(BIG, w, QA, stage_pool)
        w = load_wsec(wpool, win, 1)
        proj_heads64(BIG, w, KA, stage_pool)
        st_pool = b.pool([128, 512], BF16, 2, "vst")
        w = load_wsec(wpool, win, 2)
        proj_tok(BIG, w, VA, st_pool)
        w = load_wsec(wpool, win, 5)
        proj_tok(BIG, w, VH, st_pool)
        fst_pool = b.pool([128, 512], F32, 3, "fst")
        bst_pool = b.pool([128, 512], BF16, 2, "bst")
        w = load_wsec(wpool, win, 3)
        for h in range(4):
            for ch in range(NCH):
                pq = bank(ch % 2)
                for kc in range(8):
                    b.mm(pq, w[:, kc, h * 128:(h + 1) * 128], BIG[:, kc, ch * 512:(ch + 1) * 512], kc == 0, kc == 7)
                st = fst_pool()
                b.act(st, pq, AF.Silu)
                b.dma('sync', QST[h * 128:(h + 1) * 128, ch * 512:(ch + 1) * 512], st)
        w = load_wsec(wpool, win, 6)
        for h in range(4):
            for ch in range(NCH):
                pq = bank(ch % 2)
                for kc in range(8):
                    b.mm(pq, w[:, kc, h * 128:(h + 1) * 128], BIG[:, kc, ch * 512:(ch + 1) * 512], kc == 0, kc == 7)
                st = bst_pool()
                b.act(st, pq, AF.Silu)
                b.dma('sync', SGT[h * 128:(h + 1) * 128, ch * 512:(ch + 1) * 512], st)
        w = load_wsec(wpool, win, 4)
        for h in range(4):
            for ch in range(NCH):
                pq = bank(ch % 2)
                for kc in range(8):
                    b.mm(pq, w[:, kc, h * 128:(h + 1) * 128], BIG[:, kc, ch * 512:(ch + 1) * 512], kc == 0, kc == 7)
                sg = fst_pool()
                b.act(sg, pq, AF.Sigmoid, scale=-1.0)
                st = fst_pool()
                b.ts('vector', st, sg, omlF[:, h:h + 1], ALU.mult)
                b.dma('sync', KTs[h * 128:(h + 1) * 128, ch * 512:(ch + 1) * 512], st)
        for ti in range(NT):
            pv = bank(2 + ti % 2)
            for kc in range(8):
                b.mm(pv, BIG[:, kc, ti * 128:(ti + 1) * 128], w[:, kc, :], kc == 0, kc == 7)
            sg = fst_pool()
            b.act(sg, pv, AF.Sigmoid)
            tq = fst_pool()
            b.tt('vector', tq, sg, omlB, ALU.mult)
            b.tt('gpsimd', sg, tq, lbB, ALU.add)
            lf = fst_pool()
            b.act(lf, sg, AF.Ln)
            b.dma('sync', LF[ti * 128:(ti + 1) * 128, :], lf)
            b.tt('gpsimd', sg, omlB, tq, ALU.subtract)
            b.dma('sync', KTOK[ti * 128:(ti + 1) * 128, :], sg)
        b.pop()
        stop_at('eA')
        b.push()
        mkI = b.sb([128, 4, 512], BF16, "mkI")
        b.dma('gpsimd', mkI, c_mask[0:4].re("n p q -> p n q"))
        qT = b.sb([68, 2, T], BF16, "qTa")
        kT = b.sb([68, 2, T], BF16, "kTa")
        vv = b.sb([128, NT, 128], BF16, "vA")
        qTg = V(qT.ap, Res("qaug"))
        kTg = V(kT.ap, Res("kaug"))
        p_pool = b.pool([128, 512], BF16, 5, "Pa")
        rl_pool = b.pool([128, 512], F32, 4, "rl")
        oc_pool = b.pool([128, 512], F32, 4, "oc")
        VAv = VA.re("(n p) c -> p n c", p=128)
        blk = [0]
        for h in range(4):
            for c in range(2):
                b.dma('sync', qT[0:64, c, :], QA[2 * h + c])
                b.dma('sync', kT[0:64, c, :], KA[2 * h + c])
                b.dma('gpsimd', qTg[64:68, c, :], c_aug[h, 0])
                b.dma('gpsimd', kTg[64:68, c, :], c_aug[h, 1])
            b.dma('sync', vv, VAv[:, :, h * 128:(h + 1) * 128])
            for Q in range(NCH):
                qs = slice(Q * 512, (Q + 1) * 512)
                oc = []
                for c in range(2):
                    blk[0] += 1
                    OT, LT = bank(2 + blk[0] % 2), bank(4 + blk[0] % 2)
                    nJ = 4 * Q + 4

                    def s1(J):
                        r = J - 4 * Q
                        st = bank(J % 2)
                        b.mm(st, kT[0:68, c, J * 128:(J + 1) * 128], qT[0:68, c, qs], xr=(qTg, kTg))
                        P = p_pool()
                        if r >= 0:
                            cl = rl_pool()
                            b.ts('vector', cl, st, 0.125, ALU.mult, 80.0, ALU.min)
                            b.act(P, cl, AF.Exp)
                            b.tt('gpsimd', P, P, mkI[:, r, :], ALU.mult)
                        else:
                            b.act(P, st, AF.Exp, scale=0.125)
                        return P

                    Ps = {}
                    for J in range(-2, nJ):
                        if J + 2 < nJ:
                            Ps[J + 2] = s1(J + 2)
                        if J >= 0:
                            P = Ps.pop(J)
                            b.mm(OT, vv[:, J, :], P, J == 0, J == nJ - 1)
                            b.mm(LT, onesb, P, J == 0, J == nJ - 1)
                    rl = rl_pool()
                    b.recip(rl, LT)
                    o_c = oc_pool()
                    b.tt('vector', o_c, OT, rl, ALU.mult)
                    oc.append(o_c)
                o = oc[0]
                b.stt('vector', o, oc[1], neglam, oc[0], ALU.mult, ALU.add)
                sq = oc[1]
                b.tt('gpsimd', sq, o, o, ALU.mult)
                ss = bank(6)
                b.mm(ss, onesf, sq)
                rstd = rl_pool()
                b.rsqrt(rstd, ss, 128e-6, rstd)
                b.stt('vector', BIG[:, h, qs], o, dgS, rstd, ALU.mult, ALU.mult)
        b.pop()
        stop_at('eB')
        b.push()
        hgGT = csq[:, 256:512]
        hgU = csq[:, 512:640]
        tri4 = b.sb([128, 4, 128], F32, "tri4")
        for h in range(4):
            b.cp('gpsimd', tri4[:, h, :], csq[:, 384:512])
        S0 = [b.sb([128, 128], F32, "S0") for _ in range(4)]
        S1 = [b.sb([128, 128], F32, "S1") for _ in range(4)]
        Sb0 = [b.sb([128, 128], BF16, "Sb0") for _ in range(4)]
        Sb1 = [b.sb([128, 128], BF16, "Sb1") for _ in range(4)]
        for h in range(4):
            b.memset('vector', S0[h], 0.0)
            b.memset('gpsimd', Sb0[h], 0.0)
        lf_pool = b.pool([128, 512], F32, 2, "LFt")
        kt_pool = b.pool([128, 512], F32, 2, "Kt")
        vt_pool = b.pool([128, 512], BF16, 2, "Vt")
        qs_pool = b.pool([128, 4, 128], F32, 2, "QsT")
        ktt_pool = b.pool([128, 4, 128], F32, 2, "KTt")
        sg_pool = b.pool([128, 4, 128], BF16, 2, "SGt")
        E_pool = [b.pool([128, 4, 128], F32, 2, "E%d" % i) for i in range(3)]
        E3_pool = b.pool([128, 512], F32, 2, "E3")
        bf_pool = [b.pool([128, 4, 128], BF16, 2, "hb%d" % i) for i in range(3)]
        kd_pool = b.pool([128, 512], BF16, 4, "kd")
        at_pool = b.pool([128, 512], BF16, 2, "attm")
        sq_pool = b.pool([128, 512], F32, 2, "hsq")
        QSTv = QST.re("(h d) t -> d h t", d=128)
        KTv = KTs.re("(h d) t -> d h t", d=128)
        SGTv = SGT.re("(h d) t -> d h t", d=128)
        for ti in range(NT):
            ts_ = slice(ti * 128, (ti + 1) * 128)
            LFt, Kt, Vt = lf_pool(), kt_pool(), vt_pool()
            QsT, KTt, SGt = qs_pool(), ktt_pool(), sg_pool()
            b.dma('sync', LFt, LF[ts_, :])
            b.dma('sync', Kt, KTOK[ts_, :])
            b.dma('sync', Vt, VH[ts_, :])
            b.dma('sync', QsT, QSTv[:, :, ts_])
            b.dma('sync', KTt, KTv[:, :, ts_])
            b.dma('sync', SGt, SGTv[:, :, ts_])
            for h in range(4):
                b.mm(bank(h // 2)[:, (h % 2) * 256:(h % 2 + 1) * 256], LFt[:, h * 128:(h + 1) * 128], hgGT)
            pbu = bank(2)
            b.mm(pbu, hgU, LFt)
            E1, E1i, E2 = E_pool[0](), E_pool[1](), E_pool[2]()
            for hf2 in range(2):
                pv = bank(hf2).re("p (h a c) -> p h a c", h=2, a=2)
                hs = slice(2 * hf2, 2 * hf2 + 2)
                b.act(E1[:, hs, :], pv[:, :, 0, :], AF.Exp)
                b.act(E1i[:, hs, :], pv[:, :, 0, :], AF.Exp, scale=-1.0)
                b.act(E2[:, hs, :], pv[:, :, 1, :], AF.Exp)
            stop_at('c0')
            E3 = E3_pool()
            b.act(E3, pbu, AF.Exp)
            qbT, kbT, qeT = bf_pool[0](), bf_pool[1](), bf_pool[2]()
            b.tt('vector', qbT, QsT, E1, ALU.mult)
            b.tt('gpsimd', kbT, KTt, E1i, ALU.mult)
            b.tt('vector', qeT, QsT, E2, ALU.mult)
            kdA, kdB = kd_pool(), kd_pool()
            b.stt('vector', kdA, Kt, csq[:, 447:448], E3, ALU.mult, ALU.mult)
            b.stt('vector', kdB, Kt, csq[:, 511:512], E3, ALU.mult, ALU.mult)
            stop_at('c1')
            patt = bank(3)
            for h in range(4):
                b.mm(patt[:, h * 128:(h + 1) * 128], kbT[:, h, :], qbT[:, h, :])
            attm = at_pool()
            b.tt('vector', attm, patt, tri4.re("p h c -> p (h c)"), ALU.mult)
            for h in range(4):
                for chk in range(2):
                    reg = bank(4 + h // 2)[:, ((h % 2) * 2 + chk) * 128:((h % 2) * 2 + chk + 1) * 128]
                    b.mm(reg, (kdA, kdB)[chk][:, h * 128:(h + 1) * 128], Vt[:, h * 128:(h + 1) * 128])
            stop_at('c2')
            po = bank(6)
            for h in range(4):
                hc = slice(h * 128, (h + 1) * 128)
                ca = slice(h * 128, h * 128 + 64)
                cb_ = slice(h * 128 + 64, h * 128 + 128)
                b.mm(po[:, ca], Vt[:, hc], attm[:, ca], True, False)
                b.mm(po[:, ca], Sb0[h], qeT[:, h, 0:64], False, True)
                dSa = bank(4 + h // 2)[:, ((h % 2) * 2) * 128:((h % 2) * 2 + 1) * 128]
                dSb = bank(4 + h // 2)[:, ((h % 2) * 2 + 1) * 128:((h % 2) * 2 + 2) * 128]
                b.stt('vector', S1[h], S0[h], E2[:, h, 63:64], dSa, ALU.mult, ALU.add)
                b.cp('gpsimd', Sb1[h], S1[h])
                b.mm(po[:, cb_], Vt[:, hc], attm[:, cb_], True, False)
                b.mm(po[:, cb_], Sb1[h], qeT[:, h, 64:128], False, True)
                b.stt('vector', S0[h], S1[h], E2[:, h, 127:128], dSb, ALU.mult, ALU.add)
                b.cp('gpsimd', Sb0[h], S0[h])
            stop_at('c3')
            sq = sq_pool()
            b.act(sq, po, AF.Square)
            pss = bank(7)
            b.mm(pss, onesf, sq)
            rstd = sq_pool()
            b.rsqrt(rstd, pss, 128e-6, rstd)
            b.stt('vector', sq, po, hgS, rstd, ALU.mult, ALU.mult)
            b.tt('gpsimd', BIG[:, 4:8, ts_], sq.re("p (h c) -> p h c", h=4), SGt, ALU.mult)
        b.pop()
        b.push()
        outproj_epilogue(BIG, I['even_w_out'][j], xsrc, xdst)
        b.pop()
        b.pop()

    cur = x_in
    for si, name in enumerate(plan):
        dst = out if si == len(plan) - 1 else xs[si % 2]
        l = int(name[-1])
        try:
            if name.startswith('moe'):
                moe_sublayer(l, cur, dst)
            elif name.startswith('even'):
                even_sublayer(l, cur, dst)
            elif name.startswith('odd'):
                odd_sublayer(l, cur, dst)
        except _Stop:
            break
        cur = dst
    b.P.barrier()
    b.P.emit()
    return nc, b


def prep_inputs(inputs, T):
    f = lambda a: np.ascontiguousarray(np.asarray(a, dtype=np.float32))
    shared = {}
    for k in IN_SHAPES:
        a = f(inputs[k])
        if k in ('lru_wa', 'lru_wx'):
            bd = np.zeros((4, 128, 128), np.float32)
            for g in range(8):
                bd[g // 2, (g % 2) * 64:(g % 2 + 1) * 64, (g % 2) * 64:(g % 2 + 1) * 64] = a[0, g]
            a = bd
        elif k == 'router_b':
            a = a.reshape(1, 16)
        shared[k] = a
    shared.update(host_consts(T))
    return shared


def kernel(**inputs):
    x = np.asarray(inputs['x'], dtype=np.float32)
    c = np.asarray(inputs['c'], dtype=np.float32)
    Bn, T, _ = x.shape
    shared = prep_inputs(inputs, T)
    nc, _ = build(T, ['even0', 'moe0', 'odd1', 'moe1'])
    in_maps = []
    for i in range(Bn):
        m = dict(shared)
        m['x'] = np.ascontiguousarray(x[i])
        m['c'] = np.ascontiguousarray(c[i:i + 1])
        in_maps.append(m)
    res = run_bass_kernel_spmd(nc, in_maps, core_ids=list(range(Bn)))
    return np.stack([np.asarray(r['out'], dtype=np.float32) for r in res.results], axis=0)
```

```python
import contextlib
import numpy as np
import concourse.bass as bass
import concourse.mybir as mybir
from concourse.bass_utils import run_bass_kernel_spmd

F32 = mybir.dt.float32
BF16 = mybir.dt.bfloat16
AF = mybir.ActivationFunctionType
ALU = mybir.AluOpType
AX = mybir.AxisListType

ENGS = ['tensor', 'vector', 'scalar', 'gpsimd', 'sync']
SEM_LIMIT = 30000
D = 1024
ALPHA = 4.0 ** 0.25
N_CORES = 8


class Res:
    __slots__ = ('name', 'last_w', 'readers', 'ds')

    def __init__(self, name):
        self.name = name
        self.last_w = None
        self.readers = []
        self.ds = {}


class DSem:
    __slots__ = ('name', 'dcount', 'dsem', 'kind')

    def __init__(self, name, kind):
        self.name = name
        self.dcount = 0
        self.dsem = None
        self.kind = kind


class Prog:
    def __init__(self, nc):
        self.nc = nc
        self.ops = {e: [] for e in ENGS}
        self.seen = {x: {e: -1 for e in ENGS} for x in ENGS}
        self.seen_d = {x: {} for x in ENGS}
        self.dma_res = []
        self.free_ds = {'sw': [], 'hw': []}
        self.all_dma_toks = []
        self.stack = contextlib.ExitStack()
        self.pending = {x: [] for x in ENGS}
        self.scopes = []

    def sbuf(self, name, shape, dtype):
        st = self.scopes[-1] if self.scopes else self.stack
        return st.enter_context(self.nc.sbuf_tensor(name, list(shape), dtype))

    def push(self):
        self.scopes.append(contextlib.ExitStack())

    def pop(self):
        self.scopes.pop().close()

    def psum(self, name, shape, dtype):
        return self.stack.enter_context(self.nc.psum_tensor(name, list(shape), dtype))

    def _need(self, eng, tok, waits):
        if tok is None:
            return
        if tok[0] == 'e':
            _, e, idx = tok
            if e == eng and eng == 'tensor':
                return
            if self.seen[eng][e] >= idx:
                return
            self.seen[eng][e] = idx
            waits.append(tok)
        else:
            _, r, val = tok
            if self.seen_d[eng].get(id(r), 0) >= val:
                return
            self.seen_d[eng][id(r)] = val
            waits.append(tok)

    def op(self, eng, fn, reads=(), writes=(), dma_res=None):
        waits = []
        for r in reads:
            self._need(eng, r.last_w, waits)
        for r in writes:
            self._need(eng, r.last_w, waits)
            for t in r.readers:
                self._need(eng, t, waits)
        for t in self.pending[eng]:
            self._need(eng, t, waits)
        self.pending[eng] = []
        idx = len(self.ops[eng])
        if dma_res is not None:
            kind = 'sw' if eng == 'gpsimd' else 'hw'
            ds = dma_res.ds.get(kind)
            if ds is None:
                fl = self.free_ds[kind]
                if fl:
                    ds = fl.pop()
                else:
                    ds = DSem("%s%d" % (kind, len(self.dma_res)), kind)
                    self.dma_res.append(ds)
                dma_res.ds[kind] = ds
            dma_res = ds
            ds.dcount += 16
            tok = ('d', ds, ds.dcount)
            self.all_dma_toks.append(tok)
        else:
            tok = ('e', eng, idx)
        import sys
        fr = sys._getframe(2)
        site = "%s:%d<%s:%d" % (fr.f_code.co_name, fr.f_lineno, fr.f_back.f_code.co_name, fr.f_back.f_lineno)
        self.ops[eng].append(dict(fn=fn, waits=waits, dma_res=dma_res, site=site,
                                  dval=(dma_res.dcount if dma_res else 0), mile=False))
        for r in writes:
            r.last_w = tok
            r.readers = []
        for r in reads:
            if r not in writes:
                r.readers.append(tok)
        return tok

    def barrier(self):
        toks = []
        for e in ENGS:
            for idx in range(len(self.ops[e]) - 1, -1, -1):
                if self.ops[e][idx]['dma_res'] is None:
                    toks.append(('e', e, idx))
                    break
        dtoks = {}
        for t in self.all_dma_toks:
            dtoks[id(t[1])] = t
        self.all_dma_toks = []
        for x in ENGS:
            self.pending[x].extend(list(toks) + list(dtoks.values()))

    def emit(self):
        nc = self.nc
        self.tail = {}
        for x in ENGS:
            w = []
            for t in self.pending[x]:
                self._need(x, t, w)
            self.tail[x] = w
            for t in w:
                if t[0] == 'e':
                    self.ops[t[1]][t[2]]['mile'] = True
        for x in ENGS:
            for o in self.ops[x]:
                for t in o['waits']:
                    if t[0] == 'e':
                        self.ops[t[1]][t[2]]['mile'] = True
        nsem = 0
        for x in ENGS:
            cnt = 0
            cur = None
            for o in self.ops[x]:
                if o['mile'] and o['dma_res'] is None:
                    if cur is None or cnt >= SEM_LIMIT:
                        cur = self.stack.enter_context(nc.semaphore(f"m_{x}_{nsem}"))
                        nsem += 1
                        cnt = 0
                    cnt += 1
                    o['msem'] = (cur, cnt)
        per16 = (SEM_LIMIT // 16) * 16
        for r in self.dma_res:
            n = r.dcount
            r.dsem = []
            for i in range((n + per16 - 1) // per16):
                r.dsem.append(self.stack.enter_context(nc.semaphore(f"d_{r.name}_{i}")))
                nsem += 1
        self.nsem = nsem

        def dsem_of(r, val):
            k = (val - 16) // per16
            return r.dsem[k], val - k * per16

        ops = self.ops
        self.imap = {}
        ctr = [0]

        def body(x):
            def f(eng):
                for o in ops[x]:
                    self.imap[ctr[0] + len(o['waits'])] = (x, o['site'])
                    ctr[0] += len(o['waits']) + 1
                    for t in o['waits']:
                        if t[0] == 'e':
                            s, v = ops[t[1]][t[2]]['msem']
                        else:
                            s, v = dsem_of(t[1], t[2])
                        eng.wait_ge(s, v)
                    inst = o['fn'](eng)
                    if o['dma_res'] is not None:
                        s, v = dsem_of(o['dma_res'], o['dval'])
                        inst.then_inc(s, 16)
                    elif o['mile']:
                        s, v = o['msem']
                        inst.then_inc(s, 1)
                for t in self.tail[x]:
                    if t[0] == 'e':
                        s, v = ops[t[1]][t[2]]['msem']
                    else:
                        s, v = dsem_of(t[1], t[2])
                    eng.wait_ge(s, v)
            return f

        with nc.Block() as block:
            block.tensor(body('tensor'))
            block.vector(body('vector'))
            block.scalar(body('scalar'))
            block.gpsimd(body('gpsimd'))
            block.sync(body('sync'))
        self.stack.close()


class V:
    __slots__ = ('ap', 'r')

    def __init__(self, ap, r):
        self.ap = ap
        self.r = r

    def __getitem__(self, k):
        return V(self.ap[k], self.r)

    def re(self, s, **kw):
        return V(self.ap.rearrange(s, **kw), self.r)

    def pb(self, n=128):
        return V(self.ap.partition_broadcast(n), self.r)


def _rs(xs):
    out = []
    for x in xs:
        if isinstance(x, V) and x.r not in out:
            out.append(x.r)
    return out


def _a(x):
    return x.ap if isinstance(x, V) else x


class Bld:
    ARENA_WORDS = 52992

    def __init__(self, nc):
        self.nc = nc
        self.P = Prog(nc)
        self.n = 0
        self.arena = self.P.stack.enter_context(nc.sbuf_tensor("arena", [128, self.ARENA_WORDS], F32))
        self.off = 0
        self.marks = []
        self.peak = 0
        self.scope_res = [[]]

    def push(self):
        self.marks.append(self.off)
        self.scope_res.append([])

    def pop(self):
        self.off = self.marks.pop()
        self.P.barrier()
        for r in self.scope_res.pop():
            for kind, ds in r.ds.items():
                self.P.free_ds[kind].append(ds)
            r.ds = {}

    def sb(self, shape, dt, name=None):
        self.n += 1
        name = f"{name or 't'}_{self.n}"
        nel = int(np.prod(shape[1:]))
        nbytes = nel * (2 if dt == BF16 else 4)
        words = (nbytes + 63) // 64 * 16
        assert self.off + words <= self.ARENA_WORDS, f"SBUF arena overflow at {name}: {self.off * 4} + {words * 4}"
        ap = self.arena[0:shape[0], self.off:self.off + (nbytes + 3) // 4]
        self.off += words
        self.peak = max(self.peak, self.off * 4)
        if dt == BF16:
            ap = ap.bitcast(BF16)
            if nel % 2:
                ap = ap[:, 0:nel]
        if len(shape) == 3:
            ap = ap.rearrange("p (a b) -> p a b", a=shape[1])
        elif len(shape) == 4:
            ap = ap.rearrange("p (a b c) -> p a b c", a=shape[1], b=shape[2])
        r = Res(name)
        self.scope_res[-1].append(r)
        return V(ap, r)

    def pool(self, shape, dt, n, name=None):
        tiles = [self.sb(shape, dt, name) for _ in range(n)]
        st = [0]

        def nxt():
            st[0] += 1
            return tiles[(st[0] - 1) % n]
        return nxt

    def act(self, out, in_, func, bias=None, scale=None):
        kw = {}
        if bias is not None:
            kw['bias'] = _a(bias)
        if scale is not None:
            kw['scale'] = _a(scale)
        o, i = out.ap, in_.ap
        self.P.op('scalar', lambda e: e.activation(out=o, in_=i, func=func, **kw),
                  reads=_rs([in_, bias, scale]), writes=[out.r])

    def tt(self, eng, out, a, b, op):
        o, x, y = out.ap, a.ap, b.ap
        self.P.op(eng, lambda e: e.tensor_tensor(out=o, in0=x, in1=y, op=op), reads=_rs([a, b]), writes=[out.r])

    def ts(self, eng, out, a, s1, op0, s2=None, op1=None):
        o, x, p, q = out.ap, a.ap, _a(s1), _a(s2)
        if s2 is None:
            fn = lambda e: e.tensor_scalar(out=o, in0=x, scalar1=p, scalar2=None, op0=op0)
        else:
            fn = lambda e: e.tensor_scalar(out=o, in0=x, scalar1=p, scalar2=q, op0=op0, op1=op1)
        self.P.op(eng, fn, reads=_rs([a, s1, s2]), writes=[out.r])

    def stt(self, eng, out, a, s, b, op0, op1, xr=()):
        o, x, p, y = out.ap, a.ap, _a(s), b.ap
        eng = 'vector'
        self.P.op(eng, lambda e: e.scalar_tensor_tensor(out=o, in0=x, scalar=p, in1=y, op0=op0, op1=op1),
                  reads=_rs([a, s, b] + list(xr)), writes=[out.r])

    def rsqrt(self, out, a, eps, tmp, scale=1.0):
        self.act(tmp, a, AF.Ln, bias=eps, scale=scale)
        self.act(out, tmp, AF.Exp, scale=-0.5)

    def cp(self, eng, out, a):
        o, x = out.ap, a.ap
        if eng == 'scalar':
            self.P.op(eng, lambda e: e.copy(out=o, in_=x), reads=[a.r], writes=[out.r])
        else:
            self.P.op(eng, lambda e: e.tensor_copy(out=o, in_=x), reads=[a.r], writes=[out.r])

    def red(self, out, a, op):
        o, x = out.ap, a.ap
        self.P.op('vector', lambda e: e.tensor_reduce(out=o, in_=x, axis=AX.X, op=op), reads=[a.r], writes=[out.r])

    def recip(self, out, a):
        o, x = out.ap, a.ap
        self.P.op('vector', lambda e: e.reciprocal(out=o, in_=x), reads=[a.r], writes=[out.r])

    def scan(self, out, d0, d1, init):
        o, x, y, z = out.ap, d0.ap, d1.ap, _a(init)
        self.P.op('vector', lambda e: e.tensor_tensor_scan(out=o, data0=x, data1=y, initial=z, op0=ALU.mult,
                                                          op1=ALU.add), reads=_rs([d0, d1, init]), writes=[out.r])

    def memset(self, eng, out, val):
        o = out.ap
        self.P.op(eng, lambda e: e.memset(o, val), writes=[out.r])

    def mm(self, out, lhsT, rhs, start=True, stop=True, xr=()):
        o, l, r = out.ap, lhsT.ap, rhs.ap
        self.P.op('tensor', lambda e: e.matmul(o, l, r, start=start, stop=stop),
                  reads=_rs([lhsT, rhs] + list(xr)), writes=[out.r])

    def tr(self, out, in_, ident):
        o, i, d = out.ap, in_.ap, ident.ap
        self.P.op('tensor', lambda e: e.transpose(o, i, d), reads=_rs([in_, ident]), writes=[out.r])

    def dma(self, q, out, in_, slow=False):
        o, i = out.ap, in_.ap
        kw = dict(allow_slow_non_contiguous=True) if slow else {}
        self.P.op(q, lambda e: e.dma_start(out=o, in_=i, **kw), reads=[in_.r], writes=[out.r], dma_res=out.r)


def host_consts(T):
    p = np.arange(128)
    sq = np.zeros((5, 128, 128), np.float32)
    sq[0] = np.eye(128)
    sq[1] = (p[:, None] > p[None, :])
    same = (p[:, None] // 64) == (p[None, :] // 64)
    mid = (p // 64) * 64 + 31
    s_, c_ = p[:, None], p[None, :]
    sq[2] = (same & (s_ > mid[None, :]) & (s_ <= c_)) * 1.0 - (same & (s_ > c_) & (s_ <= mid[None, :])) * 1.0
    sq[3] = same & (s_ <= c_)
    sq[4] = same & (s_ > c_)
    csq = np.ascontiguousarray(sq.transpose(1, 0, 2).reshape(128, 640))
    i = np.arange(512)
    mk = np.zeros((8, 128, 512), np.float32)
    for r in range(4):
        mk[r] = (128 * r + p[:, None]) <= i[None, :]
        mk[4 + r] = (128 * r + p[:, None]) < i[None, :]
    t = np.arange(T)
    aug = np.zeros((4, 2, 4, T), np.float32)
    for h in range(4):
        sl = 2.0 ** (-8.0 * (h + 1) / 4)
        aug[h, 0, 0] = -8 * sl * 64 * (t // 64)
        aug[h, 0, 1] = -8 * sl * (t % 64)
        aug[h, 0, 2] = 1
        aug[h, 0, 3] = 1
        aug[h, 1, 0] = 1
        aug[h, 1, 1] = 1
        aug[h, 1, 2] = 8 * sl * 64 * (t // 64)
        aug[h, 1, 3] = 8 * sl * (t % 64)
    return dict(c_sq=csq, c_mask=mk, c_aug=aug)


IN_SHAPES = dict(
    ada_w=[2, 1024, 6144], ada_b=[2, 6144], ln_g=[2, 2, 1024], ln_b=[2, 2, 1024],
    even_w_in=[1, 1024, 3584], even_w_out=[1, 1024, 1024], diff_lambda=[1, 4, 64], diff_gain=[1, 128],
    hgrn_gamma=[3, 512], hgrn_gain=[1, 128], odd_w_in=[1, 1024, 2560], odd_w_out=[1, 1024, 1024],
    conv_w=[1, 4, 512], conv_b=[1, 512], lru_wa=[4, 128, 128], lru_ba=[1, 512], lru_wx=[4, 128, 128],
    lru_bx=[1, 512], lru_lambda=[1, 512], router_w=[1024, 16], router_b=[1, 16],
    moe_w_gate=[2, 16, 1024, 512], moe_w_up=[2, 16, 1024, 512], moe_w_down=[2, 16, 512, 1024],
)


class _Stop(Exception):
    pass


STOP = None


def build(T, plan):
    nc = bass.Bass("TRN2", target_bir_lowering=False)
    b = Bld(nc)
    NT, NCH = T // 128, T // 512

    def din(name, shape, dt=F32):
        return V(nc.dram_tensor(name, list(shape), dt, kind="ExternalInput").ap(), Res(name))

    def dscr(name, shape, dt):
        return V(nc.dram_tensor(name, list(shape), dt).ap(), Res(name))

    I = {k: din(k, s) for k, s in IN_SHAPES.items()}
    x_in = din("x", [T, D])
    c_in = din("c", [1, D])
    c_sq = din("c_sq", [128, 640])
    c_mask = din("c_mask", [8, 128, 512])
    c_aug = din("c_aug", [4, 2, 4, T])
    out = V(nc.dram_tensor("out", [T, D], F32, kind="ExternalOutput").ap(), Res("out"))
    xs = [dscr("xs0", [T, D], F32), dscr("xs1", [T, D], F32)]
    modscr = dscr("modscr", [2, 6144], F32)

    PS = []
    for i in range(4):
        t = b.P.psum(f"ps{i}", [128, 1024], F32)
        PS.append([V(t[:, 0:512], Res(f"ps{i}a")), V(t[:, 512:1024], Res(f"ps{i}b")), t])

    def bank(i):
        return PS[i // 2][i % 2]

    csq = b.sb([128, 640], F32, "csq")
    b.dma('sync', csq, c_sq)
    ident = csq[:, 0:128]
    onesf = b.sb([128, 128], F32, "onesf")
    b.memset('vector', onesf, 1.0)
    onesb = b.sb([128, 128], BF16, "onesb")
    b.memset('vector', onesb, 1.0)

    b.push()
    cT = b.sb([128, 8], F32, "cT")
    b.dma('sync', cT, c_in.re("o (k p) -> p (o k)", p=128), slow=True)
    cTs = b.sb([128, 8], F32, "cTs")
    b.act(cTs, cT, AF.Silu)
    cTb = b.sb([128, 8], BF16, "cTb")
    b.cp('vector', cTb, cTs)
    wsec_pool = b.pool([128, 8, 1024], BF16, 2, "wsec")
    layers_needed = sorted({int(p[-1]) for p in plan})
    for l in layers_needed:
        modrow = b.sb([1, 6144], F32, "modrow")
        adab = b.sb([1, 6144], F32, "adab")
        b.dma('sync', adab, I['ada_b'][l:l + 1, :])
        for sec in range(6):
            ws = wsec_pool()
            b.dma('gpsimd', ws, I['ada_w'][l][:, sec * 1024:(sec + 1) * 1024].re("(k p) n -> p k n", p=128))
            for nh in range(2):
                ps = bank(nh)
                for kc in range(8):
                    b.mm(ps[0:1, :], cTb[:, kc:kc + 1], ws[:, kc, nh * 512:(nh + 1) * 512], kc == 0, kc == 7)
                c0 = sec * 1024 + nh * 512
                b.tt('vector', modrow[0:1, c0:c0 + 512], ps[0:1, :], adab[0:1, c0:c0 + 512], ALU.add)
            if sec in (1, 2, 4, 5):
                b.ts('vector', modrow[0:1, sec * 1024:(sec + 1) * 1024], modrow[0:1, sec * 1024:(sec + 1) * 1024],
                     1.0, ALU.add)
        b.dma('sync', modscr[l:l + 1, :], modrow)
    b.pop()

    xt_pool = b.pool([128, D], F32, 3, "xt")
    hf_pool = b.pool([128, 8, 128], F32, 2, "hf")
    scp = b.sb([128, 8], F32, "scp")
    shv = b.sb([128, 8], F32, "shv")
    gB = b.sb([128, D], F32, "gB")
    lnG = b.sb([128, D], F32, "lnG")
    lnB = b.sb([128, D], F32, "lnB")
    ework = [b.pool([128, D], F32, 2, "ew%d" % i) for i in range(2)]
    small = b.pool([128, 8], F32, 4, "small")

    def load_mod(l, s):
        sec_sh, sec_sc, sec_g = (0, 1, 2) if s == 0 else (3, 4, 5)
        b.dma('sync', shv, modscr[l, sec_sh * 1024:(sec_sh + 1) * 1024].re("(k p) -> p k", p=128), slow=True)
        b.dma('sync', scp, modscr[l, sec_sc * 1024:(sec_sc + 1) * 1024].re("(k p) -> p k", p=128), slow=True)
        b.dma('sync', gB, modscr[l, sec_g * 1024:(sec_g + 1) * 1024].pb())
        b.dma('sync', lnG, I['ln_g'][l, s, :].pb())
        b.dma('sync', lnB, I['ln_b'][l, s, :].pb())

    def prologue(xsrc, tok0, ntiles, hT, col0, router=None):
        for i in range(ntiles):
            t0 = tok0 + i * 128
            xt = xt_pool()
            b.dma('sync', xt, xsrc[t0:t0 + 128, :])
            pa, pb_ = PS[3][0], PS[3][1]
            for kc in range(8):
                pk = (pa if kc < 4 else pb_)[:, (kc % 4) * 128:(kc % 4 + 1) * 128]
                b.tr(pk, xt[:, kc * 128:(kc + 1) * 128], ident)
            hf = hf_pool()
            for kc in range(8):
                pk = (pa if kc < 4 else pb_)[:, (kc % 4) * 128:(kc % 4 + 1) * 128]
                b.act(hf[:, kc, :], pk, AF.Identity, bias=shv[:, kc:kc + 1], scale=scp[:, kc:kc + 1])
            b.cp('vector', hT[:, :, col0 + i * 128:col0 + (i + 1) * 128], hf)
            if router is not None:
                router(hf, col0 + i * 128)

    def epi_a(xsrc, t0, y):
        xe = xt_pool()
        b.dma('sync', xe, xsrc[t0:t0 + 128, :])
        t1 = ework[0]()
        if isinstance(y, tuple):
            b.tt('vector', t1[:, 0:512], y[0], gB[:, 0:512], ALU.mult)
            b.tt('vector', t1[:, 512:1024], y[1], gB[:, 512:1024], ALU.mult)
        else:
            b.tt('vector', t1, y, gB, ALU.mult)
        r = ework[1]()
        b.stt('vector', r, xe, ALPHA, t1, ALU.mult, ALU.add)
        b.act(t1, r, AF.Square)
        st = small()
        b.red(st[:, 0:1], r, ALU.add)
        b.red(st[:, 1:2], t1, ALU.add)
        return (t0, xe, t1, r, st)

    def epi_b(xdst, ctx):
        t0, xe, t1, r, st = ctx
        b.ts('vector', st[:, 2:4], st[:, 0:2], 1.0 / D, ALU.mult)
        b.tt('vector', st[:, 4:5], st[:, 2:3], st[:, 2:3], ALU.mult)
        b.tt('vector', st[:, 5:6], st[:, 3:4], st[:, 4:5], ALU.subtract)
        b.rsqrt(st[:, 6:7], st[:, 5:6], 1e-5, st[:, 7:8])
        b.ts('vector', t1, r, st[:, 2:3], ALU.subtract, st[:, 6:7], ALU.mult)
        b.tt('gpsimd', r, t1, lnG, ALU.mult)
        xo = xe
        b.tt('vector', xo, r, lnB, ALU.add)
        b.dma('sync', xdst[t0:t0 + 128, :], xo)

    def epilogue_seq(xsrc, xdst, tiles, produce=None):
        n = len(tiles)
        ctx = epi_a(xsrc, tiles[0], produce(0))
        for i in range(n):
            nxt = epi_a(xsrc, tiles[i + 1], produce(i + 1)) if i + 1 < n else None
            epi_b(xdst, ctx)
            ctx = nxt

    def moe_sublayer(l, xsrc, xdst):
        b.push()
        load_mod(l, 1)
        TH = min(T, 1024)
        BIG = b.sb([128, 8, TH], BF16, "hTm")
        rw = b.sb([128, 8, 16], F32, "rw")
        b.dma('sync', rw, I['router_w'].re("(k p) e -> p k e", p=128))
        rb = b.sb([1, 16], F32, "rb")
        b.dma('sync', rb, I['router_b'])
        gatesT = b.sb([16, TH], F32, "gatesT")
        sel = b.sb([16, 16, 128], F32, "sel")
        b.memset('vector', sel, 0.0)
        for e in range(16):
            b.dma('sync', sel[e:e + 1, e, :], onesf[e:e + 1, :])
        yacc = b.sb([128, TH // 128, D], F32, "yacc")
        wg_pool = b.pool([128, 8, 512], BF16, 3, "wg")
        wu_pool = b.pool([128, 8, 512], BF16, 3, "wu")
        wd_pool = b.pool([128, 4, D], BF16, 3, "wd")
        s_pool = b.pool([128, 512], BF16, 2, "s")
        t_pool = b.pool([128, 512], BF16, 2, "tg")
        gbs_pool = b.pool([128, 512], F32, 2, "gbs")
        act_pool = b.pool([128, 4, 512], BF16, 2, "actT")
        rt = b.pool([128, 16], F32, 6, "rt")
        r4 = b.pool([128, 4], F32, 8, "r4")
        r1 = b.pool([128, 1], F32, 8, "r1")

        def router(hf, col):
            lg = bank(5)[:, 0:16]
            for kc in range(8):
                b.mm(lg, hf[:, kc, :], rw[:, kc, :], kc == 0, False)
            b.mm(lg, onesf[0:1, :], rb[0:1, :], False, True)
            mx = r1()
            b.red(mx, lg, ALU.max)
            nmx = r1()
            b.ts('vector', nmx, mx, -1.0, ALU.mult)
            ex = rt()
            b.act(ex, lg, AF.Exp, bias=nmx, scale=1.0)
            sm = r1()
            b.red(sm, ex, ALU.add)
            rs = r1()
            b.recip(rs, sm)
            p = rt()
            b.ts('vector', p, ex, rs, ALU.mult)
            pv = p.re("p (g j) -> p g j", j=4)
            prs = []
            for (i0, i1) in ((0, 1), (0, 2), (0, 3), (1, 2), (1, 3), (2, 3)):
                q = r4()
                b.tt('vector', q, pv[:, :, i0], pv[:, :, i1], ALU.add)
                prs.append(q)
            gs = prs[0]
            for q in prs[1:]:
                g2 = r4()
                b.tt('vector', g2, gs, q, ALU.max)
                gs = g2
            gm = r1()
            b.red(gm, gs, ALU.max)
            ing = r4()
            b.ts('vector', ing, gs, gm, ALU.is_equal)
            im1 = r4()
            b.ts('vector', im1, ing, -1.0, ALU.add)
            msk = rt()
            for g in range(4):
                b.ts('vector', msk[:, g * 4:(g + 1) * 4], p[:, g * 4:(g + 1) * 4], ing[:, g:g + 1], ALU.mult,
                     im1[:, g:g + 1], ALU.add)
            tp1 = r1()
            b.red(tp1, msk, ALU.max)
            is1 = rt()
            b.ts('vector', is1, msk, tp1, ALU.is_equal)
            m2 = rt()
            b.stt('vector', m2, is1, -2.0, msk, ALU.mult, ALU.add)
            tp2 = r1()
            b.red(tp2, m2, ALU.max)
            is2 = rt()
            b.ts('vector', is2, m2, tp2, ALU.is_equal)
            sl = rt()
            b.tt('vector', sl, is1, is2, ALU.add)
            den = r1()
            b.tt('vector', den, tp1, tp2, ALU.add)
            rden = r1()
            b.recip(rden, den)
            gt = rt()
            b.stt('vector', gt, p, rden, sl, ALU.mult, ALU.mult)
            gp = bank(5)[0:16, 128:256]
            b.tr(gp, gt, ident)
            b.cp('vector', gatesT[:, col:col + 128], gp)

        def load_w(e):
            wg, wu, wd = wg_pool(), wu_pool(), wd_pool()
            b.dma('gpsimd', wg, I['moe_w_gate'][l, e].re("(k p) f -> p k f", p=128))
            b.dma('gpsimd', wu, I['moe_w_up'][l, e].re("(k p) f -> p k f", p=128))
            b.dma('gpsimd', wd, I['moe_w_down'][l, e].re("(k p) n -> p k n", p=128))
            return wg, wu, wd

        ycnt = [0]
        for half in range(T // TH):
            tok0 = half * TH
            wnext = load_w(0)
            prologue(xsrc, tok0, TH // 128, BIG, 0, router=router)
            nch = TH // 512
            items = [(e, ch) for e in range(16) for ch in range(nch)]
            wcur = {}

            def stage1_begin(e, ch):
                c0 = ch * 512
                gbp = bank(4)
                b.mm(gbp, sel[:, e, :], gatesT[:, c0:c0 + 512], True, True)
                gbs = gbs_pool()
                b.cp('scalar', gbs, gbp)
                return gbs, act_pool()

            def stage1_fc(e, ch, wts, gbs, actT, fc):
                wg, wu, wd = wts
                c0 = ch * 512
                pg, pu = bank(2 * (fc % 2)), bank(2 * (fc % 2) + 1)
                for kc in range(8):
                    b.mm(pg, wg[:, kc, fc * 128:(fc + 1) * 128], BIG[:, kc, c0:c0 + 512], kc == 0, kc == 7)
                for kc in range(8):
                    b.mm(pu, wu[:, kc, fc * 128:(fc + 1) * 128], BIG[:, kc, c0:c0 + 512], kc == 0, kc == 7)
                s_ = s_pool()
                b.act(s_, pg, AF.Silu)
                tg = t_pool()
                b.tt('vector', tg, pu, gbs, ALU.mult)
                b.tt('gpsimd', actT[:, fc, :], s_, tg, ALU.mult)

            def stage2_tile(e, ch, wts, actT, ti):
                wd = wts[2]
                ycnt[0] += 1
                ya, yb = (PS[3][0], PS[3][1]) if ycnt[0] % 2 else (bank(4), bank(5))
                for nh, yp in ((0, ya), (1, yb)):
                    for fc in range(4):
                        b.mm(yp, actT[:, fc, ti * 128:(ti + 1) * 128], wd[:, fc, nh * 512:(nh + 1) * 512],
                             fc == 0, fc == 3)
                tix = ch * 4 + ti
                for nh, yp in ((0, ya), (1, yb)):
                    ys = yacc[:, tix, nh * 512:(nh + 1) * 512]
                    if e == 0:
                        b.cp('vector', ys, yp)
                    else:
                        b.tt('vector', ys, ys, yp, ALU.add)

            prev = None
            for (e, ch) in items:
                if ch == 0:
                    wcur[e] = wnext
                    if e + 1 < 16:
                        wnext = load_w(e + 1)
                gbs, actT = stage1_begin(e, ch)
                for fc in range(4):
                    stage1_fc(e, ch, wcur[e], gbs, actT, fc)
                    if prev is not None:
                        stage2_tile(*prev, fc)
                prev = (e, ch, wcur[e], actT)
            for ti in range(4):
                stage2_tile(*prev, ti)
            epilogue_seq(xsrc, xdst, [tok0 + i * 128 for i in range(TH // 128)], lambda i: yacc[:, i, :])
        b.pop()

    def load_wsec(pool, win, sec):
        w = pool()
        b.dma('gpsimd', w, win[:, sec * 512:(sec + 1) * 512].re("(k p) n -> p k n", p=128))
        return w

    def proj_heads64(BIG, w, dst, stage_pool):
        for g in range(8):
            stg = stage_pool()
            for ch in range(NCH):
                pq = bank(ch % 2)
                for kc in range(8):
                    b.mm(pq[0:64, :], w[:, kc, g * 64:(g + 1) * 64], BIG[:, kc, ch * 512:(ch + 1) * 512], kc == 0, kc == 7)
                b.cp('scalar' if ch % 2 else 'vector', stg[0:64, ch * 512:(ch + 1) * 512], pq[0:64, :])
            b.dma('sync', dst[g], stg[0:64, :])

    def proj_tok(BIG, w, dst, st_pool):
        for ti in range(NT):
            pv = bank(ti % 2)
            for kc in range(8):
                b.mm(pv, BIG[:, kc, ti * 128:(ti + 1) * 128], w[:, kc, :], kc == 0, kc == 7)
            st = st_pool()
            b.cp('scalar' if ti % 2 else 'vector', st, pv)
            b.dma('sync', dst[ti * 128:(ti + 1) * 128, :], st)

    def outproj_epilogue(BIG, wout, xsrc, xdst):
        wo = b.sb([128, 8, D], BF16, "wo")
        b.dma('gpsimd', wo, wout.re("(k p) n -> p k n", p=128))
        def produce(ti):
            ya, yb = PS[2 + ti % 2][0], PS[2 + ti % 2][1]
            for nh, yp in ((0, ya), (1, yb)):
                for kc in range(8):
                    b.mm(yp, BIG[:, kc, ti * 128:(ti + 1) * 128], wo[:, kc, nh * 512:(nh + 1) * 512], kc == 0, kc == 7)
            return (ya, yb)

        epilogue_seq(xsrc, xdst, [ti * 128 for ti in range(NT)], produce)

    def stop_at(tag):
        if STOP == tag:
            raise _Stop()

    def colvec(src_row, n):
        t = b.sb([128, n], F32, "colv")
        b.dma('sync', t, src_row.re("(c p) -> p c", p=128), slow=True)
        return t

    def odd_sublayer(l, xsrc, xdst):
        j = l // 2
        b.push()
        load_mod(l, 0)
        BIG = b.sb([128, 8, T], BF16, "BIGo")
        prologue(xsrc, 0, NT, BIG, 0)
        win = I['odd_w_in'][j]
        QS = dscr("QS", [8, 64, T], BF16)
        KS = dscr("KS", [8, 64, T], BF16)
        VS = dscr("VS", [T, 512], BF16)
        YR = dscr("YR", [512, T], BF16)
        b.push()
        wpool = b.pool([128, 8, 512], BF16, 2, "wsec")
        cw = b.sb([128, 4, 4], F32, "cw")
        for tap in range(4):
            b.dma('sync', cw[:, tap, :], I['conv_w'][j][tap].re("(c p) -> p c", p=128), slow=True)
        cbv = colvec(I['conv_b'][j], 4)
        bav = colvec(I['lru_ba'][j], 4)
        bxv = colvec(I['lru_bx'][j], 4)
        lamv = colvec(I['lru_lambda'][j], 4)
        e1 = b.sb([128, 4], F32, "e1")
        b.act(e1, lamv, AF.Exp, scale=-1.0)
        spl = b.sb([128, 4], F32, "spl")
        b.act(spl, e1, AF.Ln, bias=1.0)
        negc8 = b.sb([128, 4], F32, "negc8")
        b.ts('vector', negc8, spl, -8.0, ALU.mult)
        wa = b.sb([128, 4, 128], F32, "wa")
        b.dma('sync', wa, I['lru_wa'].re("c p q -> p c q"))
        wx = b.sb([128, 4, 128], F32, "wx")
        b.dma('sync', wx, I['lru_wx'].re("c p q -> p c q"))
        wxr = load_wsec(wpool, win, 0)
        wgr = load_wsec(wpool, win, 1)
        hlast = b.sb([128, 4], F32, "hlast")
        b.memset('vector', hlast, 0.0)
        xrp = [b.sb([128, 515], F32, "xrp") for _ in range(4)]
        for cc in range(4):
            b.memset('gpsimd', xrp[cc][:, 0:3], 0.0)
        f_pool = [b.pool([128, 512], F32, 2, "lf%d" % i) for i in range(6)]
        yr_pool = b.pool([128, 512], BF16, 2, "yr")
        for tc in range(NCH):
            tsl = slice(tc * 512, (tc + 1) * 512)
            for cc in range(4):
                px = bank(0)
                for kc in range(8):
                    b.mm(px, wxr[:, kc, cc * 128:(cc + 1) * 128], BIG[:, kc, tsl], kc == 0, kc == 7)
                xp = xrp[cc]
                b.cp('scalar', xp[:, 3:515], px)
                xc = f_pool[0]()
                b.ts('vector', xc, xp[:, 3:515], cw[:, 3, cc:cc + 1], ALU.mult, cbv[:, cc:cc + 1], ALU.add)
                for tap in (2, 1, 0):
                    b.stt('vector', xc, xp[:, tap:tap + 512], cw[:, tap, cc:cc + 1], xc, ALU.mult, ALU.add)
                hal = small()
                b.cp('gpsimd', hal[:, 0:3], xp[:, 512:515])
                b.cp('gpsimd', xp[:, 0:3], hal[:, 0:3])
                pr, pi = bank(1), bank(2)
                b.mm(pr, wa[:, cc, :], xc)
                b.mm(pi, wx[:, cc, :], xc)
                r = f_pool[1]()
                b.act(r, pr, AF.Sigmoid, bias=bav[:, cc:cc + 1])
                ig = f_pool[2]()
                b.act(ig, pi, AF.Sigmoid, bias=bxv[:, cc:cc + 1])
                a = f_pool[3]()
                b.act(a, r, AF.Exp, scale=negc8[:, cc:cc + 1])
                om = r
                b.tt('gpsimd', om, a, a, ALU.mult)
                b.ts('vector', om, om, -1.0, ALU.mult, 1.0, ALU.add)
                b.ts('vector', om, om, 1e-12, ALU.max)
                b.act(om, om, AF.Sqrt)
                b.tt('gpsimd', ig, ig, xc, ALU.mult)
                u = f_pool[4]()
                b.tt('vector', u, ig, om, ALU.mult)
                hh = f_pool[5]()
                b.scan(hh, a, u, 0.0 if tc == 0 else hlast[:, cc:cc + 1])
                b.cp('vector', hlast[:, cc:cc + 1], hh[:, 511:512])
                pg = bank(3)
                for kc in range(8):
                    b.mm(pg, wgr[:, kc, cc * 128:(cc + 1) * 128], BIG[:, kc, tsl], kc == 0, kc == 7)
                g = xc
                b.cp('scalar', g, pg)
                g2 = ig
                b.tt('gpsimd', g2, g, g, ALU.mult)
                b.ts('vector', g2, g2, 0.044715, ALU.mult, 1.0, ALU.add)
                b.tt('gpsimd', g2, g2, g, ALU.mult)
                b.act(g2, g2, AF.Sigmoid, scale=1.5957691216057308)
                b.tt('gpsimd', g2, g2, g, ALU.mult)
                yr = yr_pool()
                b.tt('vector', yr, g2, hh, ALU.mult)
                b.dma('sync', YR[cc * 128:(cc + 1) * 128, tsl], yr)
        stop_at('oA1')
        stage_pool = b.pool([64, T], BF16, 2, "stg")
        w = load_wsec(wpool, win, 2)
        proj_heads64(BIG, w, QS, stage_pool)
        w = load_wsec(wpool, win, 3)
        proj_heads64(BIG, w, KS, stage_pool)
        w = load_wsec(wpool, win, 4)
        proj_tok(BIG, w, VS, b.pool([128, 512], BF16, 2, "vst"))
        b.pop()
        stop_at('oA2')
        b.push()
        for cc in range(4):
            b.dma('sync', BIG[:, cc, :], YR[cc * 128:(cc + 1) * 128, :])
        tge = b.sb([128, 128], F32, "tge")
        b.tt('vector', tge, csq[:, 128:256], csq[:, 0:128], ALU.add)
        triGEb = b.sb([128, 128], BF16, "triGEb")
        b.cp('vector', triGEb, tge)
        zer = b.sb([128, 512], F32, "zer")
        b.memset('vector', zer, 0.0)
        mkS = b.sb([128, 4, 512], BF16, "mkS")
        b.dma('gpsimd', mkS, c_mask[4:8].re("n p q -> p n q"))
        q_pool = b.pool([64, T], BF16, 2, "qT")
        k_pool = b.pool([64, T], BF16, 2, "kT")
        v_pool = b.pool([128, NT, 64], BF16, 2, "vS")
        e_pool = b.pool([128, 512], F32, 3, "e")
        sp_pool = b.pool([128, 512], F32, 3, "sp")
        spb_pool = b.pool([128, 512], BF16, 4, "spb")
        arg_pool = b.pool([128, 512], F32, 4, "arg")
        p_pool = b.pool([128, 512], BF16, 3, "P")
        sfx_pool = b.pool([128, 512], F32, 2, "sfx")
        ost_pool = b.pool([64, 512], BF16, 2, "ost")
        VSv = VS.re("(n p) c -> p n c", p=128)
        blk = [0]
        for h in range(8):
            qT, kT, vv = q_pool(), k_pool(), v_pool()
            b.dma('sync', qT, QS[h])
            b.dma('sync', kT, KS[h])
            b.dma('sync', vv, VSv[:, :, h * 64:(h + 1) * 64])
            for Q in range(NCH):
                qs = slice(Q * 512, (Q + 1) * 512)
                blk[0] += 1
                OT = bank(6 + blk[0] % 2)[0:64, :]
                sfx = sfx_pool()
                Js = list(reversed(range(4 * Q + 4)))
                last = len(Js) - 1
                st_ = {}
                for k in range(-2, last + 1):
                    ia, ib, ic = k + 2, k + 1, k
                    if ia <= last:
                        zt = bank(ia % 2)
                        J = Js[ia]
                        b.mm(zt, kT[:, J * 128:(J + 1) * 128], qT[:, qs])
                        st_[ia] = dict(zt=zt, r=J - 4 * Q, J=J)
                    if ic >= 0:
                        c_ = st_[ic]
                        P = p_pool()
                        b.act(P, c_['arg'], AF.Exp)
                        if c_['r'] >= 0:
                            b.tt('gpsimd', P, P, mkS[:, c_['r'], :], ALU.mult)
                        c_['P'] = P
                    if ia <= last:
                        a_ = st_[ia]
                        e = e_pool()
                        b.act(e, a_['zt'], AF.Exp, scale=0.125)
                        spb = spb_pool()
                        b.act(spb, e, AF.Ln, bias=1.0)
                        if a_['r'] >= 0:
                            b.tt('gpsimd', spb, spb, mkS[:, a_['r'], :], ALU.mult)
                        a_['e'], a_['spb'] = e, spb
                    if 0 <= ib <= last:
                        b_ = st_[ib]
                        ge = bank(2 + ib % 2)
                        b.mm(ge, triGEb, b_['spb'])
                        bt = None
                        if ib < last:
                            bt = bank(4 + ib % 2)
                            b.mm(bt, onesb, b_['spb'])
                        arg = arg_pool()
                        b.stt('vector', arg, b_['zt'], 0.125, zer if ib == 0 else sfx, ALU.mult, ALU.subtract,
                              xr=(b_['e'],))
                        b.tt('vector', arg, arg, ge, ALU.subtract)
                        if ib < last:
                            if ib == 0:
                                b.cp('vector', sfx, bt)
                            else:
                                b.tt('vector', sfx, sfx, bt, ALU.add)
                        b_['arg'] = arg
                    if ic >= 0:
                        c_ = st_[ic]
                        b.mm(OT, vv[:, c_['J'], :], c_['P'], ic == 0, ic == last)
                        del st_[ic]
                if h % 2 == 0:
                    b.cp('vector', BIG[0:64, 4 + h // 2, qs], OT)
                else:
                    ost = ost_pool()
                    b.cp('vector', ost, OT)
                    b.dma('sync', BIG[64:128, 4 + h // 2, qs], ost)
        b.pop()
        stop_at('oB')
        b.push()
        outproj_epilogue(BIG, I['odd_w_out'][j], xsrc, xdst)
        b.pop()
        b.pop()

    def even_sublayer(l, xsrc, xdst):
        import math
        j = l // 2
        lam_init = 0.8 - 0.6 * math.exp(-0.3 * l)
        b.push()
        load_mod(l, 0)
        BIG = b.sb([128, 8, T], BF16, "BIGe")
        prologue(xsrc, 0, NT, BIG, 0)
        win = I['even_w_in'][j]
        QA = dscr("QA", [8, 64, T], BF16)
        KA = dscr("KA", [8, 64, T], BF16)
        VA = dscr("VA", [T, 512], BF16)
        QST = dscr("QST", [512, T], F32)
        KTs = dscr("KTs", [512, T], F32)
        SGT = dscr("SGT", [512, T], BF16)
        LF = dscr("LF", [T, 512], F32)
        KTOK = dscr("KTOK", [T, 512], F32)
        VH = dscr("VH", [T, 512], BF16)
        lbscr = dscr("lbscr", [2, 512], F32)
        gm = b.sb([1, 3, 512], F32, "gm")
        b.dma('sync', gm, I['hgrn_gamma'].re("(o a) c -> o a c", o=1))
        eg = b.sb([1, 3, 512], F32, "eg")
        b.act(eg, gm, AF.Exp)
        den = b.sb([1, 512], F32, "den")
        b.tt('vector', den, eg[:, 0, :], eg[:, 1, :], ALU.add)
        b.tt('vector', den, den, eg[:, 2, :], ALU.add)
        num = b.sb([1, 512], F32, "num")
        b.cp('vector', num, eg[:, 0, :])
        for i in range(1, l + 1):
            b.tt('vector', num, num, eg[:, i, :], ALU.add)
        rden = b.sb([1, 512], F32, "rden")
        b.recip(rden, den)
        lbr = b.sb([1, 2, 512], F32, "lbr")
        b.tt('vector', lbr[:, 0, :], num, rden, ALU.mult)
        b.ts('vector', lbr[:, 1, :], lbr[:, 0, :], -1.0, ALU.mult, 1.0, ALU.add)
        b.dma('sync', lbscr.re("(o a) c -> o a c", o=1), lbr)
        omlF = colvec(lbscr[1], 4)
        lbB = b.sb([128, 512], F32, "lbB")
        b.dma('sync', lbB, lbscr[0].pb())
        omlB = b.sb([128, 512], F32, "omlB")
        b.dma('sync', omlB, lbscr[1].pb())
        dl = b.sb([128, 256], F32, "dl")
        b.dma('sync', dl, I['diff_lambda'][j].re("a d -> (a d)").pb())
        lw = b.sb([128, 128], F32, "lw")
        lsm = b.sb([128, 8], F32, "lsm")
        b.tt('vector', lw[:, 0:64], dl[:, 0:64], dl[:, 64:128], ALU.mult)
        b.tt('vector', lw[:, 64:128], dl[:, 128:192], dl[:, 192:256], ALU.mult)
        b.red(lsm[:, 0:1], lw[:, 0:64], ALU.add)
        b.red(lsm[:, 1:2], lw[:, 64:128], ALU.add)
        b.act(lsm[:, 2:4], lsm[:, 0:2], AF.Exp)
        b.tt('vector', lsm[:, 4:5], lsm[:, 3:4], lsm[:, 2:3], ALU.subtract)
        b.ts('vector', lsm[:, 5:6], lsm[:, 4:5], -lam_init, ALU.add)
        neglam = lsm[:, 5:6]
        gv = b.sb([128, 4], F32, "gv")
        b.dma('sync', gv[:, 0:1], I['diff_gain'][j].re("(p o) -> p o", o=1), slow=True)
        b.dma('sync', gv[:, 1:2], I['hgrn_gain'][j].re("(p o) -> p o", o=1), slow=True)
        b.ts('vector', gv[:, 2:3], gv[:, 0:1], math.sqrt(128.0) * (1.0 - lam_init), ALU.mult)
        b.ts('vector', gv[:, 3:4], gv[:, 1:2], math.sqrt(128.0), ALU.mult)
        dgS, hgS = gv[:, 2:3], gv[:, 3:4]
        stop_at('e0')
        b.push()
        wpool = b.pool([128, 8, 512], BF16, 2, "wsec")
        stage_pool = b.pool([64, T], BF16, 2, "stg")
        w = load_wsec(wpool, win, 0)
        proj_heads64(BIG, w, QA, stage_pool)
        w = load_wsec(wpool, win, 1)
        proj_heads64(BIG, w, KA, stage_pool)
        st_pool = b.pool([128, 512], BF16, 2, "vst")
        w = load_wsec(wpool, win, 2)
        proj_tok(BIG, w, VA, st_pool)
        w = load_wsec(wpool, win, 5)
        proj_tok(BIG, w, VH, st_pool)
        fst_pool = b.pool([128, 512], F32, 3, "fst")
        bst_pool = b.pool([128, 512], BF16, 2, "bst")
        w = load_wsec(wpool, win, 3)
        for h in range(4):
            for ch in range(NCH):
                pq = bank(ch % 2)
                for kc in range(8):
                    b.mm(pq, w[:, kc, h * 128:(h + 1) * 128], BIG[:, kc, ch * 512:(ch + 1) * 512], kc == 0, kc == 7)
                st = fst_pool()
                b.act(st, pq, AF.Silu)
                b.dma('sync', QST[h * 128:(h + 1) * 128, ch * 512:(ch + 1) * 512], st)
        w = load_wsec(wpool, win, 6)
        for h in range(4):
            for ch in range(NCH):
                pq = bank(ch % 2)
                for kc in range(8):
                    b.mm(pq, w[:, kc, h * 128:(h + 1) * 128], BIG[:, kc, ch * 512:(ch + 1) * 512], kc == 0, kc == 7)
                st = bst_pool()
                b.act(st, pq, AF.Silu)
                b.dma('sync', SGT[h * 128:(h + 1) * 128, ch * 512:(ch + 1) * 512], st)
        w = load_wsec(wpool, win, 4)
        for h in range(4):
            for ch in range(NCH):
                pq = bank(ch % 2)
                for kc in range(8):
                    b.mm(pq, w[:, kc, h * 128:(h + 1) * 128], BIG[:, kc, ch * 512:(ch + 1) * 512], kc == 0, kc == 7)
                sg = fst_pool()
                b.act(sg, pq, AF.Sigmoid, scale=-1.0)
                st = fst_pool()
                b.ts('vector', st, sg, omlF[:, h:h + 1], ALU.mult)
                b.dma('sync', KTs[h * 128:(h + 1) * 128, ch * 512:(ch + 1) * 512], st)
        for ti in range(NT):
            pv = bank(2 + ti % 2)
            for kc in range(8):
                b.mm(pv, BIG[:, kc, ti * 128:(ti + 1) * 128], w[:, kc, :], kc == 0, kc == 7)
            sg = fst_pool()
            b.act(sg, pv, AF.Sigmoid)
            tq = fst_pool()
            b.tt('vector', tq, sg, omlB, ALU.mult)
            b.tt('gpsimd', sg, tq, lbB, ALU.add)
            lf = fst_pool()
            b.act(lf, sg, AF.Ln)
            b.dma('sync', LF[ti * 128:(ti + 1) * 128, :], lf)
            b.tt('gpsimd', sg, omlB, tq, ALU.subtract)
            b.dma('sync', KTOK[ti * 128:(ti + 1) * 128, :], sg)
        b.pop()
        stop_at('eA')
        b.push()
        mkI = b.sb([128, 4, 512], BF16, "mkI")
        b.dma('gpsimd', mkI, c_mask[0:4].re("n p q -> p n q"))
        qT = b.sb([68, 2, T], BF16, "qTa")
        kT = b.sb([68, 2, T], BF16, "kTa")
        vv = b.sb([128, NT, 128], BF16, "vA")
        qTg = V(qT.ap, Res("qaug"))
        kTg = V(kT.ap, Res("kaug"))
        p_pool = b.pool([128, 512], BF16, 4, "Pa")
        rl_pool = b.pool([128, 512], F32, 3, "rl")
        oc_pool = b.pool([128, 512], F32, 4, "oc")
        VAv = VA.re("(n p) c -> p n c", p=128)
        blk = [0]
        for h in range(4):
            for c in range(2):
                b.dma('sync', qT[0:64, c, :], QA[2 * h + c])
                b.dma('sync', kT[0:64, c, :], KA[2 * h + c])
                b.dma('gpsimd', qTg[64:68, c, :], c_aug[h, 0])
                b.dma('gpsimd', kTg[64:68, c, :], c_aug[h, 1])
            b.dma('sync', vv, VAv[:, :, h * 128:(h + 1) * 128])
            for Q in range(NCH):
                qs = slice(Q * 512, (Q + 1) * 512)
                oc = []
                for c in range(2):
                    blk[0] += 1
                    OT, LT = bank(2 + blk[0] % 2), bank(4 + blk[0] % 2)
                    nJ = 4 * Q + 4

                    def s1(J):
                        r = J - 4 * Q
                        st = bank(J % 2)
                        b.mm(st, kT[0:68, c, J * 128:(J + 1) * 128], qT[0:68, c, qs], xr=(qTg, kTg))
                        P = p_pool()
                        if r >= 0:
                            cl = rl_pool()
                            b.ts('vector', cl, st, 0.125, ALU.mult, 80.0, ALU.min)
                            b.act(P, cl, AF.Exp)
                            b.tt('gpsimd', P, P, mkI[:, r, :], ALU.mult)
                        else:
                            b.act(P, st, AF.Exp, scale=0.125)
                        return P

                    Ps = {}
                    for J in range(-2, nJ):
                        if J + 2 < nJ:
                            Ps[J + 2] = s1(J + 2)
                        if J >= 0:
                            P = Ps.pop(J)
                            b.mm(OT, vv[:, J, :], P, J == 0, J == nJ - 1)
                            b.mm(LT, onesb, P, J == 0, J == nJ - 1)
                    rl = rl_pool()
                    b.recip(rl, LT)
                    o_c = oc_pool()
                    b.tt('vector', o_c, OT, rl, ALU.mult)
                    oc.append(o_c)
                o = oc[0]
                b.stt('vector', o, oc[1], neglam, oc[0], ALU.mult, ALU.add)
                sq = oc[1]
                b.tt('gpsimd', sq, o, o, ALU.mult)
                ss = bank(6)
                b.mm(ss, onesf, sq)
                rstd = rl_pool()
                b.rsqrt(rstd, ss, 128e-6, rstd)
                b.stt('vector', BIG[:, h, qs], o, dgS, rstd, ALU.mult, ALU.mult)
        b.pop()
        stop_at('eB')
        b.push()
        hgGT = csq[:, 256:512]
        hgU = csq[:, 512:640]
        tri4 = b.sb([128, 4, 128], F32, "tri4")
        for h in range(4):
            b.cp('gpsimd', tri4[:, h, :], csq[:, 384:512])
        S0 = [b.sb([128, 128], F32, "S0") for _ in range(4)]
        S1 = [b.sb([128, 128], F32, "S1") for _ in range(4)]
        Sb0 = [b.sb([128, 128], BF16, "Sb0") for _ in range(4)]
        Sb1 = [b.sb([128, 128], BF16, "Sb1") for _ in range(4)]
        for h in range(4):
            b.memset('vector', S0[h], 0.0)
            b.memset('gpsimd', Sb0[h], 0.0)
        lf_pool = b.pool([128, 512], F32, 2, "LFt")
        kt_pool = b.pool([128, 512], F32, 2, "Kt")
        vt_pool = b.pool([128, 512], BF16, 2, "Vt")
        qs_pool = b.pool([128, 4, 128], F32, 2, "QsT")
        ktt_pool = b.pool([128, 4, 128], F32, 2, "KTt")
        sg_pool = b.pool([128, 4, 128], BF16, 2, "SGt")
        E_pool = [b.pool([128, 4, 128], F32, 2, "E%d" % i) for i in range(3)]
        E3_pool = b.pool([128, 512], F32, 2, "E3")
        bf_pool = [b.pool([128, 4, 128], BF16, 2, "hb%d" % i) for i in range(3)]
        kd_pool = b.pool([128, 512], BF16, 4, "kd")
        at_pool = b.pool([128, 512], BF16, 2, "attm")
        sq_pool = b.pool([128, 512], F32, 2, "hsq")
        QSTv = QST.re("(h d) t -> d h t", d=128)
        KTv = KTs.re("(h d) t -> d h t", d=128)
        SGTv = SGT.re("(h d) t -> d h t", d=128)
        for ti in range(NT):
            ts_ = slice(ti * 128, (ti + 1) * 128)
            LFt, Kt, Vt = lf_pool(), kt_pool(), vt_pool()
            QsT, KTt, SGt = qs_pool(), ktt_pool(), sg_pool()
            b.dma('sync', LFt, LF[ts_, :])
            b.dma('sync', Kt, KTOK[ts_, :])
            b.dma('sync', Vt, VH[ts_, :])
            b.dma('sync', QsT, QSTv[:, :, ts_])
            b.dma('sync', KTt, KTv[:, :, ts_])
            b.dma('sync', SGt, SGTv[:, :, ts_])
            for h in range(4):
                b.mm(bank(h // 2)[:, (h % 2) * 256:(h % 2 + 1) * 256], LFt[:, h * 128:(h + 1) * 128], hgGT)
            pbu = bank(2)
            b.mm(pbu, hgU, LFt)
            E1, E1i, E2 = E_pool[0](), E_pool[1](), E_pool[2]()
            for hf2 in range(2):
                pv = bank(hf2).re("p (h a c) -> p h a c", h=2, a=2)
                hs = slice(2 * hf2, 2 * hf2 + 2)
                b.act(E1[:, hs, :], pv[:, :, 0, :], AF.Exp)
                b.act(E1i[:, hs, :], pv[:, :, 0, :], AF.Exp, scale=-1.0)
                b.act(E2[:, hs, :], pv[:, :, 1, :], AF.Exp)
            stop_at('c0')
            E3 = E3_pool()
            b.act(E3, pbu, AF.Exp)
            qbT, kbT, qeT = bf_pool[0](), bf_pool[1](), bf_pool[2]()
            b.tt('vector', qbT, QsT, E1, ALU.mult)
            b.tt('gpsimd', kbT, KTt, E1i, ALU.mult)
            b.tt('vector', qeT, QsT, E2, ALU.mult)
            kdA, kdB = kd_pool(), kd_pool()
            b.stt('vector', kdA, Kt, csq[:, 447:448], E3, ALU.mult, ALU.mult)
            b.stt('vector', kdB, Kt, csq[:, 511:512], E3, ALU.mult, ALU.mult)
            stop_at('c1')
            patt = bank(3)
            for h in range(4):
                b.mm(patt[:, h * 128:(h + 1) * 128], kbT[:, h, :], qbT[:, h, :])
            attm = at_pool()
            b.tt('vector', attm, patt, tri4.re("p h c -> p (h c)"), ALU.mult)
            for h in range(4):
                for chk in range(2):
                    reg = bank(4 + h // 2)[:, ((h % 2) * 2 + chk) * 128:((h % 2) * 2 + chk + 1) * 128]
                    b.mm(reg, (kdA, kdB)[chk][:, h * 128:(h + 1) * 128], Vt[:, h * 128:(h + 1) * 128])
            stop_at('c2')
            po = bank(6)
            for h in range(4):
                hc = slice(h * 128, (h + 1) * 128)
                ca = slice(h * 128, h * 128 + 64)
                cb_ = slice(h * 128 + 64, h * 128 + 128)
                b.mm(po[:, ca], Vt[:, hc], attm[:, ca], True, False)
                b.mm(po[:, ca], Sb0[h], qeT[:, h, 0:64], False, True)
                dSa = bank(4 + h // 2)[:, ((h % 2) * 2) * 128:((h % 2) * 2 + 1) * 128]
                dSb = bank(4 + h // 2)[:, ((h % 2) * 2 + 1) * 128:((h % 2) * 2 + 2) * 128]
                b.stt('vector', S1[h], S0[h], E2[:, h, 63:64], dSa, ALU.mult, ALU.add)
                b.cp('gpsimd', Sb1[h], S1[h])
                b.mm(po[:, cb_], Vt[:, hc], attm[:, cb_], True, False)
                b.mm(po[:, cb_], Sb1[h], qeT[:, h, 64:128], False, True)
                b.stt('vector', S0[h], S1[h], E2[:, h, 127:128], dSb, ALU.mult, ALU.add)
                b.cp('gpsimd', Sb0[h], S0[h])
            stop_at('c3')
            sq = sq_pool()
            b.act(sq, po, AF.Square)
            pss = bank(7)
            b.mm(pss, onesf, sq)
            rstd = sq_pool()
            b.rsqrt(rstd, pss, 128e-6, rstd)
            b.stt('vector', sq, po, hgS, rstd, ALU.mult, ALU.mult)
            b.tt('gpsimd', BIG[:, 4:8, ts_], sq.re("p (h c) -> p h c", h=4), SGt, ALU.mult)
        b.pop()
        b.push()
        outproj_epilogue(BIG, I['even_w_out'][j], xsrc, xdst)
        b.pop()
        b.pop()

    cur = x_in
    for si, name in enumerate(plan):
        dst = out if si == len(plan) - 1 else xs[si % 2]
        l = int(name[-1])
        try:
            if name.startswith('moe'):
                moe_sublayer(l, cur, dst)
            elif name.startswith('even'):
                even_sublayer(l, cur, dst)
            elif name.startswith('odd'):
                odd_sublayer(l, cur, dst)
        except _Stop:
            break
        cur = dst
    b.P.barrier()
    b.P.emit()
    return nc, b


def prep_inputs(inputs, T):
    f = lambda a: np.ascontiguousarray(np.asarray(a, dtype=np.float32))
    shared = {}
    for k in IN_SHAPES:
        a = f(inputs[k])
        if k in ('lru_wa', 'lru_wx'):
            bd = np.zeros((4, 128, 128), np.float32)
            for g in range(8):
                bd[g // 2, (g % 2) * 64:(g % 2 + 1) * 64, (g % 2) * 64:(g % 2 + 1) * 64] = a[0, g]
            a = bd
        elif k == 'router_b':
            a = a.reshape(1, 16)
        shared[k] = a
    shared.update(host_consts(T))
    return shared


def kernel(**inputs):
    x = np.asarray(inputs['x'], dtype=np.float32)
    c = np.asarray(inputs['c'], dtype=np.float32)
    Bn, T, _ = x.shape
    shared = prep_inputs(inputs, T)
    nc, _ = build(T, ['even0', 'moe0', 'odd1', 'moe1'])
    in_maps = []
    for i in range(Bn):
        m = dict(shared)
        m['x'] = np.ascontiguousarray(x[i])
        m['c'] = np.ascontiguousarray(c[i:i + 1])
        in_maps.append(m)
    res = run_bass_kernel_spmd(nc, in_maps, core_ids=list(range(Bn)))
    return np.stack([np.asarray(r['out'], dtype=np.float32) for r in res.results], axis=0)
```

```python
import contextlib
import numpy as np
import concourse.bass as bass
import concourse.mybir as mybir
from concourse.bass_utils import run_bass_kernel_spmd

F32 = mybir.dt.float32
BF16 = mybir.dt.bfloat16
AF = mybir.ActivationFunctionType
ALU = mybir.AluOpType
AX = mybir.AxisListType

ENGS = ['tensor', 'vector', 'scalar', 'gpsimd', 'sync']
SEM_LIMIT = 30000
D = 1024
ALPHA = 4.0 ** 0.25
N_CORES = 8


class Res:
    __slots__ = ('name', 'last_w', 'readers', 'ds')

    def __init__(self, name):
        self.name = name
        self.last_w = None
        self.readers = []
        self.ds = {}


class DSem:
    __slots__ = ('name', 'dcount', 'dsem', 'kind')

    def __init__(self, name, kind):
        self.name = name
        self.dcount = 0
        self.dsem = None
        self.kind = kind


class Prog:
    def __init__(self, nc):
        self.nc = nc
        self.ops = {e: [] for e in ENGS}
        self.seen = {x: {e: -1 for e in ENGS} for x in ENGS}
        self.seen_d = {x: {} for x in ENGS}
        self.dma_res = []
        self.free_ds = {'sw': [], 'hw': []}
        self.all_dma_toks = []
        self.stack = contextlib.ExitStack()
        self.pending = {x: [] for x in ENGS}
        self.scopes = []

    def sbuf(self, name, shape, dtype):
        st = self.scopes[-1] if self.scopes else self.stack
        return st.enter_context(self.nc.sbuf_tensor(name, list(shape), dtype))

    def push(self):
        self.scopes.append(contextlib.ExitStack())

    def pop(self):
        self.scopes.pop().close()

    def psum(self, name, shape, dtype):
        return self.stack.enter_context(self.nc.psum_tensor(name, list(shape), dtype))

    def _need(self, eng, tok, waits):
        if tok is None:
            return
        if tok[0] == 'e':
            _, e, idx = tok
            if e == eng and eng == 'tensor':
                return
            if self.seen[eng][e] >= idx:
                return
            self.seen[eng][e] = idx
            waits.append(tok)
        else:
            _, r, val = tok
            if self.seen_d[eng].get(id(r), 0) >= val:
                return
            self.seen_d[eng][id(r)] = val
            waits.append(tok)

    def op(self, eng, fn, reads=(), writes=(), dma_res=None):
        waits = []
        for r in reads:
            self._need(eng, r.last_w, waits)
        for r in writes:
            self._need(eng, r.last_w, waits)
            for t in r.readers:
                self._need(eng, t, waits)
        for t in self.pending[eng]:
            self._need(eng, t, waits)
        self.pending[eng] = []
        idx = len(self.ops[eng])
        if dma_res is not None:
            kind = 'sw' if eng == 'gpsimd' else 'hw'
            ds = dma_res.ds.get(kind)
            if ds is None:
                fl = self.free_ds[kind]
                if fl:
                    ds = fl.pop()
                else:
                    ds = DSem("%s%d" % (kind, len(self.dma_res)), kind)
                    self.dma_res.append(ds)
                dma_res.ds[kind] = ds
            dma_res = ds
            ds.dcount += 16
            tok = ('d', ds, ds.dcount)
            self.all_dma_toks.append(tok)
        else:
            tok = ('e', eng, idx)
        import sys
        fr = sys._getframe(2)
        site = "%s:%d<%s:%d" % (fr.f_code.co_name, fr.f_lineno, fr.f_back.f_code.co_name, fr.f_back.f_lineno)
        self.ops[eng].append(dict(fn=fn, waits=waits, dma_res=dma_res, site=site,
                                  dval=(dma_res.dcount if dma_res else 0), mile=False))
        for r in writes:
            r.last_w = tok
            r.readers = []
        for r in reads:
            if r not in writes:
                r.readers.append(tok)
        return tok

    def barrier(self):
        toks = []
        for e in ENGS:
            for idx in range(len(self.ops[e]) - 1, -1, -1):
                if self.ops[e][idx]['dma_res'] is None:
                    toks.append(('e', e, idx))
                    break
        dtoks = {}
        for t in self.all_dma_toks:
            dtoks[id(t[1])] = t
        self.all_dma_toks = []
        for x in ENGS:
            self.pending[x].extend(list(toks) + list(dtoks.values()))

    def emit(self):
        nc = self.nc
        self.tail = {}
        for x in ENGS:
            w = []
            for t in self.pending[x]:
                self._need(x, t, w)
            self.tail[x] = w
            for t in w:
                if t[0] == 'e':
                    self.ops[t[1]][t[2]]['mile'] = True
        for x in ENGS:
            for o in self.ops[x]:
                for t in o['waits']:
                    if t[0] == 'e':
                        self.ops[t[1]][t[2]]['mile'] = True
        nsem = 0
        for x in ENGS:
            cnt = 0
            cur = None
            for o in self.ops[x]:
                if o['mile'] and o['dma_res'] is None:
                    if cur is None or cnt >= SEM_LIMIT:
                        cur = self.stack.enter_context(nc.semaphore(f"m_{x}_{nsem}"))
                        nsem += 1
                        cnt = 0
                    cnt += 1
                    o['msem'] = (cur, cnt)
        per16 = (SEM_LIMIT // 16) * 16
        for r in self.dma_res:
            n = r.dcount
            r.dsem = []
            for i in range((n + per16 - 1) // per16):
                r.dsem.append(self.stack.enter_context(nc.semaphore(f"d_{r.name}_{i}")))
                nsem += 1
        self.nsem = nsem

        def dsem_of(r, val):
            k = (val - 16) // per16
            return r.dsem[k], val - k * per16

        ops = self.ops
        self.imap = {}
        ctr = [0]

        def body(x):
            def f(eng):
                for o in ops[x]:
                    self.imap[ctr[0] + len(o['waits'])] = (x, o['site'])
                    ctr[0] += len(o['waits']) + 1
                    for t in o['waits']:
                        if t[0] == 'e':
                            s, v = ops[t[1]][t[2]]['msem']
                        else:
                            s, v = dsem_of(t[1], t[2])
                        eng.wait_ge(s, v)
                    inst = o['fn'](eng)
                    if o['dma_res'] is not None:
                        s, v = dsem_of(o['dma_res'], o['dval'])
                        inst.then_inc(s, 16)
                    elif o['mile']:
                        s, v = o['msem']
                        inst.then_inc(s, 1)
                for t in self.tail[x]:
                    if t[0] == 'e':
                        s, v = ops[t[1]][t[2]]['msem']
                    else:
                        s, v = dsem_of(t[1], t[2])
                    eng.wait_ge(s, v)
            return f

        with nc.Block() as block:
            block.tensor(body('tensor'))
            block.vector(body('vector'))
            block.scalar(body('scalar'))
            block.gpsimd(body('gpsimd'))
            block.sync(body('sync'))
        self.stack.close()


class V:
    __slots__ = ('ap', 'r')

    def __init__(self, ap, r):
        self.ap = ap
        self.r = r

    def __getitem__(self, k):
        return V(self.ap[k], self.r)

    def re(self, s, **kw):
        return V(self.ap.rearrange(s, **kw), self.r)

    def pb(self, n=128):
        return V(self.ap.partition_broadcast(n), self.r)


def _rs(xs):
    out = []
    for x in xs:
        if isinstance(x, V) and x.r not in out:
            out.append(x.r)
    return out


def _a(x):
    return x.ap if isinstance(x, V) else x


class Bld:
    ARENA_WORDS = 52992

    def __init__(self, nc):
        self.nc = nc
        self.P = Prog(nc)
        self.n = 0
        self.arena = self.P.stack.enter_context(nc.sbuf_tensor("arena", [128, self.ARENA_WORDS], F32))
        self.off = 0
        self.marks = []
        self.peak = 0
        self.scope_res = [[]]

    def push(self):
        self.marks.append(self.off)
        self.scope_res.append([])

    def pop(self):
        self.off = self.marks.pop()
        self.P.barrier()
        for r in self.scope_res.pop():
            for kind, ds in r.ds.items():
                self.P.free_ds[kind].append(ds)
            r.ds = {}

    def sb(self, shape, dt, name=None):
        self.n += 1
        name = f"{name or 't'}_{self.n}"
        nel = int(np.prod(shape[1:]))
        nbytes = nel * (2 if dt == BF16 else 4)
        words = (nbytes + 63) // 64 * 16
        assert self.off + words <= self.ARENA_WORDS, f"SBUF arena overflow at {name}: {self.off * 4} + {words * 4}"
        ap = self.arena[0:shape[0], self.off:self.off + (nbytes + 3) // 4]
        self.off += words
        self.peak = max(self.peak, self.off * 4)
        if dt == BF16:
            ap = ap.bitcast(BF16)
            if nel % 2:
                ap = ap[:, 0:nel]
        if len(shape) == 3:
            ap = ap.rearrange("p (a b) -> p a b", a=shape[1])
        elif len(shape) == 4:
            ap = ap.rearrange("p (a b c) -> p a b c", a=shape[1], b=shape[2])
        r = Res(name)
        self.scope_res[-1].append(r)
        return V(ap, r)

    def pool(self, shape, dt, n, name=None):
        tiles = [self.sb(shape, dt, name) for _ in range(n)]
        st = [0]

        def nxt():
            st[0] += 1
            return tiles[(st[0] - 1) % n]
        return nxt

    def act(self, out, in_, func, bias=None, scale=None):
        kw = {}
        if bias is not None:
            kw['bias'] = _a(bias)
        if scale is not None:
            kw['scale'] = _a(scale)
        o, i = out.ap, in_.ap
        self.P.op('scalar', lambda e: e.activation(out=o, in_=i, func=func, **kw),
                  reads=_rs([in_, bias, scale]), writes=[out.r])

    def tt(self, eng, out, a, b, op):
        o, x, y = out.ap, a.ap, b.ap
        self.P.op(eng, lambda e: e.tensor_tensor(out=o, in0=x, in1=y, op=op), reads=_rs([a, b]), writes=[out.r])

    def ts(self, eng, out, a, s1, op0, s2=None, op1=None):
        o, x, p, q = out.ap, a.ap, _a(s1), _a(s2)
        if s2 is None:
            fn = lambda e: e.tensor_scalar(out=o, in0=x, scalar1=p, scalar2=None, op0=op0)
        else:
            fn = lambda e: e.tensor_scalar(out=o, in0=x, scalar1=p, scalar2=q, op0=op0, op1=op1)
        self.P.op(eng, fn, reads=_rs([a, s1, s2]), writes=[out.r])

    def stt(self, eng, out, a, s, b, op0, op1, xr=()):
        o, x, p, y = out.ap, a.ap, _a(s), b.ap
        eng = 'vector'
        self.P.op(eng, lambda e: e.scalar_tensor_tensor(out=o, in0=x, scalar=p, in1=y, op0=op0, op1=op1),
                  reads=_rs([a, s, b] + list(xr)), writes=[out.r])

    def rsqrt(self, out, a, eps, tmp, scale=1.0):
        self.act(tmp, a, AF.Ln, bias=eps, scale=scale)
        self.act(out, tmp, AF.Exp, scale=-0.5)

    def cp(self, eng, out, a):
        o, x = out.ap, a.ap
        if eng == 'scalar':
            self.P.op(eng, lambda e: e.copy(out=o, in_=x), reads=[a.r], writes=[out.r])
        else:
            self.P.op(eng, lambda e: e.tensor_copy(out=o, in_=x), reads=[a.r], writes=[out.r])

    def red(self, out, a, op):
        o, x = out.ap, a.ap
        self.P.op('vector', lambda e: e.tensor_reduce(out=o, in_=x, axis=AX.X, op=op), reads=[a.r], writes=[out.r])

    def recip(self, out, a):
        o, x = out.ap, a.ap
        self.P.op('vector', lambda e: e.reciprocal(out=o, in_=x), reads=[a.r], writes=[out.r])

    def scan(self, out, d0, d1, init):
        o, x, y, z = out.ap, d0.ap, d1.ap, _a(init)
        self.P.op('vector', lambda e: e.tensor_tensor_scan(out=o, data0=x, data1=y, initial=z, op0=ALU.mult,
                                                          op1=ALU.add), reads=_rs([d0, d1, init]), writes=[out.r])

    def memset(self, eng, out, val):
        o = out.ap
        self.P.op(eng, lambda e: e.memset(o, val), writes=[out.r])

    def mm(self, out, lhsT, rhs, start=True, stop=True, xr=()):
        o, l, r = out.ap, lhsT.ap, rhs.ap
        self.P.op('tensor', lambda e: e.matmul(o, l, r, start=start, stop=stop),
                  reads=_rs([lhsT, rhs] + list(xr)), writes=[out.r])

    def tr(self, out, in_, ident):
        o, i, d = out.ap, in_.ap, ident.ap
        self.P.op('tensor', lambda e: e.transpose(o, i, d), reads=_rs([in_, ident]), writes=[out.r])

    def dma(self, q, out, in_, slow=False):
        o, i = out.ap, in_.ap
        kw = dict(allow_slow_non_contiguous=True) if slow else {}
        self.P.op(q, lambda e: e.dma_start(out=o, in_=i, **kw), reads=[in_.r], writes=[out.r], dma_res=out.r)


def host_consts(T):
    p = np.arange(128)
    sq = np.zeros((5, 128, 128), np.float32)
    sq[0] = np.eye(128)
    sq[1] = (p[:, None] > p[None, :])
    same = (p[:, None] // 64) == (p[None, :] // 64)
    mid = (p // 64) * 64 + 31
    s_, c_ = p[:, None], p[None, :]
    sq[2] = (same & (s_ > mid[None, :]) & (s_ <= c_)) * 1.0 - (same & (s_ > c_) & (s_ <= mid[None, :])) * 1.0
    sq[3] = same & (s_ <= c_)
    sq[4] = same & (s_ > c_)
    csq = np.ascontiguousarray(sq.transpose(1, 0, 2).reshape(128, 640))
    i = np.arange(512)
    mk = np.zeros((8, 128, 512), np.float32)
    for r in range(4):
        mk[r] = (128 * r + p[:, None]) <= i[None, :]
        mk[4 + r] = (128 * r + p[:, None]) < i[None, :]
    t = np.arange(T)
    aug = np.zeros((4, 2, 4, T), np.float32)
    for h in range(4):
        sl = 2.0 ** (-8.0 * (h + 1) / 4)
        aug[h, 0, 0] = -8 * sl * 64 * (t // 64)
        aug[h, 0, 1] = -8 * sl * (t % 64)
        aug[h, 0, 2] = 1
        aug[h, 0, 3] = 1
        aug[h, 1, 0] = 1
        aug[h, 1, 1] = 1
        aug[h, 1, 2] = 8 * sl * 64 * (t // 64)
        aug[h, 1, 3] = 8 * sl * (t % 64)
    return dict(c_sq=csq, c_mask=mk, c_aug=aug)


IN_SHAPES = dict(
    ada_w=[2, 1024, 6144], ada_b=[2, 6144], ln_g=[2, 2, 1024], ln_b=[2, 2, 1024],
    even_w_in=[1, 1024, 3584], even_w_out=[1, 1024, 1024], diff_lambda=[1, 4, 64], diff_gain=[1, 128],
    hgrn_gamma=[3, 512], hgrn_gain=[1, 128], odd_w_in=[1, 1024, 2560], odd_w_out=[1, 1024, 1024],
    conv_w=[1, 4, 512], conv_b=[1, 512], lru_wa=[4, 128, 128], lru_ba=[1, 512], lru_wx=[4, 128, 128],
    lru_bx=[1, 512], lru_lambda=[1, 512], router_w=[1024, 16], router_b=[1, 16],
    moe_w_gate=[2, 16, 1024, 512], moe_w_up=[2, 16, 1024, 512], moe_w_down=[2, 16, 512, 1024],
)


class _Stop(Exception):
    pass


STOP = None


def build(T, plan):
    nc = bass.Bass("TRN2", target_bir_lowering=False)
    b = Bld(nc)
    NT, NCH = T // 128, T // 512

    def din(name, shape, dt=F32):
        return V(nc.dram_tensor(name, list(shape), dt, kind="ExternalInput").ap(), Res(name))

    def dscr(name, shape, dt):
        return V(nc.dram_tensor(name, list(shape), dt).ap(), Res(name))

    I = {k: din(k, s) for k, s in IN_SHAPES.items()}
    x_in = din("x", [T, D])
    c_in = din("c", [1, D])
    c_sq = din("c_sq", [128, 640])
    c_mask = din("c_mask", [8, 128, 512])
    c_aug = din("c_aug", [4, 2, 4, T])
    out = V(nc.dram_tensor("out", [T, D], F32, kind="ExternalOutput").ap(), Res("out"))
    xs = [dscr("xs0", [T, D], F32), dscr("xs1", [T, D], F32)]
    modscr = dscr("modscr", [2, 6144], F32)

    PS = []
    for i in range(4):
        t = b.P.psum(f"ps{i}", [128, 1024], F32)
        PS.append([V(t[:, 0:512], Res(f"ps{i}a")), V(t[:, 512:1024], Res(f"ps{i}b")), t])

    def bank(i):
        return PS[i // 2][i % 2]

    csq = b.sb([128, 640], F32, "csq")
    b.dma('sync', csq, c_sq)
    ident = csq[:, 0:128]
    onesf = b.sb([128, 128], F32, "onesf")
    b.memset('vector', onesf, 1.0)
    onesb = b.sb([128, 128], BF16, "onesb")
    b.memset('vector', onesb, 1.0)

    b.push()
    cT = b.sb([128, 8], F32, "cT")
    b.dma('sync', cT, c_in.re("o (k p) -> p (o k)", p=128), slow=True)
    cTs = b.sb([128, 8], F32, "cTs")
    b.act(cTs, cT, AF.Silu)
    cTb = b.sb([128, 8], BF16, "cTb")
    b.cp('vector', cTb, cTs)
    wsec_pool = b.pool([128, 8, 1024], BF16, 2, "wsec")
    layers_needed = sorted({int(p[-1]) for p in plan})
    for l in layers_needed:
        modrow = b.sb([1, 6144], F32, "modrow")
        adab = b.sb([1, 6144], F32, "adab")
        b.dma('sync', adab, I['ada_b'][l:l + 1, :])
        for sec in range(6):
            ws = wsec_pool()
            b.dma('gpsimd', ws, I['ada_w'][l][:, sec * 1024:(sec + 1) * 1024].re("(k p) n -> p k n", p=128))
            for nh in range(2):
                ps = bank(nh)
                for kc in range(8):
                    b.mm(ps[0:1, :], cTb[:, kc:kc + 1], ws[:, kc, nh * 512:(nh + 1) * 512], kc == 0, kc == 7)
                c0 = sec * 1024 + nh * 512
                b.tt('vector', modrow[0:1, c0:c0 + 512], ps[0:1, :], adab[0:1, c0:c0 + 512], ALU.add)
            if sec in (1, 2, 4, 5):
                b.ts('vector', modrow[0:1, sec * 1024:(sec + 1) * 1024], modrow[0:1, sec * 1024:(sec + 1) * 1024],
                     1.0, ALU.add)
        b.dma('sync', modscr[l:l + 1, :], modrow)
    b.pop()

    xt_pool = b.pool([128, D], F32, 3, "xt")
    hf_pool = b.pool([128, 8, 128], F32, 2, "hf")
    scp = b.sb([128, 8], F32, "scp")
    shv = b.sb([128, 8], F32, "shv")
    gB = b.sb([128, D], F32, "gB")
    lnG = b.sb([128, D], F32, "lnG")
    lnB = b.sb([128, D], F32, "lnB")
    ework = [b.pool([128, D], F32, 2, "ew%d" % i) for i in range(2)]
    small = b.pool([128, 8], F32, 4, "small")

    def load_mod(l, s):
        sec_sh, sec_sc, sec_g = (0, 1, 2) if s == 0 else (3, 4, 5)
        b.dma('sync', shv, modscr[l, sec_sh * 1024:(sec_sh + 1) * 1024].re("(k p) -> p k", p=128), slow=True)
        b.dma('sync', scp, modscr[l, sec_sc * 1024:(sec_sc + 1) * 1024].re("(k p) -> p k", p=128), slow=True)
        b.dma('sync', gB, modscr[l, sec_g * 1024:(sec_g + 1) * 1024].pb())
        b.dma('sync', lnG, I['ln_g'][l, s, :].pb())
        b.dma('sync', lnB, I['ln_b'][l, s, :].pb())

    def prologue(xsrc, tok0, ntiles, hT, col0, router=None):
        for i in range(ntiles):
            t0 = tok0 + i * 128
            xt = xt_pool()
            b.dma('sync', xt, xsrc[t0:t0 + 128, :])
            pa, pb_ = PS[3][0], PS[3][1]
            for kc in range(8):
                pk = (pa if kc < 4 else pb_)[:, (kc % 4) * 128:(kc % 4 + 1) * 128]
                b.tr(pk, xt[:, kc * 128:(kc + 1) * 128], ident)
            hf = hf_pool()
            for kc in range(8):
                pk = (pa if kc < 4 else pb_)[:, (kc % 4) * 128:(kc % 4 + 1) * 128]
                b.act(hf[:, kc, :], pk, AF.Identity, bias=shv[:, kc:kc + 1], scale=scp[:, kc:kc + 1])
            b.cp('vector', hT[:, :, col0 + i * 128:col0 + (i + 1) * 128], hf)
            if router is not None:
                router(hf, col0 + i * 128)

    def epi_a(xsrc, t0, y):
        xe = xt_pool()
        b.dma('sync', xe, xsrc[t0:t0 + 128, :])
        t1 = ework[0]()
        if isinstance(y, tuple):
            b.tt('vector', t1[:, 0:512], y[0], gB[:, 0:512], ALU.mult)
            b.tt('vector', t1[:, 512:1024], y[1], gB[:, 512:1024], ALU.mult)
        else:
            b.tt('vector', t1, y, gB, ALU.mult)
        r = ework[1]()
        b.stt('vector', r, xe, ALPHA, t1, ALU.mult, ALU.add)
        b.act(t1, r, AF.Square)
        st = small()
        b.red(st[:, 0:1], r, ALU.add)
        b.red(st[:, 1:2], t1, ALU.add)
        return (t0, xe, t1, r, st)

    def epi_b(xdst, ctx):
        t0, xe, t1, r, st = ctx
        b.ts('vector', st[:, 2:4], st[:, 0:2], 1.0 / D, ALU.mult)
        b.tt('vector', st[:, 4:5], st[:, 2:3], st[:, 2:3], ALU.mult)
        b.tt('vector', st[:, 5:6], st[:, 3:4], st[:, 4:5], ALU.subtract)
        b.rsqrt(st[:, 6:7], st[:, 5:6], 1e-5, st[:, 7:8])
        b.ts('vector', t1, r, st[:, 2:3], ALU.subtract, st[:, 6:7], ALU.mult)
        b.tt('gpsimd', r, t1, lnG, ALU.mult)
        xo = xe
        b.tt('vector', xo, r, lnB, ALU.add)
        b.dma('scalar', xdst[t0:t0 + 128, :], xo)

    def epilogue_seq(xsrc, xdst, tiles, produce=None):
        n = len(tiles)
        ctx = epi_a(xsrc, tiles[0], produce(0))
        for i in range(n):
            nxt = epi_a(xsrc, tiles[i + 1], produce(i + 1)) if i + 1 < n else None
            epi_b(xdst, ctx)
            ctx = nxt

    def moe_sublayer(l, xsrc, xdst):
        b.push()
        load_mod(l, 1)
        TH = min(T, 1024)
        BIG = b.sb([128, 8, TH], BF16, "hTm")
        rw = b.sb([128, 8, 16], F32, "rw")
        b.dma('sync', rw, I['router_w'].re("(k p) e -> p k e", p=128))
        rb = b.sb([1, 16], F32, "rb")
        b.dma('sync', rb, I['router_b'])
        gatesT = b.sb([16, TH], F32, "gatesT")
        sel = b.sb([16, 16, 128], F32, "sel")
        b.memset('vector', sel, 0.0)
        for e in range(16):
            b.dma('sync', sel[e:e + 1, e, :], onesf[e:e + 1, :])
        yacc = b.sb([128, TH // 128, D], F32, "yacc")
        wg_pool = b.pool([128, 8, 512], BF16, 3, "wg")
        wu_pool = b.pool([128, 8, 512], BF16, 3, "wu")
        wd_pool = b.pool([128, 4, D], BF16, 3, "wd")
        s_pool = b.pool([128, 512], BF16, 2, "s")
        t_pool = b.pool([128, 512], BF16, 2, "tg")
        gbs_pool = b.pool([128, 512], F32, 2, "gbs")
        act_pool = b.pool([128, 4, 512], BF16, 2, "actT")
        rt = b.pool([128, 16], F32, 6, "rt")
        r4 = b.pool([128, 4], F32, 8, "r4")
        r1 = b.pool([128, 1], F32, 8, "r1")

        def router(hf, col):
            lg = bank(5)[:, 0:16]
            for kc in range(8):
                b.mm(lg, hf[:, kc, :], rw[:, kc, :], kc == 0, False)
            b.mm(lg, onesf[0:1, :], rb[0:1, :], False, True)
            mx = r1()
            b.red(mx, lg, ALU.max)
            nmx = r1()
            b.ts('vector', nmx, mx, -1.0, ALU.mult)
            ex = rt()
            b.act(ex, lg, AF.Exp, bias=nmx, scale=1.0)
            sm = r1()
            b.red(sm, ex, ALU.add)
            rs = r1()
            b.recip(rs, sm)
            p = rt()
            b.ts('vector', p, ex, rs, ALU.mult)
            pv = p.re("p (g j) -> p g j", j=4)
            prs = []
            for (i0, i1) in ((0, 1), (0, 2), (0, 3), (1, 2), (1, 3), (2, 3)):
                q = r4()
                b.tt('vector', q, pv[:, :, i0], pv[:, :, i1], ALU.add)
                prs.append(q)
            gs = prs[0]
            for q in prs[1:]:
                g2 = r4()
                b.tt('vector', g2, gs, q, ALU.max)
                gs = g2
            gm = r1()
            b.red(gm, gs, ALU.max)
            ing = r4()
            b.ts('vector', ing, gs, gm, ALU.is_equal)
            im1 = r4()
            b.ts('vector', im1, ing, -1.0, ALU.add)
            msk = rt()
            for g in range(4):
                b.ts('vector', msk[:, g * 4:(g + 1) * 4], p[:, g * 4:(g + 1) * 4], ing[:, g:g + 1], ALU.mult,
                     im1[:, g:g + 1], ALU.add)
            tp1 = r1()
            b.red(tp1, msk, ALU.max)
            is1 = rt()
            b.ts('vector', is1, msk, tp1, ALU.is_equal)
            m2 = rt()
            b.stt('vector', m2, is1, -2.0, msk, ALU.mult, ALU.add)
            tp2 = r1()
            b.red(tp2, m2, ALU.max)
            is2 = rt()
            b.ts('vector', is2, m2, tp2, ALU.is_equal)
            sl = rt()
            b.tt('vector', sl, is1, is2, ALU.add)
            den = r1()
            b.tt('vector', den, tp1, tp2, ALU.add)
            rden = r1()
            b.recip(rden, den)
            gt = rt()
            b.stt('vector', gt, p, rden, sl, ALU.mult, ALU.mult)
            gp = bank(5)[0:16, 128:256]
            b.tr(gp, gt, ident)
            b.cp('vector', gatesT[:, col:col + 128], gp)

        def load_w(e):
            wg, wu, wd = wg_pool(), wu_pool(), wd_pool()
            b.dma('gpsimd', wg, I['moe_w_gate'][l, e].re("(k p) f -> p k f", p=128))
            b.dma('gpsimd', wu, I['moe_w_up'][l, e].re("(k p) f -> p k f", p=128))
            b.dma('gpsimd', wd, I['moe_w_down'][l, e].re("(k p) n -> p k n", p=128))
            return wg, wu, wd

        ycnt = [0]
        for half in range(T // TH):
            tok0 = half * TH
            wnext = load_w(0)
            prologue(xsrc, tok0, TH // 128, BIG, 0, router=router)
            nch = TH // 512
            items = [(e, ch) for e in range(16) for ch in range(nch)]
            wcur = {}

            def stage1_begin(e, ch):
                c0 = ch * 512
                gbp = bank(4)
                b.mm(gbp, sel[:, e, :], gatesT[:, c0:c0 + 512], True, True)
                gbs = gbs_pool()
                b.cp('scalar', gbs, gbp)
                return gbs, act_pool()

            def stage1_fc(e, ch, wts, gbs, actT, fc):
                wg, wu, wd = wts
                c0 = ch * 512
                pg, pu = bank(2 * (fc % 2)), bank(2 * (fc % 2) + 1)
                for kc in range(8):
                    b.mm(pg, wg[:, kc, fc * 128:(fc + 1) * 128], BIG[:, kc, c0:c0 + 512], kc == 0, kc == 7)
                for kc in range(8):
                    b.mm(pu, wu[:, kc, fc * 128:(fc + 1) * 128], BIG[:, kc, c0:c0 + 512], kc == 0, kc == 7)
                s_ = s_pool()
                b.act(s_, pg, AF.Silu)
                tg = t_pool()
                b.tt('vector', tg, pu, gbs, ALU.mult)
                b.tt('gpsimd', actT[:, fc, :], s_, tg, ALU.mult)

            def stage2_tile(e, ch, wts, actT, ti):
                wd = wts[2]
                ycnt[0] += 1
                ya, yb = (PS[3][0], PS[3][1]) if ycnt[0] % 2 else (bank(4), bank(5))
                for nh, yp in ((0, ya), (1, yb)):
                    for fc in range(4):
                        b.mm(yp, actT[:, fc, ti * 128:(ti + 1) * 128], wd[:, fc, nh * 512:(nh + 1) * 512],
                             fc == 0, fc == 3)
                tix = ch * 4 + ti
                for nh, yp in ((0, ya), (1, yb)):
                    ys = yacc[:, tix, nh * 512:(nh + 1) * 512]
                    if e == 0:
                        b.cp('vector', ys, yp)
                    else:
                        b.tt('vector', ys, ys, yp, ALU.add)

            prev = None
            for (e, ch) in items:
                if ch == 0:
                    wcur[e] = wnext
                    if e + 1 < 16:
                        wnext = load_w(e + 1)
                gbs, actT = stage1_begin(e, ch)
                for fc in range(4):
                    stage1_fc(e, ch, wcur[e], gbs, actT, fc)
                    if prev is not None:
                        stage2_tile(*prev, fc)
                prev = (e, ch, wcur[e], actT)
            for ti in range(4):
                stage2_tile(*prev, ti)
            epilogue_seq(xsrc, xdst, [tok0 + i * 128 for i in range(TH // 128)], lambda i: yacc[:, i, :])
        b.pop()

    def load_wsec(pool, win, sec):
        w = pool()
        b.dma('gpsimd', w, win[:, sec * 512:(sec + 1) * 512].re("(k p) n -> p k n", p=128))
        return w

    def proj_heads64(BIG, w, dst, stage_pool):
        for g in range(8):
            stg = stage_pool()
            for ch in range(NCH):
                pq = bank(ch % 2)
                for kc in range(8):
                    b.mm(pq[0:64, :], w[:, kc, g * 64:(g + 1) * 64], BIG[:, kc, ch * 512:(ch + 1) * 512], kc == 0, kc == 7)
                b.cp('scalar' if ch % 2 else 'vector', stg[0:64, ch * 512:(ch + 1) * 512], pq[0:64, :])
            b.dma('sync', dst[g], stg[0:64, :])

    def proj_tok(BIG, w, dst, st_pool):
        for ti in range(NT):
            pv = bank(ti % 2)
            for kc in range(8):
                b.mm(pv, BIG[:, kc, ti * 128:(ti + 1) * 128], w[:, kc, :], kc == 0, kc == 7)
            st = st_pool()
            b.cp('scalar' if ti % 2 else 'vector', st, pv)
            b.dma('sync', dst[ti * 128:(ti + 1) * 128, :], st)

    def outproj_epilogue(BIG, wout, xsrc, xdst):
        wo = b.sb([128, 8, D], BF16, "wo")
        b.dma('gpsimd', wo, wout.re("(k p) n -> p k n", p=128))
        def produce(ti):
            ya, yb = PS[2 + ti % 2][0], PS[2 + ti % 2][1]
            for nh, yp in ((0, ya), (1, yb)):
                for kc in range(8):
                    b.mm(yp, BIG[:, kc, ti * 128:(ti + 1) * 128], wo[:, kc, nh * 512:(nh + 1) * 512], kc == 0, kc == 7)
            return (ya, yb)

        epilogue_seq(xsrc, xdst, [ti * 128 for ti in range(NT)], produce)

    def stop_at(tag):
        if STOP == tag:
            raise _Stop()

    def colvec(src_row, n):
        t = b.sb([128, n], F32, "colv")
        b.dma('sync', t, src_row.re("(c p) -> p c", p=128), slow=True)
        return t

    def odd_sublayer(l, xsrc, xdst):
        j = l // 2
        b.push()
        load_mod(l, 0)
        BIG = b.sb([128, 8, T], BF16, "BIGo")
        prologue(xsrc, 0, NT, BIG, 0)
        win = I['odd_w_in'][j]
        QS = dscr("QS", [8, 64, T], BF16)
        KS = dscr("KS", [8, 64, T], BF16)
        VS = dscr("VS", [T, 512], BF16)
        YR = dscr("YR", [512, T], BF16)
        b.push()
        wpool = b.pool([128, 8, 512], BF16, 2, "wsec")
        cw = b.sb([128, 4, 4], F32, "cw")
        for tap in range(4):
            b.dma('sync', cw[:, tap, :], I['conv_w'][j][tap].re("(c p) -> p c", p=128), slow=True)
        cbv = colvec(I['conv_b'][j], 4)
        bav = colvec(I['lru_ba'][j], 4)
        bxv = colvec(I['lru_bx'][j], 4)
        lamv = colvec(I['lru_lambda'][j], 4)
        e1 = b.sb([128, 4], F32, "e1")
        b.act(e1, lamv, AF.Exp, scale=-1.0)
        spl = b.sb([128, 4], F32, "spl")
        b.act(spl, e1, AF.Ln, bias=1.0)
        negc8 = b.sb([128, 4], F32, "negc8")
        b.ts('vector', negc8, spl, -8.0, ALU.mult)
        wa = b.sb([128, 4, 128], F32, "wa")
        b.dma('sync', wa, I['lru_wa'].re("c p q -> p c q"))
        wx = b.sb([128, 4, 128], F32, "wx")
        b.dma('sync', wx, I['lru_wx'].re("c p q -> p c q"))
        wxr = load_wsec(wpool, win, 0)
        wgr = load_wsec(wpool, win, 1)
        hlast = b.sb([128, 4], F32, "hlast")
        b.memset('vector', hlast, 0.0)
        xrp = [b.sb([128, 515], F32, "xrp") for _ in range(4)]
        for cc in range(4):
            b.memset('gpsimd', xrp[cc][:, 0:3], 0.0)
        f_pool = [b.pool([128, 512], F32, 2, "lf%d" % i) for i in range(6)]
        yr_pool = b.pool([128, 512], BF16, 2, "yr")
        for tc in range(NCH):
            tsl = slice(tc * 512, (tc + 1) * 512)
            for cc in range(4):
                px = bank(0)
                for kc in range(8):
                    b.mm(px, wxr[:, kc, cc * 128:(cc + 1) * 128], BIG[:, kc, tsl], kc == 0, kc == 7)
                xp = xrp[cc]
                b.cp('scalar', xp[:, 3:515], px)
                xc = f_pool[0]()
                b.ts('vector', xc, xp[:, 3:515], cw[:, 3, cc:cc + 1], ALU.mult, cbv[:, cc:cc + 1], ALU.add)
                for tap in (2, 1, 0):
                    b.stt('vector', xc, xp[:, tap:tap + 512], cw[:, tap, cc:cc + 1], xc, ALU.mult, ALU.add)
                hal = small()
                b.cp('gpsimd', hal[:, 0:3], xp[:, 512:515])
                b.cp('gpsimd', xp[:, 0:3], hal[:, 0:3])
                pr, pi = bank(1), bank(2)
                b.mm(pr, wa[:, cc, :], xc)
                b.mm(pi, wx[:, cc, :], xc)
                r = f_pool[1]()
                b.act(r, pr, AF.Sigmoid, bias=bav[:, cc:cc + 1])
                ig = f_pool[2]()
                b.act(ig, pi, AF.Sigmoid, bias=bxv[:, cc:cc + 1])
                a = f_pool[3]()
                b.act(a, r, AF.Exp, scale=negc8[:, cc:cc + 1])
                om = r
                b.tt('gpsimd', om, a, a, ALU.mult)
                b.ts('vector', om, om, -1.0, ALU.mult, 1.0, ALU.add)
                b.ts('vector', om, om, 1e-12, ALU.max)
                b.act(om, om, AF.Sqrt)
                b.tt('gpsimd', ig, ig, xc, ALU.mult)
                u = f_pool[4]()
                b.tt('vector', u, ig, om, ALU.mult)
                hh = f_pool[5]()
                b.scan(hh, a, u, 0.0 if tc == 0 else hlast[:, cc:cc + 1])
                b.cp('vector', hlast[:, cc:cc + 1], hh[:, 511:512])
                pg = bank(3)
                for kc in range(8):
                    b.mm(pg, wgr[:, kc, cc * 128:(cc + 1) * 128], BIG[:, kc, tsl], kc == 0, kc == 7)
                g = xc
                b.cp('scalar', g, pg)
                g2 = ig
                b.tt('gpsimd', g2, g, g, ALU.mult)
                b.ts('vector', g2, g2, 0.044715, ALU.mult, 1.0, ALU.add)
                b.tt('gpsimd', g2, g2, g, ALU.mult)
                b.act(g2, g2, AF.Sigmoid, scale=1.5957691216057308)
                b.tt('gpsimd', g2, g2, g, ALU.mult)
                yr = yr_pool()
                b.tt('vector', yr, g2, hh, ALU.mult)
                b.dma('sync', YR[cc * 128:(cc + 1) * 128, tsl], yr)
        stop_at('oA1')
        stage_pool = b.pool([64, T], BF16, 2, "stg")
        w = load_wsec(wpool, win, 2)
        proj_heads64(BIG, w, QS, stage_pool)
        w = load_wsec(wpool, win, 3)
        proj_heads64(BIG, w, KS, stage_pool)
        w = load_wsec(wpool, win, 4)
        proj_tok(BIG, w, VS, b.pool([128, 512], BF16, 2, "vst"))
        b.pop()
        stop_at('oA2')
        b.push()
        for cc in range(4):
            b.dma('sync', BIG[:, cc, :], YR[cc * 128:(cc + 1) * 128, :])
        tge = b.sb([128, 128], F32, "tge")
        b.tt('vector', tge, csq[:, 128:256], csq[:, 0:128], ALU.add)
        triGEb = b.sb([128, 128], BF16, "triGEb")
        b.cp('vector', triGEb, tge)
        zer = b.sb([128, 512], F32, "zer")
        b.memset('vector', zer, 0.0)
        mkS = b.sb([128, 4, 512], BF16, "mkS")
        b.dma('gpsimd', mkS, c_mask[4:8].re("n p q -> p n q"))
        q_pool = b.pool([64, T], BF16, 2, "qT")
        k_pool = b.pool([64, T], BF16, 2, "kT")
        v_pool = b.pool([128, NT, 64], BF16, 2, "vS")
        e_pool = b.pool([128, 512], F32, 3, "e")
        sp_pool = b.pool([128, 512], F32, 3, "sp")
        spb_pool = b.pool([128, 512], BF16, 4, "spb")
        arg_pool = b.pool([128, 512], F32, 4, "arg")
        p_pool = b.pool([128, 512], BF16, 3, "P")
        sfx_pool = b.pool([128, 512], F32, 2, "sfx")
        ost_pool = b.pool([64, 512], BF16, 2, "ost")
        VSv = VS.re("(n p) c -> p n c", p=128)
        blk = [0]
        for h in range(8):
            qT, kT, vv = q_pool(), k_pool(), v_pool()
            b.dma('sync', qT, QS[h])
            b.dma('sync', kT, KS[h])
            b.dma('sync', vv, VSv[:, :, h * 64:(h + 1) * 64])
            for Q in range(NCH):
                qs = slice(Q * 512, (Q + 1) * 512)
                blk[0] += 1
                OT = bank(6 + blk[0] % 2)[0:64, :]
                sfx = sfx_pool()
                Js = list(reversed(range(4 * Q + 4)))
                last = len(Js) - 1
                st_ = {}
                for k in range(-2, last + 1):
                    ia, ib, ic = k + 2, k + 1, k
                    if ia <= last:
                        zt = bank(ia % 2)
                        J = Js[ia]
                        b.mm(zt, kT[:, J * 128:(J + 1) * 128], qT[:, qs])
                        st_[ia] = dict(zt=zt, r=J - 4 * Q, J=J)
                    if ic >= 0:
                        c_ = st_[ic]
                        P = p_pool()
                        b.act(P, c_['arg'], AF.Exp)
                        if c_['r'] >= 0:
                            b.tt('gpsimd', P, P, mkS[:, c_['r'], :], ALU.mult)
                        c_['P'] = P
                    if ia <= last:
                        a_ = st_[ia]
                        e = e_pool()
                        b.act(e, a_['zt'], AF.Exp, scale=0.125)
                        spb = spb_pool()
                        b.act(spb, e, AF.Ln, bias=1.0)
                        if a_['r'] >= 0:
                            b.tt('gpsimd', spb, spb, mkS[:, a_['r'], :], ALU.mult)
                        a_['e'], a_['spb'] = e, spb
                    if 0 <= ib <= last:
                        b_ = st_[ib]
                        ge = bank(2 + ib % 2)
                        b.mm(ge, triGEb, b_['spb'])
                        bt = None
                        if ib < last:
                            bt = bank(4 + ib % 2)
                            b.mm(bt, onesb, b_['spb'])
                        arg = arg_pool()
                        b.stt('vector', arg, b_['zt'], 0.125, zer if ib == 0 else sfx, ALU.mult, ALU.subtract,
                              xr=(b_['e'],))
                        b.tt('vector', arg, arg, ge, ALU.subtract)
                        if ib < last:
                            if ib == 0:
                                b.cp('vector', sfx, bt)
                            else:
                                b.tt('vector', sfx, sfx, bt, ALU.add)
                        b_['arg'] = arg
                    if ic >= 0:
                        c_ = st_[ic]
                        b.mm(OT, vv[:, c_['J'], :], c_['P'], ic == 0, ic == last)
                        del st_[ic]
                if h % 2 == 0:
                    b.cp('vector', BIG[0:64, 4 + h // 2, qs], OT)
                else:
                    ost = ost_pool()
                    b.cp('vector', ost, OT)
                    b.dma('sync', BIG[64:128, 4 + h // 2, qs], ost)
        b.pop()
        stop_at('oB')
        b.push()
        outproj_epilogue(BIG, I['odd_w_out'][j], xsrc, xdst)
        b.pop()
        b.pop()

    def even_sublayer(l, xsrc, xdst):
        import math
        j = l // 2
        lam_init = 0.8 - 0.6 * math.exp(-0.3 * l)
        b.push()
        load_mod(l, 0)
        BIG = b.sb([128, 8, T], BF16, "BIGe")
        prologue(xsrc, 0, NT, BIG, 0)
        win = I['even_w_in'][j]
        QA = dscr("QA", [8, 64, T], BF16)
        KA = dscr("KA", [8, 64, T], BF16)
        VA = dscr("VA", [T, 512], BF16)
        QST = dscr("QST", [512, T], F32)
        KTs = dscr("KTs", [512, T], F32)
        SGT = dscr("SGT", [512, T], BF16)
        LF = dscr("LF", [T, 512], F32)
        KTOK = dscr("KTOK", [T, 512], F32)
        VH = dscr("VH", [T, 512], BF16)
        lbscr = dscr("lbscr", [2, 512], F32)
        gm = b.sb([1, 3, 512], F32, "gm")
        b.dma('sync', gm, I['hgrn_gamma'].re("(o a) c -> o a c", o=1))
        eg = b.sb([1, 3, 512], F32, "eg")
        b.act(eg, gm, AF.Exp)
        den = b.sb([1, 512], F32, "den")
        b.tt('vector', den, eg[:, 0, :], eg[:, 1, :], ALU.add)
        b.tt('vector', den, den, eg[:, 2, :], ALU.add)
        num = b.sb([1, 512], F32, "num")
        b.cp('vector', num, eg[:, 0, :])
        for i in range(1, l + 1):
            b.tt('vector', num, num, eg[:, i, :], ALU.add)
        rden = b.sb([1, 512], F32, "rden")
        b.recip(rden, den)
        lbr = b.sb([1, 2, 512], F32, "lbr")
        b.tt('vector', lbr[:, 0, :], num, rden, ALU.mult)
        b.ts('vector', lbr[:, 1, :], lbr[:, 0, :], -1.0, ALU.mult, 1.0, ALU.add)
        b.dma('sync', lbscr.re("(o a) c -> o a c", o=1), lbr)
        omlF = colvec(lbscr[1], 4)
        lbB = b.sb([128, 512], F32, "lbB")
        b.dma('sync', lbB, lbscr[0].pb())
        omlB = b.sb([128, 512], F32, "omlB")
        b.dma('sync', omlB, lbscr[1].pb())
        dl = b.sb([128, 256], F32, "dl")
        b.dma('sync', dl, I['diff_lambda'][j].re("a d -> (a d)").pb())
        lw = b.sb([128, 128], F32, "lw")
        lsm = b.sb([128, 8], F32, "lsm")
        b.tt('vector', lw[:, 0:64], dl[:, 0:64], dl[:, 64:128], ALU.mult)
        b.tt('vector', lw[:, 64:128], dl[:, 128:192], dl[:, 192:256], ALU.mult)
        b.red(lsm[:, 0:1], lw[:, 0:64], ALU.add)
        b.red(lsm[:, 1:2], lw[:, 64:128], ALU.add)
        b.act(lsm[:, 2:4], lsm[:, 0:2], AF.Exp)
        b.tt('vector', lsm[:, 4:5], lsm[:, 3:4], lsm[:, 2:3], ALU.subtract)
        b.ts('vector', lsm[:, 5:6], lsm[:, 4:5], -lam_init, ALU.add)
        neglam = lsm[:, 5:6]
        gv = b.sb([128, 4], F32, "gv")
        b.dma('sync', gv[:, 0:1], I['diff_gain'][j].re("(p o) -> p o", o=1), slow=True)
        b.dma('sync', gv[:, 1:2], I['hgrn_gain'][j].re("(p o) -> p o", o=1), slow=True)
        b.ts('vector', gv[:, 2:3], gv[:, 0:1], math.sqrt(128.0) * (1.0 - lam_init), ALU.mult)
        b.ts('vector', gv[:, 3:4], gv[:, 1:2], math.sqrt(128.0), ALU.mult)
        dgS, hgS = gv[:, 2:3], gv[:, 3:4]
        stop_at('e0')
        b.push()
        wpool = b.pool([128, 8, 512], BF16, 2, "wsec")
        stage_pool = b.pool([64, T], BF16, 2, "stg")
        w = load_wsec(wpool, win, 0)
        proj_heads64(BIG, w, QA, stage_pool)
        w = load_wsec(wpool, win, 1)
        proj_heads64(BIG, w, KA, stage_pool)
        st_pool = b.pool([128, 512], BF16, 2, "vst")
        w = load_wsec(wpool, win, 2)
        proj_tok(BIG, w, VA, st_pool)
        w = load_wsec(wpool, win, 5)
        proj_tok(BIG, w, VH, st_pool)
        fst_pool = b.pool([128, 512], F32, 3, "fst")
        bst_pool = b.pool([128, 512], BF16, 2, "bst")
        w = load_wsec(wpool, win, 3)
        for h in range(4):
            for ch in range(NCH):
                pq = bank(ch % 2)
                for kc in range(8):
                    b.mm(pq, w[:, kc, h * 128:(h + 1) * 128], BIG[:, kc, ch * 512:(ch + 1) * 512], kc == 0, kc == 7)
                st = fst_pool()
                b.act(st, pq, AF.Silu)
                b.dma('sync', QST[h * 128:(h + 1) * 128, ch * 512:(ch + 1) * 512], st)
        w = load_wsec(wpool, win, 6)
        for h in range(4):
            for ch in range(NCH):
                pq = bank(ch % 2)
                for kc in range(8):
                    b.mm(pq, w[:, kc, h * 128:(h + 1) * 128], BIG[:, kc, ch * 512:(ch + 1) * 512], kc == 0, kc == 7)
                st = bst_pool()
                b.act(st, pq, AF.Silu)
                b.dma('sync', SGT[h * 128:(h + 1) * 128, ch * 512:(ch + 1) * 512], st)
        w = load_wsec(wpool, win, 4)
        for h in range(4):
            for ch in range(NCH):
                pq = bank(ch % 2)
                for kc in range(8):
                    b.mm(pq, w[:, kc, h * 128:(h + 1) * 128], BIG[:, kc, ch * 512:(ch + 1) * 512], kc == 0, kc == 7)
                sg = fst_pool()
                b.act(sg, pq, AF.Sigmoid, scale=-1.0)
                st = fst_pool()
                b.ts('vector', st, sg, omlF[:, h:h + 1], ALU.mult)
                b.dma('sync', KTs[h * 128:(h + 1) * 128, ch * 512:(ch + 1) * 512], st)
        for ti in range(NT):
            pv = bank(2 + ti % 2)
            for kc in range(8):
                b.mm(pv, BIG[:, kc, ti * 128:(ti + 1) * 128], w[:, kc, :], kc == 0, kc == 7)
            sg = fst_pool()
            b.act(sg, pv, AF.Sigmoid)
            tq = fst_pool()
            b.tt('vector', tq, sg, omlB, ALU.mult)
            b.tt('gpsimd', sg, tq, lbB, ALU.add)
            lf = fst_pool()
            b.act(lf, sg, AF.Ln)
            b.dma('sync', LF[ti * 128:(ti + 1) * 128, :], lf)
            b.tt('gpsimd', sg, omlB, tq, ALU.subtract)
            b.dma('sync', KTOK[ti * 128:(ti + 1) * 128, :], sg)
        b.pop()
        stop_at('eA')
        b.push()
        mkI = b.sb([128, 4, 512], BF16, "mkI")
        b.dma('gpsimd', mkI, c_mask[0:4].re("n p q -> p n q"))
        qT = b.sb([68, 2, T], BF16, "qTa")
        kT = b.sb([68, 2, T], BF16, "kTa")
        vv = b.sb([128, NT, 128], BF16, "vA")
        qTg = V(qT.ap, Res("qaug"))
        kTg = V(kT.ap, Res("kaug"))
        p_pool = b.pool([128, 512], BF16, 4, "Pa")
        rl_pool = b.pool([128, 512], F32, 3, "rl")
        oc_pool = b.pool([128, 512], F32, 4, "oc")
        VAv = VA.re("(n p) c -> p n c", p=128)
        blk = [0]
        for h in range(4):
            for c in range(2):
                b.dma('sync', qT[0:64, c, :], QA[2 * h + c])
                b.dma('sync', kT[0:64, c, :], KA[2 * h + c])
                b.dma('gpsimd', qTg[64:68, c, :], c_aug[h, 0])
                b.dma('gpsimd', kTg[64:68, c, :], c_aug[h, 1])
            b.dma('sync', vv, VAv[:, :, h * 128:(h + 1) * 128])
            for Q in range(NCH):
                qs = slice(Q * 512, (Q + 1) * 512)
                oc = []
                for c in range(2):
                    blk[0] += 1
                    OT, LT = bank(2 + blk[0] % 2), bank(4 + blk[0] % 2)
                    nJ = 4 * Q + 4

                    def s1(J):
                        r = J - 4 * Q
                        st = bank(J % 2)
                        b.mm(st, kT[0:68, c, J * 128:(J + 1) * 128], qT[0:68, c, qs], xr=(qTg, kTg))
                        P = p_pool()
                        if r >= 0:
                            cl = rl_pool()
                            b.ts('vector', cl, st, 0.125, ALU.mult, 80.0, ALU.min)
                            b.act(P, cl, AF.Exp)
                            b.tt('gpsimd', P, P, mkI[:, r, :], ALU.mult)
                        else:
                            b.act(P, st, AF.Exp, scale=0.125)
                        return P

                    Ps = {}
                    for J in range(-2, nJ):
                        if J + 2 < nJ:
                            Ps[J + 2] = s1(J + 2)
                        if J >= 0:
                            P = Ps.pop(J)
                            b.mm(OT, vv[:, J, :], P, J == 0, J == nJ - 1)
                            b.mm(LT, onesb, P, J == 0, J == nJ - 1)
                    rl = rl_pool()
                    b.recip(rl, LT)
                    o_c = oc_pool()
                    b.tt('vector', o_c, OT, rl, ALU.mult)
                    oc.append(o_c)
                o = oc[0]
                b.stt('vector', o, oc[1], neglam, oc[0], ALU.mult, ALU.add)
                sq = oc[1]
                b.tt('gpsimd', sq, o, o, ALU.mult)
                ss = bank(6)
                b.mm(ss, onesf, sq)
                rstd = rl_pool()
                b.rsqrt(rstd, ss, 128e-6, rstd)
                b.stt('vector', BIG[:, h, qs], o, dgS, rstd, ALU.mult, ALU.mult)
        b.pop()
        stop_at('eB')
        b.push()
        hgGT = csq[:, 256:512]
        hgU = csq[:, 512:640]
        tri4 = b.sb([128, 4, 128], F32, "tri4")
        for h in range(4):
            b.cp('gpsimd', tri4[:, h, :], csq[:, 384:512])
        S0 = [b.sb([128, 128], F32, "S0") for _ in range(4)]
        S1 = [b.sb([128, 128], F32, "S1") for _ in range(4)]
        Sb0 = [b.sb([128, 128], BF16, "Sb0") for _ in range(4)]
        Sb1 = [b.sb([128, 128], BF16, "Sb1") for _ in range(4)]
        for h in range(4):
            b.memset('vector', S0[h], 0.0)
            b.memset('gpsimd', Sb0[h], 0.0)
        lf_pool = b.pool([128, 512], F32, 2, "LFt")
        kt_pool = b.pool([128, 512], F32, 2, "Kt")
        vt_pool = b.pool([128, 512], BF16, 2, "Vt")
        qs_pool = b.pool([128, 4, 128], F32, 2, "QsT")
        ktt_pool = b.pool([128, 4, 128], F32, 2, "KTt")
        sg_pool = b.pool([128, 4, 128], BF16, 2, "SGt")
        E_pool = [b.pool([128, 4, 128], F32, 2, "E%d" % i) for i in range(3)]
        E3_pool = b.pool([128, 512], F32, 2, "E3")
        bf_pool = [b.pool([128, 4, 128], BF16, 2, "hb%d" % i) for i in range(3)]
        kd_pool = b.pool([128, 512], BF16, 4, "kd")
        at_pool = b.pool([128, 512], BF16, 2, "attm")
        sq_pool = b.pool([128, 512], F32, 2, "hsq")
        QSTv = QST.re("(h d) t -> d h t", d=128)
        KTv = KTs.re("(h d) t -> d h t", d=128)
        SGTv = SGT.re("(h d) t -> d h t", d=128)
        for ti in range(NT):
            ts_ = slice(ti * 128, (ti + 1) * 128)
            LFt, Kt, Vt = lf_pool(), kt_pool(), vt_pool()
            QsT, KTt, SGt = qs_pool(), ktt_pool(), sg_pool()
            b.dma('sync', LFt, LF[ts_, :])
            b.dma('sync', Kt, KTOK[ts_, :])
            b.dma('sync', Vt, VH[ts_, :])
            b.dma('sync', QsT, QSTv[:, :, ts_])
            b.dma('sync', KTt, KTv[:, :, ts_])
            b.dma('sync', SGt, SGTv[:, :, ts_])
            for h in range(4):
                b.mm(bank(h // 2)[:, (h % 2) * 256:(h % 2 + 1) * 256], LFt[:, h * 128:(h + 1) * 128], hgGT)
            pbu = bank(2)
            b.mm(pbu, hgU, LFt)
            E1, E1i, E2 = E_pool[0](), E_pool[1](), E_pool[2]()
            for hf2 in range(2):
                pv = bank(hf2).re("p (h a c) -> p h a c", h=2, a=2)
                hs = slice(2 * hf2, 2 * hf2 + 2)
                b.act(E1[:, hs, :], pv[:, :, 0, :], AF.Exp)
                b.act(E1i[:, hs, :], pv[:, :, 0, :], AF.Exp, scale=-1.0)
                b.act(E2[:, hs, :], pv[:, :, 1, :], AF.Exp)
            stop_at('c0')
            E3 = E3_pool()
            b.act(E3, pbu, AF.Exp)
            qbT, kbT, qeT = bf_pool[0](), bf_pool[1](), bf_pool[2]()
            b.tt('vector', qbT, QsT, E1, ALU.mult)
            b.tt('gpsimd', kbT, KTt, E1i, ALU.mult)
            b.tt('vector', qeT, QsT, E2, ALU.mult)
            kdA, kdB = kd_pool(), kd_pool()
            b.stt('vector', kdA, Kt, csq[:, 447:448], E3, ALU.mult, ALU.mult)
            b.stt('vector', kdB, Kt, csq[:, 511:512], E3, ALU.mult, ALU.mult)
            stop_at('c1')
            patt = bank(3)
            for h in range(4):
                b.mm(patt[:, h * 128:(h + 1) * 128], kbT[:, h, :], qbT[:, h, :])
            attm = at_pool()
            b.tt('vector', attm, patt, tri4.re("p h c -> p (h c)"), ALU.mult)
            for h in range(4):
                for chk in range(2):
                    reg = bank(4 + h // 2)[:, ((h % 2) * 2 + chk) * 128:((h % 2) * 2 + chk + 1) * 128]
                    b.mm(reg, (kdA, kdB)[chk][:, h * 128:(h + 1) * 128], Vt[:, h * 128:(h + 1) * 128])
            stop_at('c2')
            po = bank(6)
            for h in range(4):
                hc = slice(h * 128, (h + 1) * 128)
                ca = slice(h * 128, h * 128 + 64)
                cb_ = slice(h * 128 + 64, h * 128 + 128)
                b.mm(po[:, ca], Vt[:, hc], attm[:, ca], True, False)
                b.mm(po[:, ca], Sb0[h], qeT[:, h, 0:64], False, True)
                dSa = bank(4 + h // 2)[:, ((h % 2) * 2) * 128:((h % 2) * 2 + 1) * 128]
                dSb = bank(4 + h // 2)[:, ((h % 2) * 2 + 1) * 128:((h % 2) * 2 + 2) * 128]
                b.stt('vector', S1[h], S0[h], E2[:, h, 63:64], dSa, ALU.mult, ALU.add)
                b.cp('gpsimd', Sb1[h], S1[h])
                b.mm(po[:, cb_], Vt[:, hc], attm[:, cb_], True, False)
                b.mm(po[:, cb_], Sb1[h], qeT[:, h, 64:128], False, True)
                b.stt('vector', S0[h], S1[h], E2[:, h, 127:128], dSb, ALU.mult, ALU.add)
                b.cp('gpsimd', Sb0[h], S0[h])
            stop_at('c3')
            sq = sq_pool()
            b.act(sq, po, AF.Square)
            pss = bank(7)
            b.mm(pss, onesf, sq)
            rstd = sq_pool()
            b.rsqrt(rstd, pss, 128e-6, rstd)
            b.stt('vector', sq, po, hgS, rstd, ALU.mult, ALU.mult)
            b.tt('gpsimd', BIG[:, 4:8, ts_], sq.re("p (h c) -> p h c", h=4), SGt, ALU.mult)
        b.pop()
        b.push()
        outproj_epilogue(BIG, I['even_w_out'][j], xsrc, xdst)
        b.pop()
        b.pop()

    cur = x_in
    for si, name in enumerate(plan):
        dst = out if si == len(plan) - 1 else xs[si % 2]
        l = int(name[-1])
        try:
            if name.startswith('moe'):
                moe_sublayer(l, cur, dst)
            elif name.startswith('even'):
                even_sublayer(l, cur, dst)
            elif name.startswith('odd'):
                odd_sublayer(l, cur, dst)
        except _Stop:
            break
        cur = dst
    b.P.barrier()
    b.P.emit()
    return nc, b


def prep_inputs(inputs, T):
    f = lambda a: np.ascontiguousarray(np.asarray(a, dtype=np.float32))
    shared = {}
    for k in IN_SHAPES:
        a = f(inputs[k])
        if k in ('lru_wa', 'lru_wx'):
            bd = np.zeros((4, 128, 128), np.float32)
            for g in range(8):
                bd[g // 2, (g % 2) * 64:(g % 2 + 1) * 64, (g % 2) * 64:(g % 2 + 1) * 64] = a[0, g]
            a = bd
        elif k == 'router_b':
            a = a.reshape(1, 16)
        shared[k] = a
    shared.update(host_consts(T))
    return shared


def kernel(**inputs):
    x = np.asarray(inputs['x'], dtype=np.float32)
    c = np.asarray(inputs['c'], dtype=np.float32)
    Bn, T, _ = x.shape
    shared = prep_inputs(inputs, T)
    nc, _ = build(T, ['even0', 'moe0', 'odd1', 'moe1'])
    in_maps = []
    for i in range(Bn):
        m = dict(shared)
        m['x'] = np.ascontiguousarray(x[i])
        m['c'] = np.ascontiguousarray(c[i:i + 1])
        in_maps.append(m)
    res = run_bass_kernel_spmd(nc, in_maps, core_ids=list(range(Bn)))
    return np.stack([np.asarray(r['out'], dtype=np.float32) for r in res.results], axis=0)
```
